# Optimizing a Trainium2 kernel written in Bass

```python
import math
import jax, jax.numpy as jnp
from jax import lax
import numpy as np

D_MODEL = 2048
BATCH = 2
SEQ = 8192
DEPTH = 2

CONV_DIM = 1024
CONV_WIDTH = 31
N_HEADS = 16
N_KV_HEADS = 4
HEAD_DIM = 64
WINDOW = 128
BLOCK = 128
ATTN_DIM = N_HEADS * HEAD_DIM
KV_DIM = N_KV_HEADS * HEAD_DIM
N_BRANCHES = 2
IN_DIM = 2 * CONV_DIM + ATTN_DIM + 2 * KV_DIM + N_BRANCHES * D_MODEL
NUM_BUCKETS = 32
MAX_DISTANCE = 128
N_KEYS = 128
N_EXPERTS = N_KEYS * N_KEYS
PEER_HEADS = 8
PEER_TOPK = 16
PEER_QDIM = 256
PEER_HALF = PEER_QDIM // 2
PEER_CHUNK = 128
EPS = 1e-6

kernel_name = 'hybrid_conv_swa_peer_block'


def rms_norm(x, g):
    xf = x.astype(jnp.float32)
    y = xf * lax.rsqrt(jnp.mean(xf * xf, axis=-1, keepdims=True) + EPS)
    return (y * g.astype(jnp.float32)).astype(x.dtype)


def t5_bucket_causal(dist):
    max_exact = NUM_BUCKETS // 2
    d = jnp.maximum(dist, 0)
    large = max_exact + (jnp.log(jnp.maximum(d, 1).astype(jnp.float32) / max_exact)
                         / math.log(MAX_DISTANCE / max_exact)
                         * (NUM_BUCKETS - max_exact)).astype(jnp.int32)
    large = jnp.minimum(large, NUM_BUCKETS - 1)
    return jnp.where(d < max_exact, d, large)


def conformer_conv(a, b, dw_w, dw_b, ln_g, ln_b, w_out):
    u = a * jax.nn.sigmoid(b)
    u = jnp.pad(u, ((0, 0), (CONV_WIDTH - 1, 0), (0, 0)))
    u = lax.conv_general_dilated(u, dw_w[:, None, :], window_strides=(1,), padding='VALID',
                                 dimension_numbers=('NWC', 'WIO', 'NWC'),
                                 feature_group_count=CONV_DIM) + dw_b
    uf = u.astype(jnp.float32)
    mu = jnp.mean(uf, axis=-1, keepdims=True)
    var = jnp.mean(jnp.square(uf - mu), axis=-1, keepdims=True)
    uf = (uf - mu) * lax.rsqrt(var + EPS) * ln_g.astype(jnp.float32) + ln_b.astype(jnp.float32)
    u = jax.nn.silu(uf).astype(a.dtype)
    return u @ w_out


def sliding_window_attention(q, k, v, sinks, rel_bias):
    B, S = q.shape[0], q.shape[1]
    nb = S // BLOCK
    G = N_HEADS // N_KV_HEADS
    qb = q.reshape(B, nb, BLOCK, N_KV_HEADS, G, HEAD_DIM)
    kb = k.reshape(B, nb, BLOCK, N_KV_HEADS, HEAD_DIM)
    vb = v.reshape(B, nb, BLOCK, N_KV_HEADS, HEAD_DIM)
    prev = lambda t: jnp.concatenate([jnp.zeros_like(t[:, :1]), t[:, :-1]], axis=1)
    kk = jnp.concatenate([prev(kb), kb], axis=2)
    vv = jnp.concatenate([prev(vb), vb], axis=2)
    scores = jnp.einsum('bnqkgd,bnskd->bnkgqs', qb, kk).astype(jnp.float32) * (HEAD_DIM ** -0.5)
    qi = jnp.arange(BLOCK)[:, None] + BLOCK
    kj = jnp.arange(2 * BLOCK)[None, :]
    dist = qi - kj
    bias = rel_bias.astype(jnp.float32)[t5_bucket_causal(dist)]
    bias = bias.transpose(2, 0, 1).reshape(N_KV_HEADS, G, BLOCK, 2 * BLOCK)
    band = (dist >= 0) & (dist < WINDOW)
    kpos = jnp.arange(nb)[:, None] * BLOCK - BLOCK + kj
    valid = band[None] & (kpos >= 0)[:, None, :]
    scores = jnp.where(valid[None, :, None, None], scores + bias, -jnp.inf)
    sink = jnp.broadcast_to(sinks.astype(jnp.float32).reshape(N_KV_HEADS, G, 1, 1),
                            scores.shape[:-1] + (1,))
    probs = jax.nn.softmax(jnp.concatenate([scores, sink], axis=-1), axis=-1)[..., :-1]
    out = jnp.einsum('bnkgqs,bnskd->bnqkgd', probs.astype(v.dtype), vv)
    return out.reshape(B, S, ATTN_DIM)


def peer(h, w_pq, sub_keys, u_tab, v_tab):
    B, S, D = h.shape
    q = (h @ w_pq).reshape(B, S, PEER_HEADS, 2, PEER_HALF)
    s = jnp.einsum('bshpd,hpnd->bshpn', q, sub_keys).astype(jnp.float32)
    s_top, i_top = lax.top_k(s, PEER_TOPK)
    cand = s_top[..., 0, :, None] + s_top[..., 1, None, :]
    cand_idx = i_top[..., 0, :, None] * N_KEYS + i_top[..., 1, None, :]
    cand = cand.reshape(B, S, PEER_HEADS, PEER_TOPK * PEER_TOPK)
    cand_idx = cand_idx.reshape(B, S, PEER_HEADS, PEER_TOPK * PEER_TOPK)
    best, pos = lax.top_k(cand, PEER_TOPK)
    idx = jnp.take_along_axis(cand_idx, pos, axis=-1)
    gates = jax.nn.softmax(best, axis=-1)
    T = B * S
    n_chunks = T // PEER_CHUNK
    hc = h.reshape(n_chunks, PEER_CHUNK, D)
    ic = idx.reshape(n_chunks, PEER_CHUNK, PEER_HEADS * PEER_TOPK)
    gc = gates.reshape(n_chunks, PEER_CHUNK, PEER_HEADS * PEER_TOPK).astype(h.dtype)

    def chunk(args):
        xt, it, gt = args
        act = jnp.einsum('td,tkd->tk', xt, u_tab[it])
        w = gt * jax.nn.gelu(act, approximate=False)
        return jnp.einsum('tk,tkd->td', w, v_tab[it])

    out = lax.map(chunk, (hc, ic, gc))
    return out.reshape(B, S, D)


def setup_inputs(seed: int = 0) -> dict:
    key = jax.random.key(seed)
    ks = jax.random.split(key, 24)
    nrm = lambda k, shape, scale: jax.random.normal(k, shape, jnp.float32) * scale
    L, D = DEPTH, D_MODEL
    return {
        'x': nrm(ks[0], (BATCH, SEQ, D), 1.0),
        'c': nrm(ks[1], (BATCH, D), 1.0),
        'rel_bias': nrm(ks[2], (NUM_BUCKETS, N_HEADS), 0.2),
        'ada_w': nrm(ks[3], (L, D, 6 * D), 0.5 * D ** -0.5),
        'ada_b': nrm(ks[4], (L, 6 * D), 0.01),
        'norm1_g': 1.0 + nrm(ks[5], (L, D), 0.02),
        'w_in': nrm(ks[6], (L, D, IN_DIM), D ** -0.5),
        'dw_w': nrm(ks[7], (L, CONV_WIDTH, CONV_DIM), CONV_WIDTH ** -0.5),
        'dw_b': nrm(ks[8], (L, CONV_DIM), 0.01),
        'conv_ln_g': 1.0 + nrm(ks[9], (L, CONV_DIM), 0.02),
        'conv_ln_b': nrm(ks[10], (L, CONV_DIM), 0.01),
        'w_conv_out': nrm(ks[11], (L, CONV_DIM, D), CONV_DIM ** -0.5),
        'attn_sinks': nrm(ks[12], (L, N_HEADS), 0.5),
        'w_attn_out': nrm(ks[13], (L, ATTN_DIM, D), ATTN_DIM ** -0.5),
        'w_out': nrm(ks[14], (L, D, D), D ** -0.5),
        'norm2_g': 1.0 + nrm(ks[15], (L, D), 0.02),
        'w_pq': nrm(ks[16], (L, D, PEER_HEADS * PEER_QDIM), D ** -0.5),
        'sub_keys': nrm(ks[17], (L, PEER_HEADS, 2, N_KEYS, PEER_HALF), PEER_HALF ** -0.5),
        'peer_u': nrm(ks[18], (L, N_EXPERTS, D), D ** -0.5),
        'peer_v': nrm(ks[19], (L, N_EXPERTS, D), PEER_HEADS ** -0.5),
        'final_g': 1.0 + nrm(ks[20], (D,), 0.02),
    }


def reference(x, c, rel_bias, ada_w, ada_b, norm1_g, w_in, dw_w, dw_b, conv_ln_g, conv_ln_b,
              w_conv_out, attn_sinks, w_attn_out, w_out, norm2_g, w_pq, sub_keys, peer_u, peer_v,
              final_g):
    split_pts = [CONV_DIM, 2 * CONV_DIM, 2 * CONV_DIM + ATTN_DIM,
                 2 * CONV_DIM + ATTN_DIM + KV_DIM, 2 * CONV_DIM + ATTN_DIM + 2 * KV_DIM]
    cs = jax.nn.silu(c)
    for l in range(DEPTH):
        mod = cs @ ada_w[l] + ada_b[l]
        sh1, sc1, g1, sh2, sc2, g2 = jnp.split(mod, 6, axis=-1)
        h = rms_norm(x, norm1_g[l]) * (1.0 + sc1[:, None]) + sh1[:, None]
        proj = h @ w_in[l]
        a, b, q, k, v, gate_logits = jnp.split(proj, split_pts, axis=-1)
        y_conv = conformer_conv(a, b, dw_w[l], dw_b[l], conv_ln_g[l], conv_ln_b[l], w_conv_out[l])
        y_attn = sliding_window_attention(q, k, v, attn_sinks[l], rel_bias) @ w_attn_out[l]
        gates = jax.nn.sigmoid(gate_logits.astype(jnp.float32)).astype(x.dtype)
        gate_conv, gate_attn = jnp.split(gates, N_BRANCHES, axis=-1)
        mixed = (gate_conv * y_conv + gate_attn * y_attn) @ w_out[l]
        x = x + g1[:, None] * mixed
        h2 = rms_norm(x, norm2_g[l]) * (1.0 + sc2[:, None]) + sh2[:, None]
        x = x + g2[:, None] * peer(h2, w_pq[l], sub_keys[l], peer_u[l], peer_v[l])
    return rms_norm(x, final_g)
```

```python
import numpy as np
from contextlib import ExitStack
import concourse.bass as bass
import concourse.mybir as mybir
from concourse.bass_utils import run_bass_kernel_spmd

F32 = mybir.dt.float32
BF16 = mybir.dt.bfloat16
AF = mybir.ActivationFunctionType
ALU = mybir.AluOpType
AX = mybir.AxisListType

L = 2
D = 2048
KD = 16
NB = 18
T = NB * 128
GS = 384
NG = T // GS
SEQ = 8192
NEG = -30000.0
EPS = 1e-6
NCH = 128
TB = 3
EG = 4

ENGS = ["pe", "act", "dve", "pool", "sp"]
ENGMAP = {"pe": "tensor", "act": "scalar", "dve": "vector", "pool": "gpsimd", "sp": "sync"}
EPOCH = 1 << 30
NDMASEM = 6


class Prog:
    def __init__(self, nc, stack, block):
        self.nc, self.stack, self.block = nc, stack, block
        self.q = {e: [] for e in ENGS}
        self.cnt = {e: 0 for e in ENGS}
        self.known = {e: {} for e in ENGS}
        self.last_w, self.readers = {}, {}
        self.nsem = 0
        self.sems = {e: self._newsem(e) for e in ENGS}
        self.dsem = {qn: [[self._newsem("d" + qn), 0] for _ in range(NDMASEM)] for qn in ["sp", "act", "pool"]}
        self.dsem_i = {qn: 0 for qn in self.dsem}
        self.n_instr = 0

    def _newsem(self, name):
        self.nsem += 1
        return self.stack.enter_context(self.nc.semaphore(f"s{self.nsem}_{name}"))

    def _deps(self, eng, reads, writes):
        deps = []
        for k in reads:
            deps.extend(self.last_w.get(k, {}).values())
        for k in writes:
            for ev in self.last_w.get(k, {}).values():
                if ev[2] != eng:
                    deps.append(ev)
            for ev in self.readers.get(k, {}).values():
                if ev[2] != eng:
                    deps.append(ev)
        waits = {}
        kn = self.known[eng]
        for (sem, val, src) in deps:
            sid = id(sem)
            if kn.get(sid, 0) >= val:
                continue
            if sid not in waits or waits[sid][1] < val:
                waits[sid] = (sem, val)
        for sid, (sem, val) in waits.items():
            kn[sid] = val
        return list(waits.values())

    def _commit(self, ev, reads, writes):
        sid = id(ev[0])
        for k in writes:
            self.last_w.setdefault(k, {})[sid] = ev
        for k in reads:
            self.readers.setdefault(k, {})[sid] = ev

    def op(self, eng, fn, reads=(), writes=()):
        psr = [k for k in reads if k.startswith("ps") and k[2:].isdigit()]
        if psr:
            writes = list(writes) + psr
        waits = self._deps(eng, reads, writes)
        if self.cnt[eng] >= EPOCH:
            self.sems[eng] = self._newsem(eng)
            self.cnt[eng] = 0
        self.cnt[eng] += 1
        sem = self.sems[eng]
        ev = (sem, self.cnt[eng], eng)
        self.q[eng].append((waits, fn, sem, 1))
        self._commit(ev, reads, writes)
        self.n_instr += 1

    def dma(self, qn, out, in_, reads=(), writes=(), **kw):
        slot = self.dsem[qn][self.dsem_i[qn] % NDMASEM]
        self.dsem_i[qn] += 1
        sem, cnt = slot
        waits = self._deps(qn, reads, writes)
        if cnt > 0 and self.known[qn].get(id(sem), 0) < cnt:
            waits.append((sem, cnt))
            self.known[qn][id(sem)] = cnt
        slot[1] = cnt + 16
        ev = (sem, cnt + 16, "dma_" + qn)
        self.q[qn].append((waits, lambda e: e.dma_start(out=out, in_=in_, **kw), sem, 16))
        self._commit(ev, reads, writes)
        self.n_instr += 1

    def barrier(self):
        evs = [(self.sems[e], self.cnt[e]) for e in ENGS if self.cnt[e] > 0]
        for qn in self.dsem:
            evs += [(s, c) for (s, c) in self.dsem[qn] if c > 0]
        for e in ENGS:
            waits = []
            for (s, v) in evs:
                if s is self.sems[e]:
                    continue
                if self.known[e].get(id(s), 0) < v:
                    waits.append((s, v))
                    self.known[e][id(s)] = v
            if waits:
                self.q[e].append((waits, None, None, 0))

    def flush(self):
        for e in ENGS:
            items, self.q[e] = self.q[e], []
            if not items:
                continue

            def body(engobj, items=items):
                for (waits, fn, sem, inc) in items:
                    for (s, v) in waits:
                        engobj.wait_ge(s, v)
                    if fn is not None:
                        fn(engobj).then_inc(sem, inc)

            getattr(self.block, ENGMAP[e])(body)


def MM(p, out, lhsT, rhs, st, sp, r, w):
    p.op("pe", lambda e: e.matmul(out, lhsT=lhsT, rhs=rhs, start=st, stop=sp), r, w)


def ACTV(p, out, in_, func, r, w, bias=None, scale=None, accum=None):
    kw = {}
    if bias is not None:
        kw["bias"] = bias
    if scale is not None:
        kw["scale"] = scale
    if accum is not None:
        kw["accum_out"] = accum
    p.op("act", lambda e: e.activation(out=out, in_=in_, func=func, **kw), r, w)


def TT(p, eng, out, a, b, op, r, w):
    p.op(eng, lambda e: e.tensor_tensor(out=out, in0=a, in1=b, op=op), r, w)


def TS(p, eng, out, a, s1, op0, r, w, s2=None, op1=None):
    if op1 is None:
        p.op(eng, lambda e: e.tensor_scalar(out=out, in0=a, scalar1=s1, scalar2=None, op0=op0), r, w)
    else:
        p.op(eng, lambda e: e.tensor_scalar(out=out, in0=a, scalar1=s1, scalar2=s2, op0=op0, op1=op1), r, w)


def STT(p, out, a, s, b, op0, op1, r, w):
    p.op("dve", lambda e: e.scalar_tensor_tensor(out=out, in0=a, scalar=s, in1=b, op0=op0, op1=op1), r, w)


def CPY(p, eng, out, in_, r, w):
    if eng == "act":
        p.op("act", lambda e: e.activation(out=out, in_=in_, func=AF.Copy), r, w)
    else:
        p.op(eng, lambda e: e.tensor_copy(out=out, in_=in_), r, w)


class K:
    def __init__(self, dbg=None, nlayers=L):
        self.dbg = dbg
        self.nlayers = nlayers
        self.gstop = 0
        self.geg = 1
        self.in_shapes = {}
        self.only_g = False
        self.g1hp = 16
        self.g1mode = 1
        self.g1var = 0
        self.pipe_out = False
        nc = self.nc = bass.Bass("TRN2", target_bir_lowering=False)

        def din(name, shape, dt=F32):
            self.in_shapes[name] = list(shape)
            return nc.dram_tensor(name, list(shape), dt, kind="ExternalInput").ap()

        def dscr(name, shape, dt):
            kind = "ExternalOutput" if dbg else "Internal"
            return nc.dram_tensor(name, list(shape), dt, kind=kind).ap()

        self.x_in = din("x_in", [T, D])
        self.c_in = din("c_in", [128, KD])
        self.flags = din("flags", [128, 2])
        self.biasT = din("biasT", [128, 16 * 256])
        self.ada_w = din("ada_w", [L * 24, 128, KD * 512])
        self.ada_b = din("ada_b", [L, 6 * D])
        self.n1g = din("n1g", [L, D])
        self.n2g = din("n2g", [L, D])
        self.fing = din("fing", [1, D])
        self.w_in_c = din("w_in_c", [L * 60, 128, D])
        self.w_kd = din("w_kd", [L * 4, 128, D])
        self.w_v = din("w_v", [L, 128, KD * 256])
        self.w_co = din("w_co", [L * 16, 128, 1024])
        self.w_ao = din("w_ao", [L * 16, 128, 1024])
        self.w_o = din("w_o", [L * 4, 128, KD * 512])
        self.w_pq = din("w_pq", [L * 16, 128, D])
        self.skT = din("skT", [L, 128, D])
        self.uT = din("uT", [L * NCH, 128, D])
        self.vv = din("vv", [L * NCH, 128, D])
        self.chanpar = din("chanpar", [L, 128, 8 * 34])
        self.sinks = din("sinks", [L, 16])
        self.y = nc.dram_tensor("y", [16 * 128, D], F32, kind="ExternalOutput").ap()

        self.xres = dscr("xres", [T, D], F32)
        self.modscr = dscr("modscr", [L, 6 * D], F32)
        self.qT_s = dscr("qT_s", [8, 128, T], BF16)
        self.kdT_s = dscr("kdT_s", [4, 128, T], BF16)
        self.v_s = dscr("v_s", [T, 256], BF16)
        self.gT_s = dscr("gT_s", [32, 128, T], BF16)
        self.mixT_s = dscr("mixT_s", [16, 128, T], BF16)
        if dbg:
            self.dbgA = dscr("dbgA", [16, 128, T], BF16)
            self.dbgB = dscr("dbgB", [16, 128, T], BF16)

    def build(self):
        nc = self.nc
        with ExitStack() as st, nc.Block() as block:
            p = self.p = Prog(nc, st, block)
            self.PS = [st.enter_context(nc.psum_tensor(f"ps{i}", [128, 512], F32)) for i in range(8)]
            self.ident = st.enter_context(nc.sbuf_tensor("ident", [128, 128], BF16))
            self.ones = st.enter_context(nc.sbuf_tensor("ones", [128, 128], BF16))
            self.flg = st.enter_context(nc.sbuf_tensor("flg", [128, 2], F32))
            ident, ones = self.ident, self.ones
            p.op("pool", lambda e: e.memset(ident[:], 0.0), [], ["ident"])
            p.op("pool", lambda e: e.affine_select(out=ident[:], in_=ident[:], pattern=[[-1, 128]],
                                                   compare_op=ALU.not_equal, fill=1.0, base=0,
                                                   channel_multiplier=1), ["ident"], ["ident"])
            p.op("pool", lambda e: e.memset(ones[:], 1.0), [], ["ones"])
            p.dma("sp", self.flg[:], self.flags, [], ["flg"])
            if self.only_g:
                self.stage_G(0)
                p.barrier()
                p.flush()
                return nc
            self.stage_mods()
            done = False
            for l in range(self.nlayers):
                for nm in ["A", "B", "C", "D", "E", "F", "G"]:
                    getattr(self, "stage_" + nm)(l)
                    if self.dbg == f"{l}{nm}":
                        done = True
                        break
                if done:
                    break
            p.barrier()
            p.flush()
            if getattr(self, "lst", None) is not None:
                self.lst.close()
        return nc

    def stage_ctx(self):
        self.p.barrier()
        return ExitStack()

    def sb(self, st, name, shape, dt):
        self.uid = getattr(self, "uid", 0) + 1
        return st.enter_context(self.nc.sbuf_tensor(f"{name}_u{self.uid}", list(shape), dt))

    def stage_mods(self):
        p = self.p
        with self.stage_ctx() as st:
            cs = self.sb(st, "cs", [128, KD], F32)
            wb = [self.sb(st, f"adab{i}", [128, KD * 512], F32) for i in range(2)]
            ab = [self.sb(st, f"ab{i}", [1, 512], F32) for i in range(2)]
            mr = [self.sb(st, f"mr{i}", [1, 512], F32) for i in range(2)]
            p.dma("sp", cs[:], self.c_in, [], ["cs"])
            ACTV(p, cs[:], cs[:], AF.Silu, ["cs"], ["cs"])
            for l in range(self.nlayers):
                for n in range(24):
                    i = n % 2
                    p.dma("sp" if i == 0 else "act", wb[i][:], self.ada_w[l * 24 + n], [], [f"adab{i}"])
                    p.dma("sp", ab[i][:], self.ada_b[l:l + 1, n * 512:(n + 1) * 512], [], [f"ab{i}"])
                    ps = self.PS[i]
                    for k in range(KD):
                        MM(p, ps[0:1, :], cs[:, k:k + 1], wb[i][:, k * 512:(k + 1) * 512], k == 0, k == KD - 1,
                           ["cs", f"adab{i}"], [f"ps{i}"])
                    TT(p, "dve", mr[i][:], ps[0:1, :], ab[i][:], ALU.add, [f"ps{i}", f"ab{i}"], [f"mr{i}"])
                    p.dma("sp", self.modscr[l:l + 1, n * 512:(n + 1) * 512], mr[i][:], [f"mr{i}"], ["modscr"])
            p.flush()

    def load_bcast(self, q, dst, src_row, key, reads=()):
        self.p.dma(q, dst, src_row.partition_broadcast(128), list(reads), [key])

    def mod_row(self, l, i):
        return self.modscr[l, i * D:(i + 1) * D]

    def make_gm(self, gm, sh, tmp, tk, l, gain_row, sc_i, sh_i, pfx):
        p = self.p
        self.load_bcast("sp", gm[:], gain_row, pfx + "gm")
        self.load_bcast("sp", tmp[:], self.mod_row(l, sc_i), tk, ["modscr"])
        self.load_bcast("sp", sh[:], self.mod_row(l, sh_i), pfx + "sh", ["modscr"])
        STT(p, gm[:], tmp[:], 1.0, gm[:], ALU.add, ALU.mult, [tk, pfx + "gm"], [pfx + "gm"])

    def norm_tile(self, xt, xk, gm, sh, gk, hb, hk, sm, smk, junk, jk, tmp):
        p = self.p
        ACTV(p, junk, xt, AF.Square, [xk], [jk, smk + "ss"], accum=sm[:, 0:1])
        TS(p, "dve", sm[:, 1:2], sm[:, 0:1], 1.0 / D, ALU.mult, [smk + "ss"], [smk + "a"], s2=EPS, op1=ALU.add)
        ACTV(p, sm[:, 2:3], sm[:, 1:2], AF.Sqrt, [smk + "a"], [smk + "b"])
        p.op("dve", lambda e: e.reciprocal(out=sm[:, 3:4], in_=sm[:, 2:3]), [smk + "b"], [smk + "r"])
        if sh is None:
            STT(p, hb, xt, sm[:, 3:4], gm[:], ALU.mult, ALU.mult, [xk, smk + "r"] + gk, [hk])
        else:
            STT(p, tmp, xt, sm[:, 3:4], gm[:], ALU.mult, ALU.mult, [xk, smk + "r"] + gk, ["ntmp"])
            TT(p, "dve", hb, tmp, sh[:], ALU.add, ["ntmp"] + gk, [hk])

    def transpose_tile(self, hb, hk, dst_fn, dk, bank0):
        p = self.p
        for half in range(2):
            bi = bank0 + half
            psb = self.PS[bi][:].bitcast(BF16)
            for j in range(8):
                k = half * 8 + j
                p.op("pe", lambda e, j=j, k=k, psb=psb: e.transpose(psb[:, j * 128:(j + 1) * 128],
                                                                   hb[:, k * 128:(k + 1) * 128], self.ident[:]),
                     [hk, "ident"], [f"ps{bi}"])
            src = psb[:, 0:1024].rearrange("p (j t) -> p j t", j=8)
            CPY(p, "act" if half == 0 else "dve", dst_fn(half * 8), src, [f"ps{bi}"], [dk])

    def xsrc(self, l):
        return self.x_in if l == 0 else self.xres

    def xkeys(self, t, nq=None):
        return [f"xr{t}_{q}" for q in (range(4) if nq is None else [nq])]

    def stage_B(self, l):
        p = self.p
        self.convT = convT = self.sb(self.lst, "convT", [128, 8, T], BF16)
        with self.stage_ctx() as st:
            hT = self.sb(st, "hT", [128, KD, T], BF16)
            with ExitStack() as st2:
                gm = self.sb(st2, "n1gm", [128, D], F32)
                sh = self.sb(st2, "n1sh", [128, D], F32)
                tmp = self.sb(st2, "ntmp", [128, D], F32)
                self.make_gm(gm, sh, tmp, "ntmp", l, self.n1g[l], 1, 0, "n1")
                xb = [self.sb(st2, f"xb{i}", [128, D], F32) for i in range(2)]
                hb = [self.sb(st2, f"hb{i}", [128, D], BF16) for i in range(2)]
                sm = [self.sb(st2, f"sm{i}", [128, 4], F32) for i in range(2)]
                for t in range(NB):
                    i = t % 2
                    p.dma("sp", xb[i][:], self.xsrc(l)[t * 128:(t + 1) * 128, :], self.xkeys(t), [f"xb{i}"])
                    self.norm_tile(xb[i][:], f"xb{i}", gm, sh, ["n1gm", "n1sh"], hb[i][:], f"hb{i}", sm[i], f"sm{i}",
                                   hb[i][:], f"hb{i}", tmp[:])
                    self.transpose_tile(hb[i], f"hb{i}", lambda k0, t=t: hT[:, k0:k0 + 8, t * 128:(t + 1) * 128],
                                        f"hT{t // 3}", 2 * i)
                p.flush()
            p.barrier()
            if self.dbg == f"{l}A":
                for k in range(KD):
                    p.dma("sp", self.dbgA[k], hT[:, k, :], [f"hT{g}" for g in range(NG)], ["dbgA"])
                p.flush()
                return
            wt = [self.sb(st, f"wt{i}", [128, D], BF16) for i in range(3)]
            stg = [self.sb(st, f"stg{i}", [128, GS], BF16) for i in range(3)]
            sig = [self.sb(st, f"sig{i}", [128, GS], F32) for i in range(2)]
            upad = [self.sb(st, f"upad{i}", [128, 32 + T], F32) for i in range(2)]
            cacc = self.sb(st, "cacc", [128, T], F32)
            cp = self.sb(st, "cp", [128, 8, 34], F32)
            wv = self.sb(st, "wv", [128, KD * 256], BF16)
            vst = [self.sb(st, f"vst{i}", [128, 256], BF16) for i in range(2)]
            p.dma("sp", cp[:], self.chanpar[l].rearrange("p (c t) -> p c t", c=8), [], ["cp"])
            for i in range(2):
                p.op("pool", lambda e, i=i: e.memset(upad[i][:], 0.0), [], [f"upad{i}"])
            state = {"w": 0, "ps": 0, "stg": 0}

            def proj(wsrc, evac):
                wi = state["w"] % 3
                state["w"] += 1
                p.dma("pool", wt[wi][:], wsrc, [], [f"wt{wi}"])
                for g in range(NG):
                    bi = state["ps"] % 4
                    state["ps"] += 1
                    ps = self.PS[bi]
                    for k in range(KD):
                        MM(p, ps[:, 0:GS], wt[wi][:, k * 128:(k + 1) * 128], hT[:, k, g * GS:(g + 1) * GS],
                           k == 0, k == KD - 1, [f"wt{wi}", f"hT{g}"], [f"ps{bi}"])
                    evac(g, ps[:, 0:GS], f"ps{bi}")

            def evac_store(dst, func):
                def f(g, ps, pk):
                    si = state["stg"] % 3
                    state["stg"] += 1
                    ACTV(p, stg[si][:], ps, func, [pk], [f"stg{si}"])
                    p.dma("sp", dst[:, g * GS:(g + 1) * GS], stg[si][:], [f"stg{si}"], ["projout"])
                return f

            for c in range(8):
                proj(self.w_in_c[l * 60 + 16 + c], evac_store(self.qT_s[c], AF.Copy))
            for c in range(4):
                proj(self.w_kd[l * 4 + c], evac_store(self.kdT_s[c], AF.Copy))
            for c in range(32):
                proj(self.w_in_c[l * 60 + 28 + c], evac_store(self.gT_s[c], AF.Sigmoid))
            p.dma("pool", wv[:], self.w_v[l], [], ["wv"])
            for t in range(NB):
                bi = 4 + (t % 2)
                ps = self.PS[bi]
                for k in range(KD):
                    MM(p, ps[:, 0:256], hT[:, k, t * 128:(t + 1) * 128], wv[:, k * 256:(k + 1) * 256],
                       k == 0, k == KD - 1, ["wv", f"hT{t // 3}"], [f"ps{bi}"])
                CPY(p, "act", vst[t % 2][:], ps[:, 0:256], [f"ps{bi}"], [f"vst{t % 2}"])
                p.dma("sp", self.v_s[t * 128:(t + 1) * 128, :], vst[t % 2][:], [f"vst{t % 2}"], ["projout"])
            p.flush()
            for c in range(8):
                ui = c % 2
                up = upad[ui]
                wa = state["w"] % 3
                state["w"] += 1
                wbi = state["w"] % 3
                state["w"] += 1
                p.dma("pool", wt[wa][:], self.w_in_c[l * 60 + c], [], [f"wt{wa}"])
                p.dma("pool", wt[wbi][:], self.w_in_c[l * 60 + 8 + c], [], [f"wt{wbi}"])
                for g in range(NG):
                    ba = (2 * g) % 4
                    bb = (2 * g + 1) % 4
                    for k in range(KD):
                        MM(p, self.PS[ba][:, 0:GS], wt[wa][:, k * 128:(k + 1) * 128], hT[:, k, g * GS:(g + 1) * GS],
                           k == 0, k == KD - 1, [f"wt{wa}", f"hT{g}"], [f"ps{ba}"])
                    for k in range(KD):
                        MM(p, self.PS[bb][:, 0:GS], wt[wbi][:, k * 128:(k + 1) * 128], hT[:, k, g * GS:(g + 1) * GS],
                           k == 0, k == KD - 1, [f"wt{wbi}", f"hT{g}"], [f"ps{bb}"])
                    ACTV(p, sig[g % 2][:], self.PS[bb][:, 0:GS], AF.Sigmoid, [f"ps{bb}"], [f"sig{g % 2}"])
                    TT(p, "dve", up[:, 32 + g * GS:32 + (g + 1) * GS], self.PS[ba][:, 0:GS], sig[g % 2][:], ALU.mult,
                       [f"ps{ba}", f"sig{g % 2}"], [f"upad{ui}"])
                TS(p, "dve", up[:, 32:32 + 256], up[:, 32:32 + 256], self.flg[:, 0:1], ALU.mult,
                   [f"upad{ui}", "flg"], [f"upad{ui}"])
                TS(p, "dve", cacc[:], up[:, 2:2 + T], cp[:, c, 0:1], ALU.mult, [f"upad{ui}", "cp"], ["cacc"],
                   s2=cp[:, c, 31:32], op1=ALU.add)
                for j in range(1, 31):
                    dst = cacc[:] if j < 30 else convT[:, c, :]
                    STT(p, dst, up[:, 2 + j:2 + j + T], cp[:, c, j:j + 1], cacc[:], ALU.mult, ALU.add,
                        [f"upad{ui}", "cp", "cacc"], ["cacc"] if j < 30 else ["convT"])
                p.flush()

    def stage_C(self, l):
        p = self.p
        convT = self.convT
        self.p.barrier()
        self.uactT = uactT = self.sb(self.lst, "uactT", [128, 8, T], BF16)
        with ExitStack() as st2:
            cp = self.sb(st2, "cp2", [128, 8, 34], F32)
            sq = [self.sb(st2, f"sq{i}", [128, GS], BF16) for i in range(2)]
            mean = self.sb(st2, "mean", [128, GS], F32)
            m2 = self.sb(st2, "m2", [128, GS], F32)
            var = self.sb(st2, "var", [128, GS], F32)
            rstd = self.sb(st2, "rstd", [128, GS], F32)
            t1 = [self.sb(st2, f"t1{i}", [128, GS], F32) for i in range(2)]
            t2 = [self.sb(st2, f"t2{i}", [128, GS], F32) for i in range(2)]
            p.dma("sp", cp[:], self.chanpar[l].rearrange("p (c t) -> p c t", c=8), [], ["cp2"])
            for g in range(NG):
                sl = slice(g * GS, (g + 1) * GS)
                b1, b2 = 2 * (g % 2), 2 * (g % 2) + 1
                for c in range(8):
                    MM(p, self.PS[b1][:, 0:GS], self.ones[:], convT[:, c, sl], c == 0, c == 7, ["ones", "convT"],
                       [f"ps{b1}"])
                for c in range(8):
                    ACTV(p, sq[c % 2][:], convT[:, c, sl], AF.Square, ["convT"], [f"sq{c % 2}"])
                    MM(p, self.PS[b2][:, 0:GS], self.ones[:], sq[c % 2][:], c == 0, c == 7, ["ones", f"sq{c % 2}"],
                       [f"ps{b2}"])
                ACTV(p, mean[:], self.PS[b1][:, 0:GS], AF.Copy, [f"ps{b1}"], ["mean"], scale=1.0 / 1024)
                TT(p, "dve", m2[:], mean[:], mean[:], ALU.mult, ["mean"], ["m2"])
                STT(p, var[:], self.PS[b2][:, 0:GS], 1.0 / 1024, m2[:], ALU.mult, ALU.subtract, [f"ps{b2}", "m2"], ["var"])
                TS(p, "dve", var[:], var[:], EPS, ALU.add, ["var"], ["var"])
                ACTV(p, var[:], var[:], AF.Sqrt, ["var"], ["var"])
                p.op("dve", lambda e: e.reciprocal(out=rstd[:], in_=var[:]), ["var"], ["rstd"])
                for c in range(8):
                    i = c % 2
                    TT(p, "dve", t1[i][:], convT[:, c, sl], mean[:], ALU.subtract, ["convT", "mean"], [f"t1{i}"])
                    TT(p, "pool", t2[i][:], t1[i][:], rstd[:], ALU.mult, [f"t1{i}", "rstd"], [f"t2{i}"])
                    ACTV(p, uactT[:, c, sl], t2[i][:], AF.Silu, [f"t2{i}", "cp2"], ["uactT"],
                         bias=cp[:, c, 33:34], scale=cp[:, c, 32:33])
            p.flush()
        if self.dbg == f"{l}C":
            p.barrier()
            for c in range(8):
                p.dma("sp", self.dbgA[c], uactT[:, c, :], ["uactT"], ["dbgA"])
                p.dma("sp", self.dbgB[c], convT[:, c, :], ["convT"], ["dbgB"])
            p.flush()

    def stage_D(self, l):
        p = self.p
        p.barrier()
        self.attnT = attnT = self.sb(self.lst, "attnT", [128, 8, T], BF16)
        p.op("pool", lambda e: e.memset(attnT[:, :, 0:128], 0.0), [], ["attnT"])
        with ExitStack() as st:
            bias = self.sb(st, "bias", [128, 16, 256], F32)
            bias2 = self.sb(st, "bias2", [128, 16, 256], F32)
            snk = self.sb(st, "snk", [128, 16], F32)
            qb = [self.sb(st, f"qb{i}", [128, 8, 128], BF16) for i in range(2)]
            kb = [self.sb(st, f"kb{i}", [128, 4, 256], BF16) for i in range(2)]
            vb = [self.sb(st, f"vb{i}", [128, 2, 256], BF16) for i in range(2)]
            sc = [self.sb(st, f"sc{i}", [128, 2, 256], F32) for i in range(2)]
            pb = [self.sb(st, f"pb{i}", [128, 2, 256], BF16) for i in range(2)]
            pT = [self.sb(st, f"pT{i}", [128, 4, 128], BF16) for i in range(2)]
            sm = [self.sb(st, f"asm{i}", [128, 16], F32) for i in range(2)]
            rs = [self.sb(st, f"rs{i}", [128, 16], F32) for i in range(2)]
            es = [self.sb(st, f"es{i}", [128, 16], F32) for i in range(2)]
            ab = [self.sb(st, f"ab_{i}", [128, 1024], BF16) for i in range(2)]
            p.dma("sp", bias[:], self.biasT.rearrange("p (h s) -> p h s", h=16), [], ["bias"])
            p.dma("sp", bias2[:], self.biasT.rearrange("p (h s) -> p h s", h=16), [], ["bias2"])
            self.load_bcast("sp", snk[:], self.sinks[l], "snk")
            TS(p, "dve", bias2[:, :, 0:128], bias2[:, :, 0:128], self.flg[:, 1:2], ALU.add, ["bias2", "flg"], ["bias2"])
            for b in range(1, NB):
                i = b % 2
                bt, bk = (bias2, "bias2") if b == 2 else (bias, "bias")
                t0 = b * 128
                p.dma("sp", qb[i][:], self.qT_s[:, :, t0:t0 + 128].rearrange("c p t -> p c t"), ["projout"], [f"qb{i}"])
                p.dma("sp", kb[i][:], self.kdT_s[:, :, t0 - 128:t0 + 128].rearrange("c p t -> p c t"), ["projout"],
                      [f"kb{i}"])
                p.dma("sp", vb[i][:], self.v_s[t0 - 128:t0 + 128, :].rearrange("(a p) d -> p a d", p=128), ["projout"],
                      [f"vb{i}"])
                ao_banks = (6, 7)
                for m in range(8):
                    j = m % 2
                    g = m // 2
                    tb_ = 4 + j
                    for hh in range(2):
                        lo = 64 * hh
                        sbk = 2 * j + hh
                        MM(p, self.PS[sbk][:, 0:256], qb[i][lo:lo + 64, m, :], kb[i][lo:lo + 64, g, :],
                           True, True, [f"qb{i}", f"kb{i}"], [f"ps{sbk}"])
                        STT(p, sc[j][:, hh, :], self.PS[sbk][:, 0:256], 0.125, bt[:, 2 * m + hh, :],
                            ALU.mult, ALU.add, [f"ps{sbk}", bk], [f"sc{j}"])
                    p.op("dve", lambda e, j=j, m=m: e.tensor_reduce(out=sm[j][:, 0:2], in_=sc[j][:], axis=AX.X,
                                                                    op=ALU.max), [f"sc{j}"], [f"asm{j}a"])
                    TT(p, "dve", sm[j][:, 2:4], sm[j][:, 0:2], snk[:, 2 * m:2 * m + 2], ALU.max, [f"asm{j}a", "snk"],
                       [f"asm{j}b"])
                    TS(p, "dve", sm[j][:, 4:6], sm[j][:, 2:4], -1.0, ALU.mult, [f"asm{j}b"], [f"asm{j}c"])
                    TT(p, "dve", sm[j][:, 6:8], snk[:, 2 * m:2 * m + 2], sm[j][:, 4:6], ALU.add, ["snk", f"asm{j}c"],
                       [f"asm{j}d"])
                    ACTV(p, es[i][:, 2 * m:2 * m + 2], sm[j][:, 6:8], AF.Exp, [f"asm{j}d"], [f"es{i}"])
                    for hh in range(2):
                        ACTV(p, pb[j][:, hh, :], sc[j][:, hh, :], AF.Exp, [f"sc{j}", f"asm{j}c"], [f"pb{j}", f"rs{i}"],
                             bias=sm[j][:, 4 + hh:5 + hh], accum=rs[i][:, 2 * m + hh:2 * m + hh + 1])
                    psb = self.PS[tb_][:].bitcast(BF16)
                    for hh in range(2):
                        for half in range(2):
                            n = hh * 2 + half
                            p.op("pe", lambda e, psb=psb, n=n, j=j, hh=hh, half=half: e.transpose(
                                psb[:, n * 128:(n + 1) * 128], pb[j][:, hh, half * 128:(half + 1) * 128], self.ident[:]),
                                 [f"pb{j}", "ident"], [f"ps{tb_}"])
                    CPY(p, "act" if j == 0 else "dve", pT[j][:], psb[:, 0:512].rearrange("p (n t) -> p n t", n=4),
                        [f"ps{tb_}"], [f"pT{j}"])
                    for hh in range(2):
                        h = 2 * m + hh
                        bo = ao_banks[h // 8]
                        col = (h % 8) * 64
                        for half in range(2):
                            MM(p, self.PS[bo][:, col:col + 64], pT[j][:, hh * 2 + half, :],
                               vb[i][:, half, g * 64:(g + 1) * 64], half == 0, half == 1,
                               [f"pT{j}", f"vb{i}"], [f"ps{bo}"])
                TT(p, "dve", rs[i][:], rs[i][:], es[i][:], ALU.add, [f"rs{i}", f"es{i}"], [f"rs{i}"])
                p.op("dve", lambda e, i=i: e.reciprocal(out=rs[i][:], in_=rs[i][:]), [f"rs{i}"], [f"rs{i}"])
                for hb_ in range(2):
                    bo = ao_banks[hb_]
                    TT(p, "dve", ab[i][:, hb_ * 512:(hb_ + 1) * 512].rearrange("p (h d) -> p h d", h=8),
                       self.PS[bo][:].rearrange("p (h d) -> p h d", h=8),
                       rs[i][:, hb_ * 8:(hb_ + 1) * 8].unsqueeze(2).to_broadcast([128, 8, 64]), ALU.mult,
                       [f"ps{bo}", f"rs{i}"], [f"ab_{i}"])
                psb = self.PS[4 + i][:].bitcast(BF16)
                for k in range(8):
                    p.op("pe", lambda e, psb=psb, k=k, i=i: e.transpose(psb[:, k * 128:(k + 1) * 128],
                                                                       ab[i][:, k * 128:(k + 1) * 128], self.ident[:]),
                         [f"ab_{i}", "ident"], [f"ps{4 + i}"])
                CPY(p, "act", attnT[:, :, t0:t0 + 128], psb[:, 0:1024].rearrange("p (k t) -> p k t", k=8),
                    [f"ps{4 + i}"], ["attnT"])
                p.flush()
        if self.dbg == f"{l}D":
            p.barrier()
            for c in range(8):
                p.dma("sp", self.dbgA[c], attnT[:, c, :], ["attnT"], ["dbgA"])
            p.flush()

    def stage_E(self, l):
        p = self.p
        p.barrier()
        uactT, attnT = self.uactT, self.attnT
        with ExitStack() as st:
            wc = [self.sb(st, f"wc{i}", [128, 1024], BF16) for i in range(2)]
            wa = [self.sb(st, f"wa{i}", [128, 1024], BF16) for i in range(2)]
            gc = [self.sb(st, f"gc{i}", [128, T], BF16) for i in range(2)]
            ga = [self.sb(st, f"ga{i}", [128, T], BF16) for i in range(2)]
            mx = [self.sb(st, f"mx{i}", [128, T], BF16) for i in range(2)]
            t1 = [self.sb(st, f"e1{i}", [128, GS], F32) for i in range(2)]
            t2 = [self.sb(st, f"e2{i}", [128, GS], F32) for i in range(2)]
            for c in range(16):
                i = c % 2
                p.dma("pool", wc[i][:], self.w_co[l * 16 + c], [], [f"wc{i}"])
                p.dma("pool", wa[i][:], self.w_ao[l * 16 + c], [], [f"wa{i}"])
                p.dma("sp", gc[i][:], self.gT_s[c], ["projout"], [f"gc{i}"])
                p.dma("sp", ga[i][:], self.gT_s[16 + c], ["projout"], [f"ga{i}"])
                for g in range(NG):
                    sl = slice(g * GS, (g + 1) * GS)
                    b1, b2 = 2 * (g % 2), 2 * (g % 2) + 1
                    j = g % 2
                    for k in range(8):
                        MM(p, self.PS[b1][:, 0:GS], wc[i][:, k * 128:(k + 1) * 128], uactT[:, k, sl], k == 0, k == 7,
                           [f"wc{i}", "uactT"], [f"ps{b1}"])
                    for k in range(8):
                        MM(p, self.PS[b2][:, 0:GS], wa[i][:, k * 128:(k + 1) * 128], attnT[:, k, sl], k == 0, k == 7,
                           [f"wa{i}", "attnT"], [f"ps{b2}"])
                    TT(p, "dve", t1[j][:], self.PS[b1][:, 0:GS], gc[i][:, sl], ALU.mult, [f"ps{b1}", f"gc{i}"], [f"e1{j}"])
                    TT(p, "dve", t2[j][:], self.PS[b2][:, 0:GS], ga[i][:, sl], ALU.mult, [f"ps{b2}", f"ga{i}"], [f"e2{j}"])
                    TT(p, "pool", mx[i][:, sl], t1[j][:], t2[j][:], ALU.add, [f"e1{j}", f"e2{j}"], [f"mx{i}"])
                p.dma("sp", self.mixT_s[c], mx[i][:], [f"mx{i}"], ["mixout"])
                p.flush()
        self.lst.close()

    def stage_F(self, l):
        p = self.p
        p.barrier()
        with ExitStack() as st:
            mixT = self.sb(st, "mixT", [128, KD, T], BF16)
            wo = [self.sb(st, f"wo{i}", [128, KD * 512], BF16) for i in range(2)]
            g1 = [self.sb(st, f"g1{i}", [128, 512], F32) for i in range(2)]
            xt = [self.sb(st, f"fx{i}", [128, 512], F32) for i in range(3)]
            tm = [self.sb(st, f"ft{i}", [128, 512], F32) for i in range(2)]
            for k in range(KD):
                p.dma("sp" if k % 2 == 0 else "act", mixT[:, k, :], self.mixT_s[k], ["mixout"], ["mixT"])
            n = 0
            for nq in range(4):
                i = nq % 2
                p.dma("pool", wo[i][:], self.w_o[l * 4 + nq], [], [f"wo{i}"])
                self.load_bcast("sp", g1[i][:], self.modscr[l, 2 * D + nq * 512:2 * D + (nq + 1) * 512], f"g1{i}", ["modscr"])
                for t in range(NB):
                    bi = t % 4
                    xi = n % 3
                    n += 1
                    rows = slice(t * 128, (t + 1) * 128)
                    cols = slice(nq * 512, (nq + 1) * 512)
                    p.dma("sp", xt[xi][:], self.xsrc(l)[rows, cols], self.xkeys(t, nq), [f"fx{xi}"])
                    for k in range(KD):
                        MM(p, self.PS[bi][:], mixT[:, k, rows], wo[i][:, k * 512:(k + 1) * 512], k == 0, k == KD - 1,
                           ["mixT", f"wo{i}"], [f"ps{bi}"])
                    TT(p, "dve", tm[t % 2][:], self.PS[bi][:], g1[i][:], ALU.mult, [f"ps{bi}", f"g1{i}"], [f"ft{t % 2}"])
                    TT(p, "pool", xt[xi][:], tm[t % 2][:], xt[xi][:], ALU.add, [f"ft{t % 2}", f"fx{xi}"], [f"fx{xi}"])
                    p.dma("act", self.xres[rows, cols], xt[xi][:], [f"fx{xi}"], self.xkeys(t, nq))
                p.flush()

    def stage_G(self, l):
        p = self.p
        p.barrier()
        last = (l == L - 1)
        with ExitStack() as st:
            bcA = self.sb(st, "bcA", [128, D], F32)
            bcB = self.sb(st, "bcB", [128, D], F32)
            skb = self.sb(st, "skb", [128, 16, 128], BF16)
            p.dma("pool", skb[:], self.skT[l].rearrange("p (h n) -> p h n", h=16), [], ["skb"])
            h2T = self.sb(st, "h2T", [128, KD, TB * 128], BF16)
            E = self.sb(st, "E", [128, TB, 8, 2, 128], F32)
            pthr = self.sb(st, "pthr", [128, TB, 8], F32)
            acc = self.sb(st, "acc", [128, TB, D], F32)
            xb1 = self.sb(st, "gx0", [128, D], F32)
            xb = [xb1, xb1]
            Dh = self.sb(st, "Dh", [128, TB, 8, 128], BF16)
            hb = [self.sb(st, f"gh{i}", [128, D], BF16) for i in range(2)]
            sm = [self.sb(st, f"gsm{i}", [128, 4], F32) for i in range(2)]
            qp = [self.sb(st, f"qp{i}", [128, TB * 128], BF16) for i in range(2)]
            nm = [self.sb(st, f"nm{i}", [128, 4], F32) for i in range(2)]
            top = self.sb(st, "top", [128, 8, 2, 16], F32)
            tops = self.sb(st, "tops", [128, 8, 16], F32)
            etmp = self.sb(st, "etmp", [128, 128], F32)
            Pc = self.sb(st, "Pc", [128, 8, 16, 16], F32)
            pct = self.sb(st, "pct", [128, 16, 16], F32)
            best = self.sb(st, "best", [128, 8, 16], F32)
            Z = self.sb(st, "Z", [128, 16], F32)
            ub = [self.sb(st, f"ub{i}", [128, D], BF16) for i in range(2)]
            vb = [self.sb(st, f"vb_{c}", [128, D], BF16) for c in range(EG)]
            gact = [self.sb(st, f"gact{i}", [128, TB * 128], F32) for i in range(3)]
            Pf = [self.sb(st, f"Pf{i}", [128, 8, 128], F32) for i in range(2)]
            Mh = [self.sb(st, f"Mh{i}", [128, 8, 128], BF16) for i in range(2)]
            wT = [[self.sb(st, f"wT{i}_{c}", [128, TB * 128], BF16) for c in range(EG)] for i in range(2)]
            t_first = 2 if last else 1
            n_t = NB - t_first
            n_b = -(-n_t // TB)
            sizes = [n_t // n_b + (1 if i < n_t % n_b else 0) for i in range(n_b)]
            starts = [t_first + sum(sizes[:i]) for i in range(n_b)]
            for b0, ntl in zip(starts, sizes):
                nt = ntl * 128
                self.make_gm(bcA, bcB, acc[:, 0, :], "acc", l, self.n2g[l], 4, 3, "bc")
                for tl in range(ntl):
                    i = tl % 2
                    t = b0 + tl
                    p.dma("sp", xb[i][:], self.xres[t * 128:(t + 1) * 128, :], self.xkeys(t), ["gx0"])
                    self.norm_tile(xb[i][:], "gx0", bcA, bcB, ["bcgm", "bcsh"], hb[i][:], f"gh{i}", sm[i], f"gsm{i}",
                                   hb[i][:], f"gh{i}", acc[:, 0, :])
                    self.transpose_tile(hb[i], f"gh{i}", lambda k0, tl=tl: h2T[:, k0:k0 + 8, tl * 128:(tl + 1) * 128],
                                        "h2T", 2 * i)
                if self.gstop == 5:
                    p.flush()
                    return
                for hp in range(self.g1hp):
                    i = hp % 2
                    p.dma("pool", ub[i][:], self.w_pq[l * 16 + hp], [], [f"ub{i}"])
                    bi = 4 + i
                    for k in range(KD):
                        MM(p, self.PS[bi][:, 0:nt], ub[i][:, k * 128:(k + 1) * 128], h2T[:, k, 0:nt], k == 0, k == KD - 1,
                           [f"ub{i}", "h2T"], [f"ps{bi}"])
                    CPY(p, "act", qp[i][:, 0:nt], self.PS[bi][:, 0:nt], [f"ps{bi}"], [f"qp{i}"])
                    bs = 6 + i
                    for tl in range(ntl if self.g1mode != 2 else 0):
                        if self.g1var == 1:
                            MM(p, self.PS[bs][:, tl * 128:(tl + 1) * 128], qp[i][:, tl * 128:(tl + 1) * 128], self.ident[:],
                               True, True, [f"qp{i}", "ident"], [f"ps{bs}"])
                        elif self.g1var == 2:
                            MM(p, self.PS[bs][:, tl * 128:(tl + 1) * 128], skb[:, hp, :], qp[i][:, tl * 128:(tl + 1) * 128],
                               True, True, [f"qp{i}", "skb"], [f"ps{bs}"])
                        elif self.g1var == 3:
                            MM(p, self.PS[2][:, tl * 128:(tl + 1) * 128], qp[i][:, tl * 128:(tl + 1) * 128], skb[:, hp, :],
                               True, True, [f"qp{i}", "skb"], [f"ps2"])
                        else:
                            MM(p, self.PS[bs][:, tl * 128:(tl + 1) * 128], qp[i][:, tl * 128:(tl + 1) * 128], skb[:, hp, :],
                               True, True, [f"qp{i}", "skb"], [f"ps{bs}"])
                    for tl in range(ntl if self.g1mode == 1 else 0):
                        pss = self.PS[bs][:, tl * 128:(tl + 1) * 128]
                        nmc = nm[i][:, tl:tl + 1]
                        p.op("dve", lambda e, pss=pss, nmc=nmc: e.tensor_reduce(out=nmc, in_=pss, axis=AX.X,
                                                                                op=ALU.max, negate=True),
                             [f"ps{bs}"], [f"nm{i}_{tl}"])
                        ACTV(p, E[:, tl, hp // 2, hp % 2, :], pss, AF.Exp, [f"ps{bs}", f"nm{i}_{tl}"], ["E"], bias=nmc)
                p.flush()
                if self.gstop == 1:
                    return
                for tl in range(ntl):
                    for hp in range(16):
                        h, pp = hp // 2, hp % 2
                        src = E[:, tl, h, pp, :]
                        p.op("dve", lambda e, src=src, h=h, pp=pp: e.max(out=top[:, h, pp, 0:8], in_=src), ["E"], ["top"])
                        p.op("dve", lambda e, src=src, h=h, pp=pp: e.match_replace(out=etmp[:], in_to_replace=top[:, h, pp, 0:8],
                                                                                   in_values=src, imm_value=-1.0),
                             ["E", "top"], ["etmp"])
                        p.op("dve", lambda e, h=h, pp=pp: e.max(out=top[:, h, pp, 8:16], in_=etmp[:]), ["etmp"], ["top"])

                    def top16_of_products(a_ap):
                        TT(p, "dve", Pc[:], a_ap.unsqueeze(3).to_broadcast([128, 8, 16, 16]),
                           top[:, :, 1, :].unsqueeze(2).to_broadcast([128, 8, 16, 16]), ALU.mult, ["top", "tops"], ["Pc"])
                        for h in range(8):
                            p.op("dve", lambda e, h=h: e.max(out=best[:, h, 0:8], in_=Pc[:, h]), ["Pc"], ["best"])
                            p.op("dve", lambda e, h=h: e.match_replace(out=pct[:], in_to_replace=best[:, h, 0:8],
                                                                       in_values=Pc[:, h], imm_value=-1.0),
                                 ["Pc", "best"], ["pct"])
                            p.op("dve", lambda e, h=h: e.max(out=best[:, h, 8:16], in_=pct[:]), ["pct"], ["best"])

                    top16_of_products(top[:, :, 0, :])
                    p.op("dve", lambda e: e.tensor_reduce(out=Z[:, 0:8], in_=best[:], axis=AX.X, op=ALU.add), ["best"], ["Z"])
                    p.op("dve", lambda e: e.reciprocal(out=Z[:, 8:16], in_=Z[:, 0:8]), ["Z"], ["Z"])
                    rz = Z[:, 8:16]
                    TT(p, "dve", E[:, tl, :, 0, :], E[:, tl, :, 0, :], rz.unsqueeze(2).to_broadcast([128, 8, 128]), ALU.mult,
                       ["E", "Z"], ["E"])
                    TT(p, "dve", tops[:], top[:, :, 0, :], rz.unsqueeze(2).to_broadcast([128, 8, 16]), ALU.mult,
                       ["top", "Z"], ["tops"])
                    top16_of_products(tops[:])
                    CPY(p, "dve", pthr[:, tl, :], best[:, :, 15], ["best"], ["pthr"])
                    p.op("dve", lambda e, tl=tl: e.reciprocal(out=Z[:, 0:8], in_=pthr[:, tl, :]), ["pthr"], ["Z"])
                    TT(p, "dve", E[:, tl, :, 0, :], E[:, tl, :, 0, :], Z[:, 0:8].unsqueeze(2).to_broadcast([128, 8, 128]),
                       ALU.mult, ["E", "Z"], ["E"])
                    TT(p, "dve", Dh[:, tl], self.ident[:].unsqueeze(1).to_broadcast([128, 8, 128]),
                       pthr[:, tl, :].unsqueeze(2).to_broadcast([128, 8, 128]), ALU.mult, ["ident", "pthr"], ["Dh"])
                p.flush()
                if self.gstop == 2:
                    return
                n_ch = (self.geg * EG) if self.gstop else NCH
                THR = 1.0 - 1e-6
                cnt = {"f": 0}

                def emit_act(c):
                    ui, gu = c % 2, c % 3
                    p.dma("pool", ub[ui][:], self.uT[l * NCH + c], [], [f"ub{ui}"])
                    for k in range(KD):
                        MM(p, self.PS[ui][:, 0:nt], ub[ui][:, k * 128:(k + 1) * 128], h2T[:, k, 0:nt], k == 0, k == KD - 1,
                           [f"ub{ui}", "h2T"], [f"ps{ui}"])
                    ACTV(p, gact[gu][:, 0:nt], self.PS[ui][:, 0:nt], AF.Gelu, [f"ps{ui}"], [f"gact{gu}"])

                def emit_masks(c):
                    bc = 2 + c % 2
                    for tl in range(ntl):
                        fi = cnt["f"] % 2
                        cnt["f"] += 1
                        TT(p, "pool", Pf[fi][:], E[:, tl, :, 1, :], E[:, tl, :, 0, c:c + 1].to_broadcast([128, 8, 128]),
                           ALU.mult, ["E"], [f"Pf{fi}"])
                        STT(p, Mh[fi][:], Pf[fi][:], THR, Pf[fi][:], ALU.is_ge, ALU.mult, [f"Pf{fi}"], [f"Mh{fi}"])
                        for h in range(8):
                            MM(p, self.PS[bc][:, tl * 128:(tl + 1) * 128], Mh[fi][:, h, :], Dh[:, tl, h, :], h == 0, h == 7,
                               [f"Mh{fi}", "Dh"], [f"ps{bc}"])

                def emit_wT(c):
                    eg, cc = divmod(c, EG)
                    gi, gu, bc = eg % 2, c % 3, 2 + c % 2
                    TT(p, "dve", wT[gi][cc][:, 0:nt], self.PS[bc][:, 0:nt], gact[gu][:, 0:nt], ALU.mult,
                       [f"ps{bc}", f"gact{gu}"], [f"wT{gi}_{cc}"])

                def emit_vload(eg):
                    for cc in range(EG):
                        p.dma("pool", vb[cc][:], self.vv[l * NCH + eg * EG + cc], [], [f"vb_{cc}"])

                def emit_out(eg):
                    gi = eg % 2
                    if self.pipe_out:
                        n = 0
                        for half in range(2):
                            for tl in range(ntl):
                                pair = n % 2
                                n += 1
                                for cc in range(EG):
                                    for q in range(2):
                                        bo, dq = 4 + 2 * pair + q, half * 2 + q
                                        MM(p, self.PS[bo][:], wT[gi][cc][:, tl * 128:(tl + 1) * 128],
                                           vb[cc][:, dq * 512:(dq + 1) * 512], cc == 0, cc == EG - 1,
                                           [f"wT{gi}_{cc}", f"vb_{cc}"], [f"ps{bo}"])
                                for q in range(2):
                                    bo, dq = 4 + 2 * pair + q, half * 2 + q
                                    dst = acc[:, tl, dq * 512:(dq + 1) * 512]
                                    if eg == 0:
                                        CPY(p, "act", dst, self.PS[bo][:], [f"ps{bo}"], ["acc"])
                                    else:
                                        TT(p, "dve", dst, self.PS[bo][:], dst, ALU.add, [f"ps{bo}", "acc"], ["acc"])
                        return
                    for tl in range(ntl):
                        for dq in range(4):
                            bo = 4 + dq
                            for cc in range(EG):
                                MM(p, self.PS[bo][:], wT[gi][cc][:, tl * 128:(tl + 1) * 128], vb[cc][:, dq * 512:(dq + 1) * 512],
                                   cc == 0, cc == EG - 1, [f"wT{gi}_{cc}", f"vb_{cc}"], [f"ps{bo}"])
                            dst = acc[:, tl, dq * 512:(dq + 1) * 512]
                            if eg == 0:
                                CPY(p, "act", dst, self.PS[bo][:], [f"ps{bo}"], ["acc"])
                            else:
                                TT(p, "dve", dst, self.PS[bo][:], dst, ALU.add, [f"ps{bo}", "acc"], ["acc"])

                emit_vload(0)
                emit_act(0)
                for c in range(n_ch):
                    eg, cc = divmod(c, EG)
                    if c + 1 < n_ch:
                        emit_act(c + 1)
                    emit_masks(c)
                    if c >= 1:
                        emit_wT(c - 1)
                        if (c - 1) % EG == EG - 1:
                            emit_out((c - 1) // EG)
                    if cc == 1 and eg >= 1:
                        emit_vload(eg)
                    if cc == EG - 1:
                        p.flush()
                emit_wT(n_ch - 1)
                emit_out(n_ch // EG - 1)
                p.flush()
                if self.gstop == 3:
                    return
                self.load_bcast("sp", bcA[:], self.mod_row(l, 5), "bcgm", ["modscr"])
                if last:
                    self.load_bcast("sp", bcB[:], self.fing[0], "bcsh")
                for tl in range(ntl):
                    i = tl % 2
                    t = b0 + tl
                    rows = slice(t * 128, (t + 1) * 128)
                    p.dma("sp", xb[i][:], self.xres[rows, :], self.xkeys(t), ["gx0"])
                    TT(p, "dve", acc[:, tl, :], acc[:, tl, :], bcA[:], ALU.mult, ["acc", "bcgm"], ["acc"])
                    TT(p, "pool", xb[i][:], xb[i][:], acc[:, tl, :], ALU.add, ["gx0", "acc"], ["gx0"])
                    if not last or self.dbg:
                        p.dma("act", self.xres[rows, :], xb[i][:], ["gx0"], self.xkeys(t))
                    if last and t >= 2:
                        self.norm_tile(xb[i][:], "gx0", bcB, None, ["bcsh"], acc[:, tl, :], "acc", sm[i], f"gsm{i}",
                                       hb[i][:], f"gh{i}", None)
                        p.dma("act", self.y[(t - 2) * 128:(t - 1) * 128, :], acc[:, tl, :], ["acc"], ["y"])
                p.flush()

    def stage_A(self, l):
        self.p.barrier()
        self.lst = ExitStack()


def _t5_bucket(dist):
    max_exact = 16
    d = np.maximum(dist, 0)
    large = max_exact + (np.log(np.maximum(d, 1).astype(np.float32) / max_exact)
                         / np.float32(np.log(128 / max_exact)) * (32 - max_exact)).astype(np.int32)
    large = np.minimum(large, 31)
    return np.where(d < max_exact, d, large)


def _chunk_cols(w, ncols):
    Lx, Kd, N = w.shape
    a = w.reshape(Lx, Kd // 128, 128, N // ncols, ncols).transpose(0, 3, 2, 1, 4)
    return np.ascontiguousarray(a).reshape(Lx * (N // ncols), 128, (Kd // 128) * ncols)


_CACHE = {}


def prepare_shared(inp):
    f = lambda a: np.asarray(a, dtype=np.float32)
    w_in = f(inp["w_in"])
    sh = {}
    sh["ada_w"] = _chunk_cols(f(inp["ada_w"]), 512)
    sh["ada_b"] = f(inp["ada_b"])
    sh["n1g"] = f(inp["norm1_g"])
    sh["n2g"] = f(inp["norm2_g"])
    sh["fing"] = f(inp["final_g"]).reshape(1, D)
    sh["w_in_c"] = _chunk_cols(w_in, 128)
    kcols = w_in[:, :, 3072:3328].reshape(L, D, 4, 64)
    kd = np.concatenate([kcols, kcols], axis=3).reshape(L, D, 512)
    sh["w_kd"] = _chunk_cols(kd, 128)
    sh["w_v"] = _chunk_cols(np.ascontiguousarray(w_in[:, :, 3328:3584]), 256)
    sh["w_co"] = _chunk_cols(f(inp["w_conv_out"]), 128)
    sh["w_ao"] = _chunk_cols(f(inp["w_attn_out"]), 128)
    sh["w_o"] = _chunk_cols(f(inp["w_out"]), 512)
    sh["w_pq"] = _chunk_cols(f(inp["w_pq"]), 128)
    sk = f(inp["sub_keys"])
    sh["skT"] = np.ascontiguousarray(sk.transpose(0, 4, 1, 2, 3)).reshape(L, 128, D)
    pu = f(inp["peer_u"])
    sh["uT"] = np.ascontiguousarray(pu.reshape(L, NCH, 128, KD, 128).transpose(0, 1, 4, 3, 2)).reshape(L * NCH, 128, D)
    sh["vv"] = f(inp["peer_v"]).reshape(L * NCH, 128, D)
    cp = np.concatenate([f(inp["dw_w"]).transpose(0, 2, 1), f(inp["dw_b"])[:, :, None],
                         f(inp["conv_ln_g"])[:, :, None], f(inp["conv_ln_b"])[:, :, None]], axis=2)
    sh["chanpar"] = np.ascontiguousarray(cp.reshape(L, 8, 128, 34).transpose(0, 2, 1, 3)).reshape(L, 128, 8 * 34)
    sh["sinks"] = f(inp["attn_sinks"])
    qi = np.arange(128)[:, None] + 128
    kj = np.arange(256)[None, :]
    dist = qi - kj
    bucket = _t5_bucket(dist)
    rb = f(inp["rel_bias"])
    tab = rb[bucket]
    band = (dist >= 0) & (dist < 128)
    tab = np.where(band[:, :, None], tab, np.float32(NEG))
    sh["biasT"] = np.ascontiguousarray(tab.transpose(0, 2, 1)).reshape(128, 16 * 256)
    return sh


def per_core(inp, c):
    b, qtr = c // 4, c % 4
    s0 = qtr * 2048
    x = np.asarray(inp["x"], dtype=np.float32)
    xw = np.zeros((T, D), np.float32)
    if qtr == 0:
        xw[256:] = x[b, 0:2048]
    else:
        xw[:] = x[b, s0 - 256:s0 + 2048]
    cc = np.asarray(inp["c"], dtype=np.float32)[b]
    flags = np.zeros((128, 2), np.float32)
    flags[:, 0] = 0.0 if qtr == 0 else 1.0
    flags[:, 1] = NEG if qtr == 0 else 0.0
    return {"x_in": xw, "c_in": np.ascontiguousarray(cc.reshape(KD, 128).T), "flags": flags}


def kernel(**inputs):
    if "nc" not in _CACHE:
        _CACHE["nc"] = K().build()
    nc = _CACHE["nc"]
    sh = prepare_shared(inputs)
    in_maps = []
    for c in range(8):
        m = dict(sh)
        m.update(per_core(inputs, c))
        in_maps.append(m)
    res = run_bass_kernel_spmd(nc, in_maps, core_ids=list(range(8)))
    out = np.zeros((2, SEQ, D), np.float32)
    for c in range(8):
        b, qtr = c // 4, c % 4
        out[b, qtr * 2048:(qtr + 1) * 2048] = res.results[c]["y"]
    return out
```

```python
import numpy as np
from contextlib import ExitStack
import concourse.bass as bass
import concourse.mybir as mybir
from concourse.bass_utils import run_bass_kernel_spmd

F32 = mybir.dt.float32
BF16 = mybir.dt.bfloat16
AF = mybir.ActivationFunctionType
ALU = mybir.AluOpType
AX = mybir.AxisListType

L = 2
D = 2048
KD = 16
NB = 18
T = NB * 128
GS = 384
NG = T // GS
SEQ = 8192
NEG = -30000.0
EPS = 1e-6
NCH = 128
TB = 3
EG = 4

ENGS = ["pe", "act", "dve", "pool", "sp"]
ENGMAP = {"pe": "tensor", "act": "scalar", "dve": "vector", "pool": "gpsimd", "sp": "sync"}
EPOCH = 1 << 30
NDMASEM = 6


class Prog:
    def __init__(self, nc, stack, block):
        self.nc, self.stack, self.block = nc, stack, block
        self.q = {e: [] for e in ENGS}
        self.cnt = {e: 0 for e in ENGS}
        self.known = {e: {} for e in ENGS}
        self.last_w, self.readers = {}, {}
        self.nsem = 0
        self.sems = {e: self._newsem(e) for e in ENGS}
        self.dsem = {qn: [[self._newsem("d" + qn), 0] for _ in range(NDMASEM)] for qn in ["sp", "act", "pool"]}
        self.dsem_i = {qn: 0 for qn in self.dsem}
        self.n_instr = 0

    def _newsem(self, name):
        self.nsem += 1
        return self.stack.enter_context(self.nc.semaphore(f"s{self.nsem}_{name}"))

    def _deps(self, eng, reads, writes):
        deps = []
        for k in reads:
            deps.extend(self.last_w.get(k, {}).values())
        for k in writes:
            for ev in self.last_w.get(k, {}).values():
                if ev[2] != eng:
                    deps.append(ev)
            for ev in self.readers.get(k, {}).values():
                if ev[2] != eng:
                    deps.append(ev)
        waits = {}
        kn = self.known[eng]
        for (sem, val, src) in deps:
            sid = id(sem)
            if kn.get(sid, 0) >= val:
                continue
            if sid not in waits or waits[sid][1] < val:
                waits[sid] = (sem, val)
        for sid, (sem, val) in waits.items():
            kn[sid] = val
        return list(waits.values())

    def _commit(self, ev, reads, writes):
        sid = id(ev[0])
        for k in writes:
            self.last_w.setdefault(k, {})[sid] = ev
        for k in reads:
            self.readers.setdefault(k, {})[sid] = ev

    def op(self, eng, fn, reads=(), writes=()):
        psr = [k for k in reads if k.startswith("ps") and k[2:].isdigit()]
        if psr:
            writes = list(writes) + psr
        waits = self._deps(eng, reads, writes)
        if self.cnt[eng] >= EPOCH:
            self.sems[eng] = self._newsem(eng)
            self.cnt[eng] = 0
        self.cnt[eng] += 1
        sem = self.sems[eng]
        ev = (sem, self.cnt[eng], eng)
        self.q[eng].append((waits, fn, sem, 1))
        self._commit(ev, reads, writes)
        self.n_instr += 1

    def dma(self, qn, out, in_, reads=(), writes=(), **kw):
        slot = self.dsem[qn][self.dsem_i[qn] % NDMASEM]
        self.dsem_i[qn] += 1
        sem, cnt = slot
        waits = self._deps(qn, reads, writes)
        if cnt > 0 and self.known[qn].get(id(sem), 0) < cnt:
            waits.append((sem, cnt))
            self.known[qn][id(sem)] = cnt
        slot[1] = cnt + 16
        ev = (sem, cnt + 16, "dma_" + qn)
        self.q[qn].append((waits, lambda e: e.dma_start(out=out, in_=in_, **kw), sem, 16))
        self._commit(ev, reads, writes)
        self.n_instr += 1

    def barrier(self):
        evs = [(self.sems[e], self.cnt[e]) for e in ENGS if self.cnt[e] > 0]
        for qn in self.dsem:
            evs += [(s, c) for (s, c) in self.dsem[qn] if c > 0]
        for e in ENGS:
            waits = []
            for (s, v) in evs:
                if s is self.sems[e]:
                    continue
                if self.known[e].get(id(s), 0) < v:
                    waits.append((s, v))
                    self.known[e][id(s)] = v
            if waits:
                self.q[e].append((waits, None, None, 0))

    def flush(self):
        for e in ENGS:
            items, self.q[e] = self.q[e], []
            if not items:
                continue

            def body(engobj, items=items):
                for (waits, fn, sem, inc) in items:
                    for (s, v) in waits:
                        engobj.wait_ge(s, v)
                    if fn is not None:
                        fn(engobj).then_inc(sem, inc)

            getattr(self.block, ENGMAP[e])(body)


def MM(p, out, lhsT, rhs, st, sp, r, w):
    p.op("pe", lambda e: e.matmul(out, lhsT=lhsT, rhs=rhs, start=st, stop=sp), r, w)


def ACTV(p, out, in_, func, r, w, bias=None, scale=None, accum=None):
    kw = {}
    if bias is not None:
        kw["bias"] = bias
    if scale is not None:
        kw["scale"] = scale
    if accum is not None:
        kw["accum_out"] = accum
    p.op("act", lambda e: e.activation(out=out, in_=in_, func=func, **kw), r, w)


def TT(p, eng, out, a, b, op, r, w):
    p.op(eng, lambda e: e.tensor_tensor(out=out, in0=a, in1=b, op=op), r, w)


def TS(p, eng, out, a, s1, op0, r, w, s2=None, op1=None):
    if op1 is None:
        p.op(eng, lambda e: e.tensor_scalar(out=out, in0=a, scalar1=s1, scalar2=None, op0=op0), r, w)
    else:
        p.op(eng, lambda e: e.tensor_scalar(out=out, in0=a, scalar1=s1, scalar2=s2, op0=op0, op1=op1), r, w)


def STT(p, out, a, s, b, op0, op1, r, w):
    p.op("dve", lambda e: e.scalar_tensor_tensor(out=out, in0=a, scalar=s, in1=b, op0=op0, op1=op1), r, w)


def CPY(p, eng, out, in_, r, w):
    if eng == "act":
        p.op("act", lambda e: e.activation(out=out, in_=in_, func=AF.Copy), r, w)
    else:
        p.op(eng, lambda e: e.tensor_copy(out=out, in_=in_), r, w)


class K:
    def __init__(self, dbg=None, nlayers=L):
        self.dbg = dbg
        self.nlayers = nlayers
        self.gstop = 0
        self.geg = 1
        self.in_shapes = {}
        self.only_g = False
        self.g1hp = 16
        self.g1mode = 1
        self.g1var = 0
        self.pipe_out = False
        nc = self.nc = bass.Bass("TRN2", target_bir_lowering=False)

        def din(name, shape, dt=F32):
            self.in_shapes[name] = list(shape)
            return nc.dram_tensor(name, list(shape), dt, kind="ExternalInput").ap()

        def dscr(name, shape, dt):
            kind = "ExternalOutput" if dbg else "Internal"
            return nc.dram_tensor(name, list(shape), dt, kind=kind).ap()

        self.x_in = din("x_in", [T, D])
        self.c_in = din("c_in", [128, KD])
        self.flags = din("flags", [128, 2])
        self.biasT = din("biasT", [128, 16 * 256])
        self.ada_w = din("ada_w", [L * 24, 128, KD * 512])
        self.ada_b = din("ada_b", [L, 6 * D])
        self.n1g = din("n1g", [L, D])
        self.n2g = din("n2g", [L, D])
        self.fing = din("fing", [1, D])
        self.w_in_c = din("w_in_c", [L * 60, 128, D])
        self.w_kd = din("w_kd", [L * 4, 128, D])
        self.w_v = din("w_v", [L, 128, KD * 256])
        self.w_co = din("w_co", [L * 16, 128, 1024])
        self.w_ao = din("w_ao", [L * 16, 128, 1024])
        self.w_o = din("w_o", [L * 4, 128, KD * 512])
        self.w_pq = din("w_pq", [L * 16, 128, D])
        self.skT = din("skT", [L, 128, D])
        self.uT = din("uT", [L * NCH, 128, D])
        self.vv = din("vv", [L * NCH, 128, D])
        self.chanpar = din("chanpar", [L, 128, 8 * 34])
        self.sinks = din("sinks", [L, 16])
        self.y = nc.dram_tensor("y", [16 * 128, D], F32, kind="ExternalOutput").ap()

        self.xres = dscr("xres", [T, D], F32)
        self.modscr = dscr("modscr", [L, 6 * D], F32)
        self.qT_s = dscr("qT_s", [8, 128, T], BF16)
        self.kdT_s = dscr("kdT_s", [4, 128, T], BF16)
        self.v_s = dscr("v_s", [T, 256], BF16)
        self.gT_s = dscr("gT_s", [32, 128, T], BF16)
        self.mixT_s = dscr("mixT_s", [16, 128, T], BF16)
        if dbg:
            self.dbgA = dscr("dbgA", [16, 128, T], BF16)
            self.dbgB = dscr("dbgB", [16, 128, T], BF16)

    def build(self):
        nc = self.nc
        with ExitStack() as st, nc.Block() as block:
            p = self.p = Prog(nc, st, block)
            self.PS = [st.enter_context(nc.psum_tensor(f"ps{i}", [128, 512], F32)) for i in range(8)]
            self.ident = st.enter_context(nc.sbuf_tensor("ident", [128, 128], BF16))
            self.ones = st.enter_context(nc.sbuf_tensor("ones", [128, 128], BF16))
            self.flg = st.enter_context(nc.sbuf_tensor("flg", [128, 2], F32))
            ident, ones = self.ident, self.ones
            p.op("pool", lambda e: e.memset(ident[:], 0.0), [], ["ident"])
            p.op("pool", lambda e: e.affine_select(out=ident[:], in_=ident[:], pattern=[[-1, 128]],
                                                   compare_op=ALU.not_equal, fill=1.0, base=0,
                                                   channel_multiplier=1), ["ident"], ["ident"])
            p.op("pool", lambda e: e.memset(ones[:], 1.0), [], ["ones"])
            p.dma("sp", self.flg[:], self.flags, [], ["flg"])
            if self.only_g:
                self.stage_G(0)
                p.barrier()
                p.flush()
                return nc
            self.stage_mods()
            done = False
            for l in range(self.nlayers):
                for nm in ["A", "B", "C", "D", "E", "F", "G"]:
                    getattr(self, "stage_" + nm)(l)
                    if self.dbg == f"{l}{nm}":
                        done = True
                        break
                if done:
                    break
            p.barrier()
            p.flush()
            if getattr(self, "lst", None) is not None:
                self.lst.close()
        return nc

    def stage_ctx(self):
        self.p.barrier()
        return ExitStack()

    def sb(self, st, name, shape, dt):
        self.uid = getattr(self, "uid", 0) + 1
        return st.enter_context(self.nc.sbuf_tensor(f"{name}_u{self.uid}", list(shape), dt))

    def stage_mods(self):
        p = self.p
        with self.stage_ctx() as st:
            cs = self.sb(st, "cs", [128, KD], F32)
            wb = [self.sb(st, f"adab{i}", [128, KD * 512], F32) for i in range(2)]
            ab = [self.sb(st, f"ab{i}", [1, 512], F32) for i in range(2)]
            mr = [self.sb(st, f"mr{i}", [1, 512], F32) for i in range(2)]
            p.dma("sp", cs[:], self.c_in, [], ["cs"])
            ACTV(p, cs[:], cs[:], AF.Silu, ["cs"], ["cs"])
            for l in range(self.nlayers):
                for n in range(24):
                    i = n % 2
                    p.dma("sp" if i == 0 else "act", wb[i][:], self.ada_w[l * 24 + n], [], [f"adab{i}"])
                    p.dma("sp", ab[i][:], self.ada_b[l:l + 1, n * 512:(n + 1) * 512], [], [f"ab{i}"])
                    ps = self.PS[i]
                    for k in range(KD):
                        MM(p, ps[0:1, :], cs[:, k:k + 1], wb[i][:, k * 512:(k + 1) * 512], k == 0, k == KD - 1,
                           ["cs", f"adab{i}"], [f"ps{i}"])
                    TT(p, "dve", mr[i][:], ps[0:1, :], ab[i][:], ALU.add, [f"ps{i}", f"ab{i}"], [f"mr{i}"])
                    p.dma("sp", self.modscr[l:l + 1, n * 512:(n + 1) * 512], mr[i][:], [f"mr{i}"], ["modscr"])
            p.flush()

    def load_bcast(self, q, dst, src_row, key, reads=()):
        self.p.dma(q, dst, src_row.partition_broadcast(128), list(reads), [key])

    def mod_row(self, l, i):
        return self.modscr[l, i * D:(i + 1) * D]

    def make_gm(self, gm, sh, tmp, tk, l, gain_row, sc_i, sh_i, pfx):
        p = self.p
        self.load_bcast("sp", gm[:], gain_row, pfx + "gm")
        self.load_bcast("sp", tmp[:], self.mod_row(l, sc_i), tk, ["modscr"])
        self.load_bcast("sp", sh[:], self.mod_row(l, sh_i), pfx + "sh", ["modscr"])
        STT(p, gm[:], tmp[:], 1.0, gm[:], ALU.add, ALU.mult, [tk, pfx + "gm"], [pfx + "gm"])

    def norm_tile(self, xt, xk, gm, sh, gk, hb, hk, sm, smk, junk, jk, tmp):
        p = self.p
        ACTV(p, junk, xt, AF.Square, [xk], [jk, smk + "ss"], accum=sm[:, 0:1])
        TS(p, "dve", sm[:, 1:2], sm[:, 0:1], 1.0 / D, ALU.mult, [smk + "ss"], [smk + "a"], s2=EPS, op1=ALU.add)
        ACTV(p, sm[:, 2:3], sm[:, 1:2], AF.Sqrt, [smk + "a"], [smk + "b"])
        p.op("dve", lambda e: e.reciprocal(out=sm[:, 3:4], in_=sm[:, 2:3]), [smk + "b"], [smk + "r"])
        if sh is None:
            STT(p, hb, xt, sm[:, 3:4], gm[:], ALU.mult, ALU.mult, [xk, smk + "r"] + gk, [hk])
        else:
            STT(p, tmp, xt, sm[:, 3:4], gm[:], ALU.mult, ALU.mult, [xk, smk + "r"] + gk, ["ntmp"])
            TT(p, "dve", hb, tmp, sh[:], ALU.add, ["ntmp"] + gk, [hk])

    def transpose_tile(self, hb, hk, dst_fn, dk, bank0):
        p = self.p
        for half in range(2):
            bi = bank0 + half
            psb = self.PS[bi][:].bitcast(BF16)
            for j in range(8):
                k = half * 8 + j
                p.op("pe", lambda e, j=j, k=k, psb=psb: e.transpose(psb[:, j * 128:(j + 1) * 128],
                                                                   hb[:, k * 128:(k + 1) * 128], self.ident[:]),
                     [hk, "ident"], [f"ps{bi}"])
            src = psb[:, 0:1024].rearrange("p (j t) -> p j t", j=8)
            CPY(p, "act" if half == 0 else "dve", dst_fn(half * 8), src, [f"ps{bi}"], [dk])

    def xsrc(self, l):
        return self.x_in if l == 0 else self.xres

    def xkeys(self, t, nq=None):
        return [f"xr{t}_{q}" for q in (range(4) if nq is None else [nq])]

    def stage_B(self, l):
        p = self.p
        self.convT = convT = self.sb(self.lst, "convT", [128, 8, T], BF16)
        with self.stage_ctx() as st:
            hT = self.sb(st, "hT", [128, KD, T], BF16)
            with ExitStack() as st2:
                gm = self.sb(st2, "n1gm", [128, D], F32)
                sh = self.sb(st2, "n1sh", [128, D], F32)
                tmp = self.sb(st2, "ntmp", [128, D], F32)
                self.make_gm(gm, sh, tmp, "ntmp", l, self.n1g[l], 1, 0, "n1")
                xb = [self.sb(st2, f"xb{i}", [128, D], F32) for i in range(2)]
                hb = [self.sb(st2, f"hb{i}", [128, D], BF16) for i in range(2)]
                sm = [self.sb(st2, f"sm{i}", [128, 4], F32) for i in range(2)]
                for t in range(NB):
                    i = t % 2
                    p.dma("sp", xb[i][:], self.xsrc(l)[t * 128:(t + 1) * 128, :], self.xkeys(t), [f"xb{i}"])
                    self.norm_tile(xb[i][:], f"xb{i}", gm, sh, ["n1gm", "n1sh"], hb[i][:], f"hb{i}", sm[i], f"sm{i}",
                                   hb[i][:], f"hb{i}", tmp[:])
                    self.transpose_tile(hb[i], f"hb{i}", lambda k0, t=t: hT[:, k0:k0 + 8, t * 128:(t + 1) * 128],
                                        f"hT{t // 3}", 2 * i)
                p.flush()
            p.barrier()
            if self.dbg == f"{l}A":
                for k in range(KD):
                    p.dma("sp", self.dbgA[k], hT[:, k, :], [f"hT{g}" for g in range(NG)], ["dbgA"])
                p.flush()
                return
            wt = [self.sb(st, f"wt{i}", [128, D], BF16) for i in range(3)]
            stg = [self.sb(st, f"stg{i}", [128, GS], BF16) for i in range(3)]
            sig = [self.sb(st, f"sig{i}", [128, GS], F32) for i in range(2)]
            upad = [self.sb(st, f"upad{i}", [128, 32 + T], F32) for i in range(2)]
            cacc = self.sb(st, "cacc", [128, T], F32)
            cp = self.sb(st, "cp", [128, 8, 34], F32)
            wv = self.sb(st, "wv", [128, KD * 256], BF16)
            vst = [self.sb(st, f"vst{i}", [128, 256], BF16) for i in range(2)]
            p.dma("sp", cp[:], self.chanpar[l].rearrange("p (c t) -> p c t", c=8), [], ["cp"])
            for i in range(2):
                p.op("pool", lambda e, i=i: e.memset(upad[i][:], 0.0), [], [f"upad{i}"])
            state = {"w": 0, "ps": 0, "stg": 0}

            def proj(wsrc, evac):
                wi = state["w"] % 3
                state["w"] += 1
                p.dma("pool", wt[wi][:], wsrc, [], [f"wt{wi}"])
                for g in range(NG):
                    bi = state["ps"] % 4
                    state["ps"] += 1
                    ps = self.PS[bi]
                    for k in range(KD):
                        MM(p, ps[:, 0:GS], wt[wi][:, k * 128:(k + 1) * 128], hT[:, k, g * GS:(g + 1) * GS],
                           k == 0, k == KD - 1, [f"wt{wi}", f"hT{g}"], [f"ps{bi}"])
                    evac(g, ps[:, 0:GS], f"ps{bi}")

            def evac_store(dst, func):
                def f(g, ps, pk):
                    si = state["stg"] % 3
                    state["stg"] += 1
                    ACTV(p, stg[si][:], ps, func, [pk], [f"stg{si}"])
                    p.dma("sp", dst[:, g * GS:(g + 1) * GS], stg[si][:], [f"stg{si}"], ["projout"])
                return f

            for c in range(8):
                proj(self.w_in_c[l * 60 + 16 + c], evac_store(self.qT_s[c], AF.Copy))
            for c in range(4):
                proj(self.w_kd[l * 4 + c], evac_store(self.kdT_s[c], AF.Copy))
            for c in range(32):
                proj(self.w_in_c[l * 60 + 28 + c], evac_store(self.gT_s[c], AF.Sigmoid))
            p.dma("pool", wv[:], self.w_v[l], [], ["wv"])
            for t in range(NB):
                bi = 4 + (t % 2)
                ps = self.PS[bi]
                for k in range(KD):
                    MM(p, ps[:, 0:256], hT[:, k, t * 128:(t + 1) * 128], wv[:, k * 256:(k + 1) * 256],
                       k == 0, k == KD - 1, ["wv", f"hT{t // 3}"], [f"ps{bi}"])
                CPY(p, "act", vst[t % 2][:], ps[:, 0:256], [f"ps{bi}"], [f"vst{t % 2}"])
                p.dma("sp", self.v_s[t * 128:(t + 1) * 128, :], vst[t % 2][:], [f"vst{t % 2}"], ["projout"])
            p.flush()
            for c in range(8):
                ui = c % 2
                up = upad[ui]
                wa = state["w"] % 3
                state["w"] += 1
                wbi = state["w"] % 3
                state["w"] += 1
                p.dma("pool", wt[wa][:], self.w_in_c[l * 60 + c], [], [f"wt{wa}"])
                p.dma("pool", wt[wbi][:], self.w_in_c[l * 60 + 8 + c], [], [f"wt{wbi}"])
                for g in range(NG):
                    ba = (2 * g) % 4
                    bb = (2 * g + 1) % 4
                    for k in range(KD):
                        MM(p, self.PS[ba][:, 0:GS], wt[wa][:, k * 128:(k + 1) * 128], hT[:, k, g * GS:(g + 1) * GS],
                           k == 0, k == KD - 1, [f"wt{wa}", f"hT{g}"], [f"ps{ba}"])
                    for k in range(KD):
                        MM(p, self.PS[bb][:, 0:GS], wt[wbi][:, k * 128:(k + 1) * 128], hT[:, k, g * GS:(g + 1) * GS],
                           k == 0, k == KD - 1, [f"wt{wbi}", f"hT{g}"], [f"ps{bb}"])
                    ACTV(p, sig[g % 2][:], self.PS[bb][:, 0:GS], AF.Sigmoid, [f"ps{bb}"], [f"sig{g % 2}"])
                    TT(p, "dve", up[:, 32 + g * GS:32 + (g + 1) * GS], self.PS[ba][:, 0:GS], sig[g % 2][:], ALU.mult,
                       [f"ps{ba}", f"sig{g % 2}"], [f"upad{ui}"])
                TS(p, "dve", up[:, 32:32 + 256], up[:, 32:32 + 256], self.flg[:, 0:1], ALU.mult,
                   [f"upad{ui}", "flg"], [f"upad{ui}"])
                TS(p, "dve", cacc[:], up[:, 2:2 + T], cp[:, c, 0:1], ALU.mult, [f"upad{ui}", "cp"], ["cacc"],
                   s2=cp[:, c, 31:32], op1=ALU.add)
                for j in range(1, 31):
                    dst = cacc[:] if j < 30 else convT[:, c, :]
                    STT(p, dst, up[:, 2 + j:2 + j + T], cp[:, c, j:j + 1], cacc[:], ALU.mult, ALU.add,
                        [f"upad{ui}", "cp", "cacc"], ["cacc"] if j < 30 else ["convT"])
                p.flush()

    def stage_C(self, l):
        p = self.p
        convT = self.convT
        self.p.barrier()
        self.uactT = uactT = self.sb(self.lst, "uactT", [128, 8, T], BF16)
        with ExitStack() as st2:
            cp = self.sb(st2, "cp2", [128, 8, 34], F32)
            sq = [self.sb(st2, f"sq{i}", [128, GS], BF16) for i in range(2)]
            mean = self.sb(st2, "mean", [128, GS], F32)
            m2 = self.sb(st2, "m2", [128, GS], F32)
            var = self.sb(st2, "var", [128, GS], F32)
            rstd = self.sb(st2, "rstd", [128, GS], F32)
            t1 = [self.sb(st2, f"t1{i}", [128, GS], F32) for i in range(2)]
            t2 = [self.sb(st2, f"t2{i}", [128, GS], F32) for i in range(2)]
            p.dma("sp", cp[:], self.chanpar[l].rearrange("p (c t) -> p c t", c=8), [], ["cp2"])
            for g in range(NG):
                sl = slice(g * GS, (g + 1) * GS)
                b1, b2 = 2 * (g % 2), 2 * (g % 2) + 1
                for c in range(8):
                    MM(p, self.PS[b1][:, 0:GS], self.ones[:], convT[:, c, sl], c == 0, c == 7, ["ones", "convT"],
                       [f"ps{b1}"])
                for c in range(8):
                    ACTV(p, sq[c % 2][:], convT[:, c, sl], AF.Square, ["convT"], [f"sq{c % 2}"])
                    MM(p, self.PS[b2][:, 0:GS], self.ones[:], sq[c % 2][:], c == 0, c == 7, ["ones", f"sq{c % 2}"],
                       [f"ps{b2}"])
                ACTV(p, mean[:], self.PS[b1][:, 0:GS], AF.Copy, [f"ps{b1}"], ["mean"], scale=1.0 / 1024)
                TT(p, "dve", m2[:], mean[:], mean[:], ALU.mult, ["mean"], ["m2"])
                STT(p, var[:], self.PS[b2][:, 0:GS], 1.0 / 1024, m2[:], ALU.mult, ALU.subtract, [f"ps{b2}", "m2"], ["var"])
                TS(p, "dve", var[:], var[:], EPS, ALU.add, ["var"], ["var"])
                ACTV(p, var[:], var[:], AF.Sqrt, ["var"], ["var"])
                p.op("dve", lambda e: e.reciprocal(out=rstd[:], in_=var[:]), ["var"], ["rstd"])
                for c in range(8):
                    i = c % 2
                    TT(p, "dve", t1[i][:], convT[:, c, sl], mean[:], ALU.subtract, ["convT", "mean"], [f"t1{i}"])
                    TT(p, "pool", t2[i][:], t1[i][:], rstd[:], ALU.mult, [f"t1{i}", "rstd"], [f"t2{i}"])
                    ACTV(p, uactT[:, c, sl], t2[i][:], AF.Silu, [f"t2{i}", "cp2"], ["uactT"],
                         bias=cp[:, c, 33:34], scale=cp[:, c, 32:33])
            p.flush()
        if self.dbg == f"{l}C":
            p.barrier()
            for c in range(8):
                p.dma("sp", self.dbgA[c], uactT[:, c, :], ["uactT"], ["dbgA"])
                p.dma("sp", self.dbgB[c], convT[:, c, :], ["convT"], ["dbgB"])
            p.flush()

    def stage_D(self, l):
        p = self.p
        p.barrier()
        self.attnT = attnT = self.sb(self.lst, "attnT", [128, 8, T], BF16)
        p.op("pool", lambda e: e.memset(attnT[:, :, 0:128], 0.0), [], ["attnT"])
        with ExitStack() as st:
            bias = self.sb(st, "bias", [128, 16, 256], F32)
            bias2 = self.sb(st, "bias2", [128, 16, 256], F32)
            snk = self.sb(st, "snk", [128, 16], F32)
            qb = [self.sb(st, f"qb{i}", [128, 8, 128], BF16) for i in range(2)]
            kb = [self.sb(st, f"kb{i}", [128, 4, 256], BF16) for i in range(2)]
            vb = [self.sb(st, f"vb{i}", [128, 2, 256], BF16) for i in range(2)]
            sc = [self.sb(st, f"sc{i}", [128, 2, 256], F32) for i in range(2)]
            pb = [self.sb(st, f"pb{i}", [128, 2, 256], BF16) for i in range(2)]
            pT = [self.sb(st, f"pT{i}", [128, 4, 128], BF16) for i in range(2)]
            sm = [self.sb(st, f"asm{i}", [128, 16], F32) for i in range(2)]
            rs = [self.sb(st, f"rs{i}", [128, 16], F32) for i in range(2)]
            es = [self.sb(st, f"es{i}", [128, 16], F32) for i in range(2)]
            ab = [self.sb(st, f"ab_{i}", [128, 1024], BF16) for i in range(2)]
            p.dma("sp", bias[:], self.biasT.rearrange("p (h s) -> p h s", h=16), [], ["bias"])
            p.dma("sp", bias2[:], self.biasT.rearrange("p (h s) -> p h s", h=16), [], ["bias2"])
            self.load_bcast("sp", snk[:], self.sinks[l], "snk")
            TS(p, "dve", bias2[:, :, 0:128], bias2[:, :, 0:128], self.flg[:, 1:2], ALU.add, ["bias2", "flg"], ["bias2"])
            for b in range(1, NB):
                i = b % 2
                bt, bk = (bias2, "bias2") if b == 2 else (bias, "bias")
                t0 = b * 128
                p.dma("sp", qb[i][:], self.qT_s[:, :, t0:t0 + 128].rearrange("c p t -> p c t"), ["projout"], [f"qb{i}"])
                p.dma("sp", kb[i][:], self.kdT_s[:, :, t0 - 128:t0 + 128].rearrange("c p t -> p c t"), ["projout"],
                      [f"kb{i}"])
                p.dma("sp", vb[i][:], self.v_s[t0 - 128:t0 + 128, :].rearrange("(a p) d -> p a d", p=128), ["projout"],
                      [f"vb{i}"])
                ao_banks = (6, 7)
                for m in range(8):
                    j = m % 2
                    g = m // 2
                    tb_ = 4 + j
                    for hh in range(2):
                        lo = 64 * hh
                        sbk = 2 * j + hh
                        MM(p, self.PS[sbk][:, 0:256], qb[i][lo:lo + 64, m, :], kb[i][lo:lo + 64, g, :],
                           True, True, [f"qb{i}", f"kb{i}"], [f"ps{sbk}"])
                        STT(p, sc[j][:, hh, :], self.PS[sbk][:, 0:256], 0.125, bt[:, 2 * m + hh, :],
                            ALU.mult, ALU.add, [f"ps{sbk}", bk], [f"sc{j}"])
                    p.op("dve", lambda e, j=j, m=m: e.tensor_reduce(out=sm[j][:, 0:2], in_=sc[j][:], axis=AX.X,
                                                                    op=ALU.max), [f"sc{j}"], [f"asm{j}a"])
                    TT(p, "dve", sm[j][:, 2:4], sm[j][:, 0:2], snk[:, 2 * m:2 * m + 2], ALU.max, [f"asm{j}a", "snk"],
                       [f"asm{j}b"])
                    TS(p, "dve", sm[j][:, 4:6], sm[j][:, 2:4], -1.0, ALU.mult, [f"asm{j}b"], [f"asm{j}c"])
                    TT(p, "dve", sm[j][:, 6:8], snk[:, 2 * m:2 * m + 2], sm[j][:, 4:6], ALU.add, ["snk", f"asm{j}c"],
                       [f"asm{j}d"])
                    ACTV(p, es[i][:, 2 * m:2 * m + 2], sm[j][:, 6:8], AF.Exp, [f"asm{j}d"], [f"es{i}"])
                    for hh in range(2):
                        ACTV(p, pb[j][:, hh, :], sc[j][:, hh, :], AF.Exp, [f"sc{j}", f"asm{j}c"], [f"pb{j}", f"rs{i}"],
                             bias=sm[j][:, 4 + hh:5 + hh], accum=rs[i][:, 2 * m + hh:2 * m + hh + 1])
                    psb = self.PS[tb_][:].bitcast(BF16)
                    for hh in range(2):
                        for half in range(2):
                            n = hh * 2 + half
                            p.op("pe", lambda e, psb=psb, n=n, j=j, hh=hh, half=half: e.transpose(
                                psb[:, n * 128:(n + 1) * 128], pb[j][:, hh, half * 128:(half + 1) * 128], self.ident[:]),
                                 [f"pb{j}", "ident"], [f"ps{tb_}"])
                    CPY(p, "act" if j == 0 else "dve", pT[j][:], psb[:, 0:512].rearrange("p (n t) -> p n t", n=4),
                        [f"ps{tb_}"], [f"pT{j}"])
                    for hh in range(2):
                        h = 2 * m + hh
                        bo = ao_banks[h // 8]
                        col = (h % 8) * 64
                        for half in range(2):
                            MM(p, self.PS[bo][:, col:col + 64], pT[j][:, hh * 2 + half, :],
                               vb[i][:, half, g * 64:(g + 1) * 64], half == 0, half == 1,
                               [f"pT{j}", f"vb{i}"], [f"ps{bo}"])
                TT(p, "dve", rs[i][:], rs[i][:], es[i][:], ALU.add, [f"rs{i}", f"es{i}"], [f"rs{i}"])
                p.op("dve", lambda e, i=i: e.reciprocal(out=rs[i][:], in_=rs[i][:]), [f"rs{i}"], [f"rs{i}"])
                for hb_ in range(2):
                    bo = ao_banks[hb_]
                    TT(p, "dve", ab[i][:, hb_ * 512:(hb_ + 1) * 512].rearrange("p (h d) -> p h d", h=8),
                       self.PS[bo][:].rearrange("p (h d) -> p h d", h=8),
                       rs[i][:, hb_ * 8:(hb_ + 1) * 8].unsqueeze(2).to_broadcast([128, 8, 64]), ALU.mult,
                       [f"ps{bo}", f"rs{i}"], [f"ab_{i}"])
                psb = self.PS[4 + i][:].bitcast(BF16)
                for k in range(8):
                    p.op("pe", lambda e, psb=psb, k=k, i=i: e.transpose(psb[:, k * 128:(k + 1) * 128],
                                                                       ab[i][:, k * 128:(k + 1) * 128], self.ident[:]),
                         [f"ab_{i}", "ident"], [f"ps{4 + i}"])
                CPY(p, "act", attnT[:, :, t0:t0 + 128], psb[:, 0:1024].rearrange("p (k t) -> p k t", k=8),
                    [f"ps{4 + i}"], ["attnT"])
                p.flush()
        if self.dbg == f"{l}D":
            p.barrier()
            for c in range(8):
                p.dma("sp", self.dbgA[c], attnT[:, c, :], ["attnT"], ["dbgA"])
            p.flush()

    def stage_E(self, l):
        p = self.p
        p.barrier()
        uactT, attnT = self.uactT, self.attnT
        with ExitStack() as st:
            wc = [self.sb(st, f"wc{i}", [128, 1024], BF16) for i in range(2)]
            wa = [self.sb(st, f"wa{i}", [128, 1024], BF16) for i in range(2)]
            gc = [self.sb(st, f"gc{i}", [128, T], BF16) for i in range(2)]
            ga = [self.sb(st, f"ga{i}", [128, T], BF16) for i in range(2)]
            mx = [self.sb(st, f"mx{i}", [128, T], BF16) for i in range(2)]
            t1 = [self.sb(st, f"e1{i}", [128, GS], F32) for i in range(2)]
            t2 = [self.sb(st, f"e2{i}", [128, GS], F32) for i in range(2)]
            for c in range(16):
                i = c % 2
                p.dma("pool", wc[i][:], self.w_co[l * 16 + c], [], [f"wc{i}"])
                p.dma("pool", wa[i][:], self.w_ao[l * 16 + c], [], [f"wa{i}"])
                p.dma("sp", gc[i][:], self.gT_s[c], ["projout"], [f"gc{i}"])
                p.dma("sp", ga[i][:], self.gT_s[16 + c], ["projout"], [f"ga{i}"])
                for g in range(NG):
                    sl = slice(g * GS, (g + 1) * GS)
                    b1, b2 = 2 * (g % 2), 2 * (g % 2) + 1
                    j = g % 2
                    for k in range(8):
                        MM(p, self.PS[b1][:, 0:GS], wc[i][:, k * 128:(k + 1) * 128], uactT[:, k, sl], k == 0, k == 7,
                           [f"wc{i}", "uactT"], [f"ps{b1}"])
                    for k in range(8):
                        MM(p, self.PS[b2][:, 0:GS], wa[i][:, k * 128:(k + 1) * 128], attnT[:, k, sl], k == 0, k == 7,
                           [f"wa{i}", "attnT"], [f"ps{b2}"])
                    TT(p, "dve", t1[j][:], self.PS[b1][:, 0:GS], gc[i][:, sl], ALU.mult, [f"ps{b1}", f"gc{i}"], [f"e1{j}"])
                    TT(p, "dve", t2[j][:], self.PS[b2][:, 0:GS], ga[i][:, sl], ALU.mult, [f"ps{b2}", f"ga{i}"], [f"e2{j}"])
                    TT(p, "pool", mx[i][:, sl], t1[j][:], t2[j][:], ALU.add, [f"e1{j}", f"e2{j}"], [f"mx{i}"])
                p.dma("sp", self.mixT_s[c], mx[i][:], [f"mx{i}"], ["mixout"])
                p.flush()
        self.lst.close()

    def stage_F(self, l):
        p = self.p
        p.barrier()
        with ExitStack() as st:
            mixT = self.sb(st, "mixT", [128, KD, T], BF16)
            wo = [self.sb(st, f"wo{i}", [128, KD * 512], BF16) for i in range(2)]
            g1 = [self.sb(st, f"g1{i}", [128, 512], F32) for i in range(2)]
            xt = [self.sb(st, f"fx{i}", [128, 512], F32) for i in range(3)]
            tm = [self.sb(st, f"ft{i}", [128, 512], F32) for i in range(2)]
            for k in range(KD):
                p.dma("sp" if k % 2 == 0 else "act", mixT[:, k, :], self.mixT_s[k], ["mixout"], ["mixT"])
            n = 0
            for nq in range(4):
                i = nq % 2
                p.dma("pool", wo[i][:], self.w_o[l * 4 + nq], [], [f"wo{i}"])
                self.load_bcast("sp", g1[i][:], self.modscr[l, 2 * D + nq * 512:2 * D + (nq + 1) * 512], f"g1{i}", ["modscr"])
                for t in range(NB):
                    bi = t % 4
                    xi = n % 3
                    n += 1
                    rows = slice(t * 128, (t + 1) * 128)
                    cols = slice(nq * 512, (nq + 1) * 512)
                    p.dma("sp", xt[xi][:], self.xsrc(l)[rows, cols], self.xkeys(t, nq), [f"fx{xi}"])
                    for k in range(KD):
                        MM(p, self.PS[bi][:], mixT[:, k, rows], wo[i][:, k * 512:(k + 1) * 512], k == 0, k == KD - 1,
                           ["mixT", f"wo{i}"], [f"ps{bi}"])
                    TT(p, "dve", tm[t % 2][:], self.PS[bi][:], g1[i][:], ALU.mult, [f"ps{bi}", f"g1{i}"], [f"ft{t % 2}"])
                    TT(p, "pool", xt[xi][:], tm[t % 2][:], xt[xi][:], ALU.add, [f"ft{t % 2}", f"fx{xi}"], [f"fx{xi}"])
                    p.dma("act", self.xres[rows, cols], xt[xi][:], [f"fx{xi}"], self.xkeys(t, nq))
                p.flush()

    def stage_G(self, l):
        p = self.p
        p.barrier()
        last = (l == L - 1)
        with ExitStack() as st:
            bcA = self.sb(st, "bcA", [128, D], F32)
            bcB = self.sb(st, "bcB", [128, D], F32)
            skb = self.sb(st, "skb", [128, 16, 128], BF16)
            p.dma("pool", skb[:], self.skT[l].rearrange("p (h n) -> p h n", h=16), [], ["skb"])
            h2T = self.sb(st, "h2T", [128, KD, TB * 128], BF16)
            E = self.sb(st, "E", [128, TB, 8, 2, 128], F32)
            pthr = self.sb(st, "pthr", [128, TB, 8], F32)
            acc = self.sb(st, "acc", [128, TB, D], F32)
            xb1 = self.sb(st, "gx0", [128, D], F32)
            xb = [xb1, xb1]
            Dh = self.sb(st, "Dh", [128, TB, 8, 128], BF16)
            hb = [self.sb(st, f"gh{i}", [128, D], BF16) for i in range(2)]
            sm = [self.sb(st, f"gsm{i}", [128, 4], F32) for i in range(2)]
            qp = [self.sb(st, f"qp{i}", [128, TB * 128], BF16) for i in range(2)]
            nm = [self.sb(st, f"nm{i}", [128, 4], F32) for i in range(2)]
            top = self.sb(st, "top", [128, 8, 2, 16], F32)
            tops = self.sb(st, "tops", [128, 8, 16], F32)
            etmp = self.sb(st, "etmp", [128, 128], F32)
            Pc = self.sb(st, "Pc", [128, 8, 16, 16], F32)
            pct = self.sb(st, "pct", [128, 16, 16], F32)
            best = self.sb(st, "best", [128, 8, 16], F32)
            Z = self.sb(st, "Z", [128, 16], F32)
            ub = [self.sb(st, f"ub{i}", [128, D], BF16) for i in range(3)]
            vb = [self.sb(st, f"vb_{c}", [128, D], BF16) for c in range(EG)]
            gact = [self.sb(st, f"gact{i}", [128, TB * 128], F32) for i in range(3)]
            Pf = [self.sb(st, f"Pf{i}", [128, 8, 128], F32) for i in range(2)]
            Mh = [self.sb(st, f"Mh{i}", [128, 8, 128], BF16) for i in range(2)]
            wT = [[self.sb(st, f"wT{i}_{c}", [128, TB * 128], BF16) for c in range(EG)] for i in range(2)]
            t_first = 2 if last else 1
            n_t = NB - t_first
            n_b = -(-n_t // TB)
            sizes = [n_t // n_b + (1 if i < n_t % n_b else 0) for i in range(n_b)]
            starts = [t_first + sum(sizes[:i]) for i in range(n_b)]
            for b0, ntl in zip(starts, sizes):
                nt = ntl * 128
                self.make_gm(bcA, bcB, acc[:, 0, :], "acc", l, self.n2g[l], 4, 3, "bc")
                for tl in range(ntl):
                    i = tl % 2
                    t = b0 + tl
                    p.dma("sp", xb[i][:], self.xres[t * 128:(t + 1) * 128, :], self.xkeys(t), ["gx0"])
                    self.norm_tile(xb[i][:], "gx0", bcA, bcB, ["bcgm", "bcsh"], hb[i][:], f"gh{i}", sm[i], f"gsm{i}",
                                   hb[i][:], f"gh{i}", acc[:, 0, :])
                    self.transpose_tile(hb[i], f"gh{i}", lambda k0, tl=tl: h2T[:, k0:k0 + 8, tl * 128:(tl + 1) * 128],
                                        "h2T", 2 * i)
                if self.gstop == 5:
                    p.flush()
                    return
                for hp in range(self.g1hp):
                    i = hp % 2
                    p.dma("pool", ub[i][:], self.w_pq[l * 16 + hp], [], [f"ub{i}"])
                    bi = 4 + i
                    for k in range(KD):
                        MM(p, self.PS[bi][:, 0:nt], ub[i][:, k * 128:(k + 1) * 128], h2T[:, k, 0:nt], k == 0, k == KD - 1,
                           [f"ub{i}", "h2T"], [f"ps{bi}"])
                    CPY(p, "act", qp[i][:, 0:nt], self.PS[bi][:, 0:nt], [f"ps{bi}"], [f"qp{i}"])
                    bs = 6 + i
                    for tl in range(ntl if self.g1mode != 2 else 0):
                        if self.g1var == 1:
                            MM(p, self.PS[bs][:, tl * 128:(tl + 1) * 128], qp[i][:, tl * 128:(tl + 1) * 128], self.ident[:],
                               True, True, [f"qp{i}", "ident"], [f"ps{bs}"])
                        elif self.g1var == 2:
                            MM(p, self.PS[bs][:, tl * 128:(tl + 1) * 128], skb[:, hp, :], qp[i][:, tl * 128:(tl + 1) * 128],
                               True, True, [f"qp{i}", "skb"], [f"ps{bs}"])
                        elif self.g1var == 3:
                            MM(p, self.PS[2][:, tl * 128:(tl + 1) * 128], qp[i][:, tl * 128:(tl + 1) * 128], skb[:, hp, :],
                               True, True, [f"qp{i}", "skb"], [f"ps2"])
                        else:
                            MM(p, self.PS[bs][:, tl * 128:(tl + 1) * 128], qp[i][:, tl * 128:(tl + 1) * 128], skb[:, hp, :],
                               True, True, [f"qp{i}", "skb"], [f"ps{bs}"])
                    for tl in range(ntl if self.g1mode == 1 else 0):
                        pss = self.PS[bs][:, tl * 128:(tl + 1) * 128]
                        nmc = nm[i][:, tl:tl + 1]
                        p.op("dve", lambda e, pss=pss, nmc=nmc: e.tensor_reduce(out=nmc, in_=pss, axis=AX.X,
                                                                                op=ALU.max, negate=True),
                             [f"ps{bs}"], [f"nm{i}_{tl}"])
                        ACTV(p, E[:, tl, hp // 2, hp % 2, :], pss, AF.Exp, [f"ps{bs}", f"nm{i}_{tl}"], ["E"], bias=nmc)
                p.flush()
                if self.gstop == 1:
                    return
                for tl in range(ntl):
                    for hp in range(16):
                        h, pp = hp // 2, hp % 2
                        src = E[:, tl, h, pp, :]
                        p.op("dve", lambda e, src=src, h=h, pp=pp: e.max(out=top[:, h, pp, 0:8], in_=src), ["E"], ["top"])
                        p.op("dve", lambda e, src=src, h=h, pp=pp: e.match_replace(out=etmp[:], in_to_replace=top[:, h, pp, 0:8],
                                                                                   in_values=src, imm_value=-1.0),
                             ["E", "top"], ["etmp"])
                        p.op("dve", lambda e, h=h, pp=pp: e.max(out=top[:, h, pp, 8:16], in_=etmp[:]), ["etmp"], ["top"])

                    def top16_of_products(a_ap):
                        TT(p, "dve", Pc[:], a_ap.unsqueeze(3).to_broadcast([128, 8, 16, 16]),
                           top[:, :, 1, :].unsqueeze(2).to_broadcast([128, 8, 16, 16]), ALU.mult, ["top", "tops"], ["Pc"])
                        for h in range(8):
                            p.op("dve", lambda e, h=h: e.max(out=best[:, h, 0:8], in_=Pc[:, h]), ["Pc"], ["best"])
                            p.op("dve", lambda e, h=h: e.match_replace(out=pct[:], in_to_replace=best[:, h, 0:8],
                                                                       in_values=Pc[:, h], imm_value=-1.0),
                                 ["Pc", "best"], ["pct"])
                            p.op("dve", lambda e, h=h: e.max(out=best[:, h, 8:16], in_=pct[:]), ["pct"], ["best"])

                    top16_of_products(top[:, :, 0, :])
                    p.op("dve", lambda e: e.tensor_reduce(out=Z[:, 0:8], in_=best[:], axis=AX.X, op=ALU.add), ["best"], ["Z"])
                    p.op("dve", lambda e: e.reciprocal(out=Z[:, 8:16], in_=Z[:, 0:8]), ["Z"], ["Z"])
                    rz = Z[:, 8:16]
                    TT(p, "dve", E[:, tl, :, 0, :], E[:, tl, :, 0, :], rz.unsqueeze(2).to_broadcast([128, 8, 128]), ALU.mult,
                       ["E", "Z"], ["E"])
                    TT(p, "dve", tops[:], top[:, :, 0, :], rz.unsqueeze(2).to_broadcast([128, 8, 16]), ALU.mult,
                       ["top", "Z"], ["tops"])
                    top16_of_products(tops[:])
                    CPY(p, "dve", pthr[:, tl, :], best[:, :, 15], ["best"], ["pthr"])
                    p.op("dve", lambda e, tl=tl: e.reciprocal(out=Z[:, 0:8], in_=pthr[:, tl, :]), ["pthr"], ["Z"])
                    TT(p, "dve", E[:, tl, :, 0, :], E[:, tl, :, 0, :], Z[:, 0:8].unsqueeze(2).to_broadcast([128, 8, 128]),
                       ALU.mult, ["E", "Z"], ["E"])
                    TT(p, "dve", Dh[:, tl], self.ident[:].unsqueeze(1).to_broadcast([128, 8, 128]),
                       pthr[:, tl, :].unsqueeze(2).to_broadcast([128, 8, 128]), ALU.mult, ["ident", "pthr"], ["Dh"])
                p.flush()
                if self.gstop == 2:
                    return
                n_ch = (self.geg * EG) if self.gstop else NCH
                THR = 1.0 - 1e-6
                cnt = {"f": 0}

                def emit_uload(c):
                    p.dma("pool", ub[c % 3][:], self.uT[l * NCH + c], [], [f"ub{c % 3}"])

                def emit_act(c):
                    ui, gu = c % 2, c % 3
                    for k in range(KD):
                        MM(p, self.PS[ui][:, 0:nt], ub[gu][:, k * 128:(k + 1) * 128], h2T[:, k, 0:nt], k == 0, k == KD - 1,
                           [f"ub{gu}", "h2T"], [f"ps{ui}"])
                    ACTV(p, gact[gu][:, 0:nt], self.PS[ui][:, 0:nt], AF.Gelu, [f"ps{ui}"], [f"gact{gu}"])

                def emit_masks(c):
                    bc = 2 + c % 2
                    for tl in range(ntl):
                        fi = cnt["f"] % 2
                        cnt["f"] += 1
                        if tl == 2:
                            for h in range(8):
                                ACTV(p, Pf[fi][:, h, :], E[:, tl, h, 1, :], AF.Copy, ["E"], [f"Pf{fi}"],
                                     scale=E[:, tl, h, 0, c:c + 1])
                        else:
                            TT(p, "pool", Pf[fi][:], E[:, tl, :, 1, :], E[:, tl, :, 0, c:c + 1].to_broadcast([128, 8, 128]),
                               ALU.mult, ["E"], [f"Pf{fi}"])
                        STT(p, Mh[fi][:], Pf[fi][:], THR, Pf[fi][:], ALU.is_ge, ALU.mult, [f"Pf{fi}"], [f"Mh{fi}"])
                        for h in range(8):
                            MM(p, self.PS[bc][:, tl * 128:(tl + 1) * 128], Mh[fi][:, h, :], Dh[:, tl, h, :], h == 0, h == 7,
                               [f"Mh{fi}", "Dh"], [f"ps{bc}"])

                def emit_wT(c):
                    eg, cc = divmod(c, EG)
                    gi, gu, bc = eg % 2, c % 3, 2 + c % 2
                    TT(p, "dve", wT[gi][cc][:, 0:nt], self.PS[bc][:, 0:nt], gact[gu][:, 0:nt], ALU.mult,
                       [f"ps{bc}", f"gact{gu}"], [f"wT{gi}_{cc}"])

                def emit_vload(eg):
                    for cc in range(EG):
                        p.dma("pool", vb[cc][:], self.vv[l * NCH + eg * EG + cc], [], [f"vb_{cc}"])

                def emit_out(eg):
                    gi = eg % 2
                    if self.pipe_out:
                        n = 0
                        for half in range(2):
                            for tl in range(ntl):
                                pair = n % 2
                                n += 1
                                for cc in range(EG):
                                    for q in range(2):
                                        bo, dq = 4 + 2 * pair + q, half * 2 + q
                                        MM(p, self.PS[bo][:], wT[gi][cc][:, tl * 128:(tl + 1) * 128],
                                           vb[cc][:, dq * 512:(dq + 1) * 512], cc == 0, cc == EG - 1,
                                           [f"wT{gi}_{cc}", f"vb_{cc}"], [f"ps{bo}"])
                                for q in range(2):
                                    bo, dq = 4 + 2 * pair + q, half * 2 + q
                                    dst = acc[:, tl, dq * 512:(dq + 1) * 512]
                                    if eg == 0:
                                        CPY(p, "act", dst, self.PS[bo][:], [f"ps{bo}"], ["acc"])
                                    else:
                                        TT(p, "dve", dst, self.PS[bo][:], dst, ALU.add, [f"ps{bo}", "acc"], ["acc"])
                        return
                    for tl in range(ntl):
                        for dq in range(4):
                            bo = 4 + dq
                            for cc in range(EG):
                                MM(p, self.PS[bo][:], wT[gi][cc][:, tl * 128:(tl + 1) * 128], vb[cc][:, dq * 512:(dq + 1) * 512],
                                   cc == 0, cc == EG - 1, [f"wT{gi}_{cc}", f"vb_{cc}"], [f"ps{bo}"])
                            dst = acc[:, tl, dq * 512:(dq + 1) * 512]
                            if eg == 0:
                                CPY(p, "act", dst, self.PS[bo][:], [f"ps{bo}"], ["acc"])
                            else:
                                TT(p, "dve", dst, self.PS[bo][:], dst, ALU.add, [f"ps{bo}", "acc"], ["acc"])

                emit_uload(0)
                emit_uload(1)
                emit_vload(0)
                emit_act(0)
                for c in range(n_ch):
                    eg, cc = divmod(c, EG)
                    if c + 2 < n_ch:
                        emit_uload(c + 2)
                    if c + 1 < n_ch:
                        emit_act(c + 1)
                    emit_masks(c)
                    if c >= 1:
                        emit_wT(c - 1)
                        if (c - 1) % EG == EG - 1:
                            emit_out((c - 1) // EG)
                    if cc == 1 and eg >= 1:
                        emit_vload(eg)
                    if cc == EG - 1:
                        p.flush()
                emit_wT(n_ch - 1)
                emit_out(n_ch // EG - 1)
                p.flush()
                if self.gstop == 3:
                    return
                self.load_bcast("sp", bcA[:], self.mod_row(l, 5), "bcgm", ["modscr"])
                if last:
                    self.load_bcast("sp", bcB[:], self.fing[0], "bcsh")
                for tl in range(ntl):
                    i = tl % 2
                    t = b0 + tl
                    rows = slice(t * 128, (t + 1) * 128)
                    p.dma("sp", xb[i][:], self.xres[rows, :], self.xkeys(t), ["gx0"])
                    TT(p, "dve", acc[:, tl, :], acc[:, tl, :], bcA[:], ALU.mult, ["acc", "bcgm"], ["acc"])
                    TT(p, "pool", xb[i][:], xb[i][:], acc[:, tl, :], ALU.add, ["gx0", "acc"], ["gx0"])
                    if not last or self.dbg:
                        p.dma("act", self.xres[rows, :], xb[i][:], ["gx0"], self.xkeys(t))
                    if last and t >= 2:
                        self.norm_tile(xb[i][:], "gx0", bcB, None, ["bcsh"], acc[:, tl, :], "acc", sm[i], f"gsm{i}",
                                       hb[i][:], f"gh{i}", None)
                        p.dma("act", self.y[(t - 2) * 128:(t - 1) * 128, :], acc[:, tl, :], ["acc"], ["y"])
                p.flush()

    def stage_A(self, l):
        self.p.barrier()
        self.lst = ExitStack()


def _t5_bucket(dist):
    max_exact = 16
    d = np.maximum(dist, 0)
    large = max_exact + (np.log(np.maximum(d, 1).astype(np.float32) / max_exact)
                         / np.float32(np.log(128 / max_exact)) * (32 - max_exact)).astype(np.int32)
    large = np.minimum(large, 31)
    return np.where(d < max_exact, d, large)


def _chunk_cols(w, ncols):
    Lx, Kd, N = w.shape
    a = w.reshape(Lx, Kd // 128, 128, N // ncols, ncols).transpose(0, 3, 2, 1, 4)
    return np.ascontiguousarray(a).reshape(Lx * (N // ncols), 128, (Kd // 128) * ncols)


_CACHE = {}


def prepare_shared(inp):
    f = lambda a: np.asarray(a, dtype=np.float32)
    w_in = f(inp["w_in"])
    sh = {}
    sh["ada_w"] = _chunk_cols(f(inp["ada_w"]), 512)
    sh["ada_b"] = f(inp["ada_b"])
    sh["n1g"] = f(inp["norm1_g"])
    sh["n2g"] = f(inp["norm2_g"])
    sh["fing"] = f(inp["final_g"]).reshape(1, D)
    sh["w_in_c"] = _chunk_cols(w_in, 128)
    kcols = w_in[:, :, 3072:3328].reshape(L, D, 4, 64)
    kd = np.concatenate([kcols, kcols], axis=3).reshape(L, D, 512)
    sh["w_kd"] = _chunk_cols(kd, 128)
    sh["w_v"] = _chunk_cols(np.ascontiguousarray(w_in[:, :, 3328:3584]), 256)
    sh["w_co"] = _chunk_cols(f(inp["w_conv_out"]), 128)
    sh["w_ao"] = _chunk_cols(f(inp["w_attn_out"]), 128)
    sh["w_o"] = _chunk_cols(f(inp["w_out"]), 512)
    sh["w_pq"] = _chunk_cols(f(inp["w_pq"]), 128)
    sk = f(inp["sub_keys"])
    sh["skT"] = np.ascontiguousarray(sk.transpose(0, 4, 1, 2, 3)).reshape(L, 128, D)
    pu = f(inp["peer_u"])
    sh["uT"] = np.ascontiguousarray(pu.reshape(L, NCH, 128, KD, 128).transpose(0, 1, 4, 3, 2)).reshape(L * NCH, 128, D)
    sh["vv"] = f(inp["peer_v"]).reshape(L * NCH, 128, D)
    cp = np.concatenate([f(inp["dw_w"]).transpose(0, 2, 1), f(inp["dw_b"])[:, :, None],
                         f(inp["conv_ln_g"])[:, :, None], f(inp["conv_ln_b"])[:, :, None]], axis=2)
    sh["chanpar"] = np.ascontiguousarray(cp.reshape(L, 8, 128, 34).transpose(0, 2, 1, 3)).reshape(L, 128, 8 * 34)
    sh["sinks"] = f(inp["attn_sinks"])
    qi = np.arange(128)[:, None] + 128
    kj = np.arange(256)[None, :]
    dist = qi - kj
    bucket = _t5_bucket(dist)
    rb = f(inp["rel_bias"])
    tab = rb[bucket]
    band = (dist >= 0) & (dist < 128)
    tab = np.where(band[:, :, None], tab, np.float32(NEG))
    sh["biasT"] = np.ascontiguousarray(tab.transpose(0, 2, 1)).reshape(128, 16 * 256)
    return sh


def per_core(inp, c):
    b, qtr = c // 4, c % 4
    s0 = qtr * 2048
    x = np.asarray(inp["x"], dtype=np.float32)
    xw = np.zeros((T, D), np.float32)
    if qtr == 0:
        xw[256:] = x[b, 0:2048]
    else:
        xw[:] = x[b, s0 - 256:s0 + 2048]
    cc = np.asarray(inp["c"], dtype=np.float32)[b]
    flags = np.zeros((128, 2), np.float32)
    flags[:, 0] = 0.0 if qtr == 0 else 1.0
    flags[:, 1] = NEG if qtr == 0 else 0.0
    return {"x_in": xw, "c_in": np.ascontiguousarray(cc.reshape(KD, 128).T), "flags": flags}


def kernel(**inputs):
    if "nc" not in _CACHE:
        _CACHE["nc"] = K().build()
    nc = _CACHE["nc"]
    sh = prepare_shared(inputs)
    in_maps = []
    for c in range(8):
        m = dict(sh)
        m.update(per_core(inputs, c))
        in_maps.append(m)
    res = run_bass_kernel_spmd(nc, in_maps, core_ids=list(range(8)))
    out = np.zeros((2, SEQ, D), np.float32)
    for c in range(8):
        b, qtr = c // 4, c % 4
        out[b, qtr * 2048:(qtr + 1) * 2048] = res.results[c]["y"]
    return out
```

```python
import numpy as np
from contextlib import ExitStack
import concourse.bass as bass
import concourse.mybir as mybir
from concourse.bass_utils import run_bass_kernel_spmd

F32 = mybir.dt.float32
BF16 = mybir.dt.bfloat16
AF = mybir.ActivationFunctionType
ALU = mybir.AluOpType
AX = mybir.AxisListType

L = 2
D = 2048
KD = 16
NB = 18
T = NB * 128
GS = 384
NG = T // GS
SEQ = 8192
NEG = -30000.0
EPS = 1e-6
NCH = 128
TB = 3
EG = 4

ENGS = ["pe", "act", "dve", "pool", "sp"]
ENGMAP = {"pe": "tensor", "act": "scalar", "dve": "vector", "pool": "gpsimd", "sp": "sync"}
EPOCH = 1 << 30
NDMASEM = 6


class Prog:
    def __init__(self, nc, stack, block):
        self.nc, self.stack, self.block = nc, stack, block
        self.q = {e: [] for e in ENGS}
        self.cnt = {e: 0 for e in ENGS}
        self.known = {e: {} for e in ENGS}
        self.last_w, self.readers = {}, {}
        self.nsem = 0
        self.sems = {e: self._newsem(e) for e in ENGS}
        self.dsem = {qn: [[self._newsem("d" + qn), 0] for _ in range(NDMASEM)] for qn in ["sp", "act", "pool"]}
        self.dsem_i = {qn: 0 for qn in self.dsem}
        self.n_instr = 0

    def _newsem(self, name):
        self.nsem += 1
        return self.stack.enter_context(self.nc.semaphore(f"s{self.nsem}_{name}"))

    def _deps(self, eng, reads, writes):
        deps = []
        for k in reads:
            deps.extend(self.last_w.get(k, {}).values())
        for k in writes:
            for ev in self.last_w.get(k, {}).values():
                if ev[2] != eng:
                    deps.append(ev)
            for ev in self.readers.get(k, {}).values():
                if ev[2] != eng:
                    deps.append(ev)
        waits = {}
        kn = self.known[eng]
        for (sem, val, src) in deps:
            sid = id(sem)
            if kn.get(sid, 0) >= val:
                continue
            if sid not in waits or waits[sid][1] < val:
                waits[sid] = (sem, val)
        for sid, (sem, val) in waits.items():
            kn[sid] = val
        return list(waits.values())

    def _commit(self, ev, reads, writes):
        sid = id(ev[0])
        for k in writes:
            self.last_w.setdefault(k, {})[sid] = ev
        for k in reads:
            self.readers.setdefault(k, {})[sid] = ev

    def op(self, eng, fn, reads=(), writes=()):
        psr = [k for k in reads if k.startswith("ps") and k[2:].isdigit()]
        if psr:
            writes = list(writes) + psr
        waits = self._deps(eng, reads, writes)
        if self.cnt[eng] >= EPOCH:
            self.sems[eng] = self._newsem(eng)
            self.cnt[eng] = 0
        self.cnt[eng] += 1
        sem = self.sems[eng]
        ev = (sem, self.cnt[eng], eng)
        self.q[eng].append((waits, fn, sem, 1))
        self._commit(ev, reads, writes)
        self.n_instr += 1

    def dma(self, qn, out, in_, reads=(), writes=(), **kw):
        slot = self.dsem[qn][self.dsem_i[qn] % NDMASEM]
        self.dsem_i[qn] += 1
        sem, cnt = slot
        waits = self._deps(qn, reads, writes)
        if cnt > 0 and self.known[qn].get(id(sem), 0) < cnt:
            waits.append((sem, cnt))
            self.known[qn][id(sem)] = cnt
        slot[1] = cnt + 16
        ev = (sem, cnt + 16, "dma_" + qn)
        self.q[qn].append((waits, lambda e: e.dma_start(out=out, in_=in_, **kw), sem, 16))
        self._commit(ev, reads, writes)
        self.n_instr += 1

    def barrier(self):
        evs = [(self.sems[e], self.cnt[e]) for e in ENGS if self.cnt[e] > 0]
        for qn in self.dsem:
            evs += [(s, c) for (s, c) in self.dsem[qn] if c > 0]
        for e in ENGS:
            waits = []
            for (s, v) in evs:
                if s is self.sems[e]:
                    continue
                if self.known[e].get(id(s), 0) < v:
                    waits.append((s, v))
                    self.known[e][id(s)] = v
            if waits:
                self.q[e].append((waits, None, None, 0))

    def flush(self):
        for e in ENGS:
            items, self.q[e] = self.q[e], []
            if not items:
                continue

            def body(engobj, items=items):
                for (waits, fn, sem, inc) in items:
                    for (s, v) in waits:
                        engobj.wait_ge(s, v)
                    if fn is not None:
                        fn(engobj).then_inc(sem, inc)

            getattr(self.block, ENGMAP[e])(body)


def MM(p, out, lhsT, rhs, st, sp, r, w):
    p.op("pe", lambda e: e.matmul(out, lhsT=lhsT, rhs=rhs, start=st, stop=sp), r, w)


def ACTV(p, out, in_, func, r, w, bias=None, scale=None, accum=None):
    kw = {}
    if bias is not None:
        kw["bias"] = bias
    if scale is not None:
        kw["scale"] = scale
    if accum is not None:
        kw["accum_out"] = accum
    p.op("act", lambda e: e.activation(out=out, in_=in_, func=func, **kw), r, w)


def TT(p, eng, out, a, b, op, r, w):
    p.op(eng, lambda e: e.tensor_tensor(out=out, in0=a, in1=b, op=op), r, w)


def TS(p, eng, out, a, s1, op0, r, w, s2=None, op1=None):
    if op1 is None:
        p.op(eng, lambda e: e.tensor_scalar(out=out, in0=a, scalar1=s1, scalar2=None, op0=op0), r, w)
    else:
        p.op(eng, lambda e: e.tensor_scalar(out=out, in0=a, scalar1=s1, scalar2=s2, op0=op0, op1=op1), r, w)


def STT(p, out, a, s, b, op0, op1, r, w):
    p.op("dve", lambda e: e.scalar_tensor_tensor(out=out, in0=a, scalar=s, in1=b, op0=op0, op1=op1), r, w)


def CPY(p, eng, out, in_, r, w):
    if eng == "act":
        p.op("act", lambda e: e.activation(out=out, in_=in_, func=AF.Copy), r, w)
    else:
        p.op(eng, lambda e: e.tensor_copy(out=out, in_=in_), r, w)


class K:
    def __init__(self, dbg=None, nlayers=L):
        self.dbg = dbg
        self.nlayers = nlayers
        self.gstop = 0
        self.geg = 1
        self.in_shapes = {}
        self.only_g = False
        self.g1hp = 16
        self.g1mode = 1
        self.g1var = 0
        self.pipe_out = False
        nc = self.nc = bass.Bass("TRN2", target_bir_lowering=False)

        def din(name, shape, dt=F32):
            self.in_shapes[name] = list(shape)
            return nc.dram_tensor(name, list(shape), dt, kind="ExternalInput").ap()

        def dscr(name, shape, dt):
            kind = "ExternalOutput" if dbg else "Internal"
            return nc.dram_tensor(name, list(shape), dt, kind=kind).ap()

        self.x_in = din("x_in", [T, D])
        self.c_in = din("c_in", [128, KD])
        self.flags = din("flags", [128, 2])
        self.biasT = din("biasT", [128, 16 * 256])
        self.ada_w = din("ada_w", [L * 24, 128, KD * 512])
        self.ada_b = din("ada_b", [L, 6 * D])
        self.n1g = din("n1g", [L, D])
        self.n2g = din("n2g", [L, D])
        self.fing = din("fing", [1, D])
        self.w_in_c = din("w_in_c", [L * 60, 128, D])
        self.w_kd = din("w_kd", [L * 4, 128, D])
        self.w_v = din("w_v", [L, 128, KD * 256])
        self.w_co = din("w_co", [L * 16, 128, 1024])
        self.w_ao = din("w_ao", [L * 16, 128, 1024])
        self.w_o = din("w_o", [L * 4, 128, KD * 512])
        self.w_pq = din("w_pq", [L * 16, 128, D])
        self.skT = din("skT", [L, 128, D])
        self.uT = din("uT", [L * NCH, 128, D])
        self.vv = din("vv", [L * NCH, 128, D])
        self.chanpar = din("chanpar", [L, 128, 8 * 34])
        self.sinks = din("sinks", [L, 16])
        self.y = nc.dram_tensor("y", [16 * 128, D], F32, kind="ExternalOutput").ap()

        self.xres = dscr("xres", [T, D], F32)
        self.modscr = dscr("modscr", [L, 6 * D], F32)
        self.qT_s = dscr("qT_s", [8, 128, T], BF16)
        self.kdT_s = dscr("kdT_s", [4, 128, T], BF16)
        self.v_s = dscr("v_s", [T, 256], BF16)
        self.gT_s = dscr("gT_s", [32, 128, T], BF16)
        self.mixT_s = dscr("mixT_s", [16, 128, T], BF16)
        if dbg:
            self.dbgA = dscr("dbgA", [16, 128, T], BF16)
            self.dbgB = dscr("dbgB", [16, 128, T], BF16)

    def build(self):
        nc = self.nc
        with ExitStack() as st, nc.Block() as block:
            p = self.p = Prog(nc, st, block)
            self.PS = [st.enter_context(nc.psum_tensor(f"ps{i}", [128, 512], F32)) for i in range(8)]
            self.ident = st.enter_context(nc.sbuf_tensor("ident", [128, 128], BF16))
            self.ones = st.enter_context(nc.sbuf_tensor("ones", [128, 128], BF16))
            self.flg = st.enter_context(nc.sbuf_tensor("flg", [128, 2], F32))
            ident, ones = self.ident, self.ones
            p.op("pool", lambda e: e.memset(ident[:], 0.0), [], ["ident"])
            p.op("pool", lambda e: e.affine_select(out=ident[:], in_=ident[:], pattern=[[-1, 128]],
                                                   compare_op=ALU.not_equal, fill=1.0, base=0,
                                                   channel_multiplier=1), ["ident"], ["ident"])
            p.op("pool", lambda e: e.memset(ones[:], 1.0), [], ["ones"])
            p.dma("sp", self.flg[:], self.flags, [], ["flg"])
            if self.only_g:
                self.stage_G(0)
                p.barrier()
                p.flush()
                return nc
            self.stage_mods()
            done = False
            for l in range(self.nlayers):
                for nm in ["A", "B", "C", "D", "E", "F", "G"]:
                    getattr(self, "stage_" + nm)(l)
                    if self.dbg == f"{l}{nm}":
                        done = True
                        break
                if done:
                    break
            p.barrier()
            p.flush()
            if getattr(self, "lst", None) is not None:
                self.lst.close()
        return nc

    def stage_ctx(self):
        self.p.barrier()
        return ExitStack()

    def sb(self, st, name, shape, dt):
        self.uid = getattr(self, "uid", 0) + 1
        return st.enter_context(self.nc.sbuf_tensor(f"{name}_u{self.uid}", list(shape), dt))

    def stage_mods(self):
        p = self.p
        with self.stage_ctx() as st:
            cs = self.sb(st, "cs", [128, KD], F32)
            wb = [self.sb(st, f"adab{i}", [128, KD * 512], F32) for i in range(2)]
            ab = [self.sb(st, f"ab{i}", [1, 512], F32) for i in range(2)]
            mr = [self.sb(st, f"mr{i}", [1, 512], F32) for i in range(2)]
            p.dma("sp", cs[:], self.c_in, [], ["cs"])
            ACTV(p, cs[:], cs[:], AF.Silu, ["cs"], ["cs"])
            for l in range(self.nlayers):
                for n in range(24):
                    i = n % 2
                    p.dma("sp" if i == 0 else "act", wb[i][:], self.ada_w[l * 24 + n], [], [f"adab{i}"])
                    p.dma("sp", ab[i][:], self.ada_b[l:l + 1, n * 512:(n + 1) * 512], [], [f"ab{i}"])
                    ps = self.PS[i]
                    for k in range(KD):
                        MM(p, ps[0:1, :], cs[:, k:k + 1], wb[i][:, k * 512:(k + 1) * 512], k == 0, k == KD - 1,
                           ["cs", f"adab{i}"], [f"ps{i}"])
                    TT(p, "dve", mr[i][:], ps[0:1, :], ab[i][:], ALU.add, [f"ps{i}", f"ab{i}"], [f"mr{i}"])
                    p.dma("sp", self.modscr[l:l + 1, n * 512:(n + 1) * 512], mr[i][:], [f"mr{i}"], ["modscr"])
            p.flush()

    def load_bcast(self, q, dst, src_row, key, reads=()):
        keys = key if isinstance(key, list) else [key]
        self.p.dma(q, dst, src_row.partition_broadcast(128), list(reads), keys)

    def mod_row(self, l, i):
        return self.modscr[l, i * D:(i + 1) * D]

    def make_gm(self, gm, sh, tmp, tk, l, gain_row, sc_i, sh_i, pfx, kgm=None, ksh=None):
        p = self.p
        kgm = kgm or pfx + "gm"
        ksh = ksh or pfx + "sh"
        self.load_bcast("sp", gm[:], gain_row, kgm)
        self.load_bcast("sp", tmp[:], self.mod_row(l, sc_i), tk, ["modscr"])
        self.load_bcast("sp", sh[:], self.mod_row(l, sh_i), ksh, ["modscr"])
        STT(p, gm[:], tmp[:], 1.0, gm[:], ALU.add, ALU.mult, [tk, kgm], [kgm])

    def norm_tile(self, xt, xk, gm, sh, gk, hb, hk, sm, smk, junk, jk, tmp):
        p = self.p
        ACTV(p, junk, xt, AF.Square, [xk], [jk, smk + "ss"], accum=sm[:, 0:1])
        TS(p, "dve", sm[:, 1:2], sm[:, 0:1], 1.0 / D, ALU.mult, [smk + "ss"], [smk + "a"], s2=EPS, op1=ALU.add)
        ACTV(p, sm[:, 2:3], sm[:, 1:2], AF.Sqrt, [smk + "a"], [smk + "b"])
        p.op("dve", lambda e: e.reciprocal(out=sm[:, 3:4], in_=sm[:, 2:3]), [smk + "b"], [smk + "r"])
        if sh is None:
            STT(p, hb, xt, sm[:, 3:4], gm[:], ALU.mult, ALU.mult, [xk, smk + "r"] + gk, [hk])
        else:
            STT(p, tmp, xt, sm[:, 3:4], gm[:], ALU.mult, ALU.mult, [xk, smk + "r"] + gk, ["ntmp"])
            TT(p, "dve", hb, tmp, sh[:], ALU.add, ["ntmp"] + gk, [hk])

    def transpose_tile(self, hb, hk, dst_fn, dk, bank0):
        p = self.p
        for half in range(2):
            bi = bank0 + half
            psb = self.PS[bi][:].bitcast(BF16)
            for j in range(8):
                k = half * 8 + j
                p.op("pe", lambda e, j=j, k=k, psb=psb: e.transpose(psb[:, j * 128:(j + 1) * 128],
                                                                   hb[:, k * 128:(k + 1) * 128], self.ident[:]),
                     [hk, "ident"], [f"ps{bi}"])
            src = psb[:, 0:1024].rearrange("p (j t) -> p j t", j=8)
            CPY(p, "act" if half == 0 else "dve", dst_fn(half * 8), src, [f"ps{bi}"], [dk])

    def xsrc(self, l):
        return self.x_in if l == 0 else self.xres

    def xkeys(self, t, nq=None):
        return [f"xr{t}_{q}" for q in (range(4) if nq is None else [nq])]

    def stage_B(self, l):
        p = self.p
        self.convT = convT = self.sb(self.lst, "convT", [128, 8, T], BF16)
        with self.stage_ctx() as st:
            hT = self.sb(st, "hT", [128, KD, T], BF16)
            with ExitStack() as st2:
                gm = self.sb(st2, "n1gm", [128, D], F32)
                sh = self.sb(st2, "n1sh", [128, D], F32)
                tmp = self.sb(st2, "ntmp", [128, D], F32)
                self.make_gm(gm, sh, tmp, "ntmp", l, self.n1g[l], 1, 0, "n1")
                xb = [self.sb(st2, f"xb{i}", [128, D], F32) for i in range(2)]
                hb = [self.sb(st2, f"hb{i}", [128, D], BF16) for i in range(2)]
                sm = [self.sb(st2, f"sm{i}", [128, 4], F32) for i in range(2)]
                for t in range(NB):
                    i = t % 2
                    p.dma("sp", xb[i][:], self.xsrc(l)[t * 128:(t + 1) * 128, :], self.xkeys(t), [f"xb{i}"])
                    self.norm_tile(xb[i][:], f"xb{i}", gm, sh, ["n1gm", "n1sh"], hb[i][:], f"hb{i}", sm[i], f"sm{i}",
                                   hb[i][:], f"hb{i}", tmp[:])
                    self.transpose_tile(hb[i], f"hb{i}", lambda k0, t=t: hT[:, k0:k0 + 8, t * 128:(t + 1) * 128],
                                        f"hT{t // 3}", 2 * i)
                p.flush()
            p.barrier()
            if self.dbg == f"{l}A":
                for k in range(KD):
                    p.dma("sp", self.dbgA[k], hT[:, k, :], [f"hT{g}" for g in range(NG)], ["dbgA"])
                p.flush()
                return
            wt = [self.sb(st, f"wt{i}", [128, D], BF16) for i in range(3)]
            stg = [self.sb(st, f"stg{i}", [128, GS], BF16) for i in range(3)]
            sig = [self.sb(st, f"sig{i}", [128, GS], F32) for i in range(2)]
            upad = [self.sb(st, f"upad{i}", [128, 32 + T], F32) for i in range(2)]
            cacc = self.sb(st, "cacc", [128, T], F32)
            cp = self.sb(st, "cp", [128, 8, 34], F32)
            wv = self.sb(st, "wv", [128, KD * 256], BF16)
            vst = [self.sb(st, f"vst{i}", [128, 256], BF16) for i in range(2)]
            p.dma("sp", cp[:], self.chanpar[l].rearrange("p (c t) -> p c t", c=8), [], ["cp"])
            for i in range(2):
                p.op("pool", lambda e, i=i: e.memset(upad[i][:], 0.0), [], [f"upad{i}"])
            state = {"w": 0, "ps": 0, "stg": 0}

            def proj(wsrc, evac):
                wi = state["w"] % 3
                state["w"] += 1
                p.dma("pool", wt[wi][:], wsrc, [], [f"wt{wi}"])
                for g in range(NG):
                    bi = state["ps"] % 4
                    state["ps"] += 1
                    ps = self.PS[bi]
                    for k in range(KD):
                        MM(p, ps[:, 0:GS], wt[wi][:, k * 128:(k + 1) * 128], hT[:, k, g * GS:(g + 1) * GS],
                           k == 0, k == KD - 1, [f"wt{wi}", f"hT{g}"], [f"ps{bi}"])
                    evac(g, ps[:, 0:GS], f"ps{bi}")

            def evac_store(dst, func):
                def f(g, ps, pk):
                    si = state["stg"] % 3
                    state["stg"] += 1
                    ACTV(p, stg[si][:], ps, func, [pk], [f"stg{si}"])
                    p.dma("sp", dst[:, g * GS:(g + 1) * GS], stg[si][:], [f"stg{si}"], ["projout"])
                return f

            for c in range(8):
                proj(self.w_in_c[l * 60 + 16 + c], evac_store(self.qT_s[c], AF.Copy))
            for c in range(4):
                proj(self.w_kd[l * 4 + c], evac_store(self.kdT_s[c], AF.Copy))
            for c in range(32):
                proj(self.w_in_c[l * 60 + 28 + c], evac_store(self.gT_s[c], AF.Sigmoid))
            p.dma("pool", wv[:], self.w_v[l], [], ["wv"])
            for t in range(NB):
                bi = 4 + (t % 2)
                ps = self.PS[bi]
                for k in range(KD):
                    MM(p, ps[:, 0:256], hT[:, k, t * 128:(t + 1) * 128], wv[:, k * 256:(k + 1) * 256],
                       k == 0, k == KD - 1, ["wv", f"hT{t // 3}"], [f"ps{bi}"])
                CPY(p, "act", vst[t % 2][:], ps[:, 0:256], [f"ps{bi}"], [f"vst{t % 2}"])
                p.dma("sp", self.v_s[t * 128:(t + 1) * 128, :], vst[t % 2][:], [f"vst{t % 2}"], ["projout"])
            p.flush()
            for c in range(8):
                ui = c % 2
                up = upad[ui]
                wa = state["w"] % 3
                state["w"] += 1
                wbi = state["w"] % 3
                state["w"] += 1
                p.dma("pool", wt[wa][:], self.w_in_c[l * 60 + c], [], [f"wt{wa}"])
                p.dma("pool", wt[wbi][:], self.w_in_c[l * 60 + 8 + c], [], [f"wt{wbi}"])
                for g in range(NG):
                    ba = (2 * g) % 4
                    bb = (2 * g + 1) % 4
                    for k in range(KD):
                        MM(p, self.PS[ba][:, 0:GS], wt[wa][:, k * 128:(k + 1) * 128], hT[:, k, g * GS:(g + 1) * GS],
                           k == 0, k == KD - 1, [f"wt{wa}", f"hT{g}"], [f"ps{ba}"])
                    for k in range(KD):
                        MM(p, self.PS[bb][:, 0:GS], wt[wbi][:, k * 128:(k + 1) * 128], hT[:, k, g * GS:(g + 1) * GS],
                           k == 0, k == KD - 1, [f"wt{wbi}", f"hT{g}"], [f"ps{bb}"])
                    ACTV(p, sig[g % 2][:], self.PS[bb][:, 0:GS], AF.Sigmoid, [f"ps{bb}"], [f"sig{g % 2}"])
                    TT(p, "dve", up[:, 32 + g * GS:32 + (g + 1) * GS], self.PS[ba][:, 0:GS], sig[g % 2][:], ALU.mult,
                       [f"ps{ba}", f"sig{g % 2}"], [f"upad{ui}"])
                TS(p, "dve", up[:, 32:32 + 256], up[:, 32:32 + 256], self.flg[:, 0:1], ALU.mult,
                   [f"upad{ui}", "flg"], [f"upad{ui}"])
                TS(p, "dve", cacc[:], up[:, 2:2 + T], cp[:, c, 0:1], ALU.mult, [f"upad{ui}", "cp"], ["cacc"],
                   s2=cp[:, c, 31:32], op1=ALU.add)
                for j in range(1, 31):
                    dst = cacc[:] if j < 30 else convT[:, c, :]
                    STT(p, dst, up[:, 2 + j:2 + j + T], cp[:, c, j:j + 1], cacc[:], ALU.mult, ALU.add,
                        [f"upad{ui}", "cp", "cacc"], ["cacc"] if j < 30 else ["convT"])
                p.flush()

    def stage_C(self, l):
        p = self.p
        convT = self.convT
        self.p.barrier()
        self.uactT = uactT = self.sb(self.lst, "uactT", [128, 8, T], BF16)
        with ExitStack() as st2:
            cp = self.sb(st2, "cp2", [128, 8, 34], F32)
            sq = [self.sb(st2, f"sq{i}", [128, GS], BF16) for i in range(2)]
            mean = self.sb(st2, "mean", [128, GS], F32)
            m2 = self.sb(st2, "m2", [128, GS], F32)
            var = self.sb(st2, "var", [128, GS], F32)
            rstd = self.sb(st2, "rstd", [128, GS], F32)
            t1 = [self.sb(st2, f"t1{i}", [128, GS], F32) for i in range(2)]
            t2 = [self.sb(st2, f"t2{i}", [128, GS], F32) for i in range(2)]
            p.dma("sp", cp[:], self.chanpar[l].rearrange("p (c t) -> p c t", c=8), [], ["cp2"])
            for g in range(NG):
                sl = slice(g * GS, (g + 1) * GS)
                b1, b2 = 2 * (g % 2), 2 * (g % 2) + 1
                for c in range(8):
                    MM(p, self.PS[b1][:, 0:GS], self.ones[:], convT[:, c, sl], c == 0, c == 7, ["ones", "convT"],
                       [f"ps{b1}"])
                for c in range(8):
                    ACTV(p, sq[c % 2][:], convT[:, c, sl], AF.Square, ["convT"], [f"sq{c % 2}"])
                    MM(p, self.PS[b2][:, 0:GS], self.ones[:], sq[c % 2][:], c == 0, c == 7, ["ones", f"sq{c % 2}"],
                       [f"ps{b2}"])
                ACTV(p, mean[:], self.PS[b1][:, 0:GS], AF.Copy, [f"ps{b1}"], ["mean"], scale=1.0 / 1024)
                TT(p, "dve", m2[:], mean[:], mean[:], ALU.mult, ["mean"], ["m2"])
                STT(p, var[:], self.PS[b2][:, 0:GS], 1.0 / 1024, m2[:], ALU.mult, ALU.subtract, [f"ps{b2}", "m2"], ["var"])
                TS(p, "dve", var[:], var[:], EPS, ALU.add, ["var"], ["var"])
                ACTV(p, var[:], var[:], AF.Sqrt, ["var"], ["var"])
                p.op("dve", lambda e: e.reciprocal(out=rstd[:], in_=var[:]), ["var"], ["rstd"])
                for c in range(8):
                    i = c % 2
                    TT(p, "dve", t1[i][:], convT[:, c, sl], mean[:], ALU.subtract, ["convT", "mean"], [f"t1{i}"])
                    TT(p, "pool", t2[i][:], t1[i][:], rstd[:], ALU.mult, [f"t1{i}", "rstd"], [f"t2{i}"])
                    ACTV(p, uactT[:, c, sl], t2[i][:], AF.Silu, [f"t2{i}", "cp2"], ["uactT"],
                         bias=cp[:, c, 33:34], scale=cp[:, c, 32:33])
            p.flush()
        if self.dbg == f"{l}C":
            p.barrier()
            for c in range(8):
                p.dma("sp", self.dbgA[c], uactT[:, c, :], ["uactT"], ["dbgA"])
                p.dma("sp", self.dbgB[c], convT[:, c, :], ["convT"], ["dbgB"])
            p.flush()

    def stage_D(self, l):
        p = self.p
        p.barrier()
        self.attnT = attnT = self.sb(self.lst, "attnT", [128, 8, T], BF16)
        p.op("pool", lambda e: e.memset(attnT[:, :, 0:128], 0.0), [], ["attnT"])
        with ExitStack() as st:
            bias = self.sb(st, "bias", [128, 16, 256], F32)
            bias2 = self.sb(st, "bias2", [128, 16, 256], F32)
            snk = self.sb(st, "snk", [128, 16], F32)
            qb = [self.sb(st, f"qb{i}", [128, 8, 128], BF16) for i in range(2)]
            kb = [self.sb(st, f"kb{i}", [128, 4, 256], BF16) for i in range(2)]
            vb = [self.sb(st, f"vb{i}", [128, 2, 256], BF16) for i in range(2)]
            sc = [self.sb(st, f"sc{i}", [128, 2, 256], F32) for i in range(2)]
            pb = [self.sb(st, f"pb{i}", [128, 2, 256], BF16) for i in range(2)]
            pT = [self.sb(st, f"pT{i}", [128, 4, 128], BF16) for i in range(2)]
            sm = [self.sb(st, f"asm{i}", [128, 16], F32) for i in range(2)]
            rs = [self.sb(st, f"rs{i}", [128, 16], F32) for i in range(2)]
            es = [self.sb(st, f"es{i}", [128, 16], F32) for i in range(2)]
            ab = [self.sb(st, f"ab_{i}", [128, 1024], BF16) for i in range(2)]
            p.dma("sp", bias[:], self.biasT.rearrange("p (h s) -> p h s", h=16), [], ["bias"])
            p.dma("sp", bias2[:], self.biasT.rearrange("p (h s) -> p h s", h=16), [], ["bias2"])
            self.load_bcast("sp", snk[:], self.sinks[l], "snk")
            TS(p, "dve", bias2[:, :, 0:128], bias2[:, :, 0:128], self.flg[:, 1:2], ALU.add, ["bias2", "flg"], ["bias2"])
            for b in range(1, NB):
                i = b % 2
                bt, bk = (bias2, "bias2") if b == 2 else (bias, "bias")
                t0 = b * 128
                p.dma("sp", qb[i][:], self.qT_s[:, :, t0:t0 + 128].rearrange("c p t -> p c t"), ["projout"], [f"qb{i}"])
                p.dma("sp", kb[i][:], self.kdT_s[:, :, t0 - 128:t0 + 128].rearrange("c p t -> p c t"), ["projout"],
                      [f"kb{i}"])
                p.dma("sp", vb[i][:], self.v_s[t0 - 128:t0 + 128, :].rearrange("(a p) d -> p a d", p=128), ["projout"],
                      [f"vb{i}"])
                ao_banks = (6, 7)
                for m in range(8):
                    j = m % 2
                    g = m // 2
                    tb_ = 4 + j
                    for hh in range(2):
                        lo = 64 * hh
                        sbk = 2 * j + hh
                        MM(p, self.PS[sbk][:, 0:256], qb[i][lo:lo + 64, m, :], kb[i][lo:lo + 64, g, :],
                           True, True, [f"qb{i}", f"kb{i}"], [f"ps{sbk}"])
                        STT(p, sc[j][:, hh, :], self.PS[sbk][:, 0:256], 0.125, bt[:, 2 * m + hh, :],
                            ALU.mult, ALU.add, [f"ps{sbk}", bk], [f"sc{j}"])
                    p.op("dve", lambda e, j=j, m=m: e.tensor_reduce(out=sm[j][:, 0:2], in_=sc[j][:], axis=AX.X,
                                                                    op=ALU.max), [f"sc{j}"], [f"asm{j}a"])
                    TT(p, "dve", sm[j][:, 2:4], sm[j][:, 0:2], snk[:, 2 * m:2 * m + 2], ALU.max, [f"asm{j}a", "snk"],
                       [f"asm{j}b"])
                    TS(p, "dve", sm[j][:, 4:6], sm[j][:, 2:4], -1.0, ALU.mult, [f"asm{j}b"], [f"asm{j}c"])
                    TT(p, "dve", sm[j][:, 6:8], snk[:, 2 * m:2 * m + 2], sm[j][:, 4:6], ALU.add, ["snk", f"asm{j}c"],
                       [f"asm{j}d"])
                    ACTV(p, es[i][:, 2 * m:2 * m + 2], sm[j][:, 6:8], AF.Exp, [f"asm{j}d"], [f"es{i}"])
                    for hh in range(2):
                        ACTV(p, pb[j][:, hh, :], sc[j][:, hh, :], AF.Exp, [f"sc{j}", f"asm{j}c"], [f"pb{j}", f"rs{i}"],
                             bias=sm[j][:, 4 + hh:5 + hh], accum=rs[i][:, 2 * m + hh:2 * m + hh + 1])
                    psb = self.PS[tb_][:].bitcast(BF16)
                    for hh in range(2):
                        for half in range(2):
                            n = hh * 2 + half
                            p.op("pe", lambda e, psb=psb, n=n, j=j, hh=hh, half=half: e.transpose(
                                psb[:, n * 128:(n + 1) * 128], pb[j][:, hh, half * 128:(half + 1) * 128], self.ident[:]),
                                 [f"pb{j}", "ident"], [f"ps{tb_}"])
                    CPY(p, "act" if j == 0 else "dve", pT[j][:], psb[:, 0:512].rearrange("p (n t) -> p n t", n=4),
                        [f"ps{tb_}"], [f"pT{j}"])
                    for hh in range(2):
                        h = 2 * m + hh
                        bo = ao_banks[h // 8]
                        col = (h % 8) * 64
                        for half in range(2):
                            MM(p, self.PS[bo][:, col:col + 64], pT[j][:, hh * 2 + half, :],
                               vb[i][:, half, g * 64:(g + 1) * 64], half == 0, half == 1,
                               [f"pT{j}", f"vb{i}"], [f"ps{bo}"])
                TT(p, "dve", rs[i][:], rs[i][:], es[i][:], ALU.add, [f"rs{i}", f"es{i}"], [f"rs{i}"])
                p.op("dve", lambda e, i=i: e.reciprocal(out=rs[i][:], in_=rs[i][:]), [f"rs{i}"], [f"rs{i}"])
                for hb_ in range(2):
                    bo = ao_banks[hb_]
                    TT(p, "dve", ab[i][:, hb_ * 512:(hb_ + 1) * 512].rearrange("p (h d) -> p h d", h=8),
                       self.PS[bo][:].rearrange("p (h d) -> p h d", h=8),
                       rs[i][:, hb_ * 8:(hb_ + 1) * 8].unsqueeze(2).to_broadcast([128, 8, 64]), ALU.mult,
                       [f"ps{bo}", f"rs{i}"], [f"ab_{i}"])
                psb = self.PS[4 + i][:].bitcast(BF16)
                for k in range(8):
                    p.op("pe", lambda e, psb=psb, k=k, i=i: e.transpose(psb[:, k * 128:(k + 1) * 128],
                                                                       ab[i][:, k * 128:(k + 1) * 128], self.ident[:]),
                         [f"ab_{i}", "ident"], [f"ps{4 + i}"])
                CPY(p, "act", attnT[:, :, t0:t0 + 128], psb[:, 0:1024].rearrange("p (k t) -> p k t", k=8),
                    [f"ps{4 + i}"], ["attnT"])
                p.flush()
        if self.dbg == f"{l}D":
            p.barrier()
            for c in range(8):
                p.dma("sp", self.dbgA[c], attnT[:, c, :], ["attnT"], ["dbgA"])
            p.flush()

    def stage_E(self, l):
        p = self.p
        p.barrier()
        uactT, attnT = self.uactT, self.attnT
        with ExitStack() as st:
            wc = [self.sb(st, f"wc{i}", [128, 1024], BF16) for i in range(2)]
            wa = [self.sb(st, f"wa{i}", [128, 1024], BF16) for i in range(2)]
            gc = [self.sb(st, f"gc{i}", [128, T], BF16) for i in range(2)]
            ga = [self.sb(st, f"ga{i}", [128, T], BF16) for i in range(2)]
            mx = [self.sb(st, f"mx{i}", [128, T], BF16) for i in range(2)]
            t1 = [self.sb(st, f"e1{i}", [128, GS], F32) for i in range(2)]
            t2 = [self.sb(st, f"e2{i}", [128, GS], F32) for i in range(2)]
            for c in range(16):
                i = c % 2
                p.dma("pool", wc[i][:], self.w_co[l * 16 + c], [], [f"wc{i}"])
                p.dma("pool", wa[i][:], self.w_ao[l * 16 + c], [], [f"wa{i}"])
                p.dma("sp", gc[i][:], self.gT_s[c], ["projout"], [f"gc{i}"])
                p.dma("sp", ga[i][:], self.gT_s[16 + c], ["projout"], [f"ga{i}"])
                for g in range(NG):
                    sl = slice(g * GS, (g + 1) * GS)
                    b1, b2 = 2 * (g % 2), 2 * (g % 2) + 1
                    j = g % 2
                    for k in range(8):
                        MM(p, self.PS[b1][:, 0:GS], wc[i][:, k * 128:(k + 1) * 128], uactT[:, k, sl], k == 0, k == 7,
                           [f"wc{i}", "uactT"], [f"ps{b1}"])
                    for k in range(8):
                        MM(p, self.PS[b2][:, 0:GS], wa[i][:, k * 128:(k + 1) * 128], attnT[:, k, sl], k == 0, k == 7,
                           [f"wa{i}", "attnT"], [f"ps{b2}"])
                    TT(p, "dve", t1[j][:], self.PS[b1][:, 0:GS], gc[i][:, sl], ALU.mult, [f"ps{b1}", f"gc{i}"], [f"e1{j}"])
                    TT(p, "dve", t2[j][:], self.PS[b2][:, 0:GS], ga[i][:, sl], ALU.mult, [f"ps{b2}", f"ga{i}"], [f"e2{j}"])
                    TT(p, "pool", mx[i][:, sl], t1[j][:], t2[j][:], ALU.add, [f"e1{j}", f"e2{j}"], [f"mx{i}"])
                p.dma("sp", self.mixT_s[c], mx[i][:], [f"mx{i}"], ["mixout"])
                p.flush()
        self.lst.close()

    def stage_F(self, l):
        p = self.p
        p.barrier()
        with ExitStack() as st:
            mixT = self.sb(st, "mixT", [128, KD, T], BF16)
            wo = [self.sb(st, f"wo{i}", [128, KD * 512], BF16) for i in range(2)]
            g1 = [self.sb(st, f"g1{i}", [128, 512], F32) for i in range(2)]
            xt = [self.sb(st, f"fx{i}", [128, 512], F32) for i in range(3)]
            tm = [self.sb(st, f"ft{i}", [128, 512], F32) for i in range(2)]
            for k in range(KD):
                p.dma("sp" if k % 2 == 0 else "act", mixT[:, k, :], self.mixT_s[k], ["mixout"], ["mixT"])
            n = 0
            for nq in range(4):
                i = nq % 2
                p.dma("pool", wo[i][:], self.w_o[l * 4 + nq], [], [f"wo{i}"])
                self.load_bcast("sp", g1[i][:], self.modscr[l, 2 * D + nq * 512:2 * D + (nq + 1) * 512], f"g1{i}", ["modscr"])
                for t in range(NB):
                    bi = t % 4
                    xi = n % 3
                    n += 1
                    rows = slice(t * 128, (t + 1) * 128)
                    cols = slice(nq * 512, (nq + 1) * 512)
                    p.dma("sp", xt[xi][:], self.xsrc(l)[rows, cols], self.xkeys(t, nq), [f"fx{xi}"])
                    for k in range(KD):
                        MM(p, self.PS[bi][:], mixT[:, k, rows], wo[i][:, k * 512:(k + 1) * 512], k == 0, k == KD - 1,
                           ["mixT", f"wo{i}"], [f"ps{bi}"])
                    TT(p, "dve", tm[t % 2][:], self.PS[bi][:], g1[i][:], ALU.mult, [f"ps{bi}", f"g1{i}"], [f"ft{t % 2}"])
                    TT(p, "pool", xt[xi][:], tm[t % 2][:], xt[xi][:], ALU.add, [f"ft{t % 2}", f"fx{xi}"], [f"fx{xi}"])
                    p.dma("act", self.xres[rows, cols], xt[xi][:], [f"fx{xi}"], self.xkeys(t, nq))
                p.flush()

    def stage_G(self, l):
        p = self.p
        p.barrier()
        last = (l == L - 1)
        with ExitStack() as st:
            skb = self.sb(st, "skb", [128, 16, 128], BF16)
            p.dma("pool", skb[:], self.skT[l].rearrange("p (h n) -> p h n", h=16), [], ["skb"])
            h2T = self.sb(st, "h2T", [128, KD, TB * 128], BF16)
            E = self.sb(st, "E", [128, TB, 8, 2, 128], F32)
            pthr = self.sb(st, "pthr", [128, TB, 8], F32)
            acc = self.sb(st, "acc", [128, TB, D], F32)
            xb1 = self.sb(st, "gx0", [128, D], F32)
            xb = [xb1, xb1]
            Dh = self.sb(st, "Dh", [128, TB, 8, 128], BF16)
            hb1 = self.sb(st, "gh0", [128, D], BF16)
            hb = [hb1, hb1]
            sm = [self.sb(st, f"gsm{i}", [128, 4], F32) for i in range(2)]
            qp = [self.sb(st, f"qp{i}", [128, TB * 128], BF16) for i in range(2)]
            nm = [self.sb(st, f"nm{i}", [128, 4], F32) for i in range(2)]
            top = self.sb(st, "top", [128, 8, 2, 16], F32)
            tops = self.sb(st, "tops", [128, 8, 16], F32)
            etmp = self.sb(st, "etmp", [128, 128], F32)
            Pc = self.sb(st, "Pc", [128, 8, 16, 16], F32)
            pct = self.sb(st, "pct", [128, 16, 16], F32)
            best = self.sb(st, "best", [128, 8, 16], F32)
            Z = self.sb(st, "Z", [128, 16], F32)
            uball = self.sb(st, "uball", [128, 3, D], BF16)
            ub = [uball[:, i, :] for i in range(3)]
            vb = [[self.sb(st, f"vb{g}_{c}", [128, D], BF16) for c in range(EG)] for g in range(2)]
            gmv, shv, tmpv = acc[:, 1, :], acc[:, 2, :], acc[:, 0, :]
            g2v = Pc[:].rearrange("p h a b -> p (h a b)")
            fgv = uball[:, 0:2, :].rearrange("p a d -> p (a d)").bitcast(F32)
            gact = [self.sb(st, f"gact{i}", [128, TB * 128], F32) for i in range(3)]
            Pf = [self.sb(st, f"Pf{i}", [128, 8, 128], F32) for i in range(2)]
            Pfa = [self.sb(st, f"Pfa{i}", [128, 8, 128], F32) for i in range(2)]
            Mh = [self.sb(st, f"Mh{i}", [128, 8, 128], BF16) for i in range(2)]
            wT = [[self.sb(st, f"wT{i}_{c}", [128, TB * 128], BF16) for c in range(EG)] for i in range(2)]
            t_first = 2 if last else 1
            n_t = NB - t_first
            n_b = -(-n_t // TB)
            sizes = [n_t // n_b + (1 if i < n_t % n_b else 0) for i in range(n_b)]
            starts = [t_first + sum(sizes[:i]) for i in range(n_b)]
            for b0, ntl in zip(starts, sizes):
                nt = ntl * 128
                self.make_gm(gmv, shv, tmpv, "acc", l, self.n2g[l], 4, 3, "bc", kgm="acc", ksh="acc")
                for tl in range(ntl):
                    i = tl % 2
                    t = b0 + tl
                    p.dma("sp", xb[i][:], self.xres[t * 128:(t + 1) * 128, :], self.xkeys(t), ["gx0"])
                    self.norm_tile(xb[i][:], "gx0", gmv, shv, ["acc"], hb[i][:], "gh0", sm[i], f"gsm{i}",
                                   hb[i][:], "gh0", tmpv)
                    self.transpose_tile(hb[i], "gh0", lambda k0, tl=tl: h2T[:, k0:k0 + 8, tl * 128:(tl + 1) * 128],
                                        "h2T", 2 * i)
                if self.gstop == 5:
                    p.flush()
                    return
                for hp in range(self.g1hp):
                    i = hp % 2
                    p.dma("pool", ub[i][:], self.w_pq[l * 16 + hp], [], [f"ub{i}"])
                    bi = 4 + i
                    for k in range(KD):
                        MM(p, self.PS[bi][:, 0:nt], ub[i][:, k * 128:(k + 1) * 128], h2T[:, k, 0:nt], k == 0, k == KD - 1,
                           [f"ub{i}", "h2T"], [f"ps{bi}"])
                    CPY(p, "act", qp[i][:, 0:nt], self.PS[bi][:, 0:nt], [f"ps{bi}"], [f"qp{i}"])
                    bs = 6 + i
                    for tl in range(ntl if self.g1mode != 2 else 0):
                        if self.g1var == 1:
                            MM(p, self.PS[bs][:, tl * 128:(tl + 1) * 128], qp[i][:, tl * 128:(tl + 1) * 128], self.ident[:],
                               True, True, [f"qp{i}", "ident"], [f"ps{bs}"])
                        elif self.g1var == 2:
                            MM(p, self.PS[bs][:, tl * 128:(tl + 1) * 128], skb[:, hp, :], qp[i][:, tl * 128:(tl + 1) * 128],
                               True, True, [f"qp{i}", "skb"], [f"ps{bs}"])
                        elif self.g1var == 3:
                            MM(p, self.PS[2][:, tl * 128:(tl + 1) * 128], qp[i][:, tl * 128:(tl + 1) * 128], skb[:, hp, :],
                               True, True, [f"qp{i}", "skb"], [f"ps2"])
                        else:
                            MM(p, self.PS[bs][:, tl * 128:(tl + 1) * 128], qp[i][:, tl * 128:(tl + 1) * 128], skb[:, hp, :],
                               True, True, [f"qp{i}", "skb"], [f"ps{bs}"])
                    for tl in range(ntl if self.g1mode == 1 else 0):
                        pss = self.PS[bs][:, tl * 128:(tl + 1) * 128]
                        nmc = nm[i][:, tl:tl + 1]
                        p.op("dve", lambda e, pss=pss, nmc=nmc: e.tensor_reduce(out=nmc, in_=pss, axis=AX.X,
                                                                                op=ALU.max, negate=True),
                             [f"ps{bs}"], [f"nm{i}_{tl}"])
                        ACTV(p, E[:, tl, hp // 2, hp % 2, :], pss, AF.Exp, [f"ps{bs}", f"nm{i}_{tl}"], ["E"], bias=nmc)
                p.flush()
                if self.gstop == 1:
                    return
                for tl in range(ntl):
                    for hp in range(16):
                        h, pp = hp // 2, hp % 2
                        src = E[:, tl, h, pp, :]
                        p.op("dve", lambda e, src=src, h=h, pp=pp: e.max(out=top[:, h, pp, 0:8], in_=src), ["E"], ["top"])
                        p.op("dve", lambda e, src=src, h=h, pp=pp: e.match_replace(out=etmp[:], in_to_replace=top[:, h, pp, 0:8],
                                                                                   in_values=src, imm_value=-1.0),
                             ["E", "top"], ["etmp"])
                        p.op("dve", lambda e, h=h, pp=pp: e.max(out=top[:, h, pp, 8:16], in_=etmp[:]), ["etmp"], ["top"])

                    def top16_of_products(a_ap):
                        TT(p, "dve", Pc[:], a_ap.unsqueeze(3).to_broadcast([128, 8, 16, 16]),
                           top[:, :, 1, :].unsqueeze(2).to_broadcast([128, 8, 16, 16]), ALU.mult, ["top", "tops"], ["Pc"])
                        for h in range(8):
                            p.op("dve", lambda e, h=h: e.max(out=best[:, h, 0:8], in_=Pc[:, h]), ["Pc"], ["best"])
                            p.op("dve", lambda e, h=h: e.match_replace(out=pct[:], in_to_replace=best[:, h, 0:8],
                                                                       in_values=Pc[:, h], imm_value=-1.0),
                                 ["Pc", "best"], ["pct"])
                            p.op("dve", lambda e, h=h: e.max(out=best[:, h, 8:16], in_=pct[:]), ["pct"], ["best"])

                    top16_of_products(top[:, :, 0, :])
                    p.op("dve", lambda e: e.tensor_reduce(out=Z[:, 0:8], in_=best[:], axis=AX.X, op=ALU.add), ["best"], ["Z"])
                    p.op("dve", lambda e: e.reciprocal(out=Z[:, 8:16], in_=Z[:, 0:8]), ["Z"], ["Z"])
                    rz = Z[:, 8:16]
                    TT(p, "dve", E[:, tl, :, 0, :], E[:, tl, :, 0, :], rz.unsqueeze(2).to_broadcast([128, 8, 128]), ALU.mult,
                       ["E", "Z"], ["E"])
                    TT(p, "dve", tops[:], top[:, :, 0, :], rz.unsqueeze(2).to_broadcast([128, 8, 16]), ALU.mult,
                       ["top", "Z"], ["tops"])
                    top16_of_products(tops[:])
                    CPY(p, "dve", pthr[:, tl, :], best[:, :, 15], ["best"], ["pthr"])
                    p.op("dve", lambda e, tl=tl: e.reciprocal(out=Z[:, 0:8], in_=pthr[:, tl, :]), ["pthr"], ["Z"])
                    TT(p, "dve", E[:, tl, :, 0, :], E[:, tl, :, 0, :], Z[:, 0:8].unsqueeze(2).to_broadcast([128, 8, 128]),
                       ALU.mult, ["E", "Z"], ["E"])
                    TT(p, "dve", Dh[:, tl], self.ident[:].unsqueeze(1).to_broadcast([128, 8, 128]),
                       pthr[:, tl, :].unsqueeze(2).to_broadcast([128, 8, 128]), ALU.mult, ["ident", "pthr"], ["Dh"])
                p.flush()
                if self.gstop == 2:
                    return
                n_ch = (self.geg * EG) if self.gstop else NCH
                THR = 1.0 - 1e-6
                cnt = {"f": 0}

                def emit_uload(c):
                    p.dma("pool", ub[c % 3][:], self.uT[l * NCH + c], [], [f"ub{c % 3}"])

                def emit_act(c):
                    ui, gu = c % 2, c % 3
                    for k in range(KD):
                        MM(p, self.PS[ui][:, 0:nt], ub[gu][:, k * 128:(k + 1) * 128], h2T[:, k, 0:nt], k == 0, k == KD - 1,
                           [f"ub{gu}", "h2T"], [f"ps{ui}"])
                    ACTV(p, gact[gu][:, 0:nt], self.PS[ui][:, 0:nt], AF.Gelu, [f"ps{ui}"], [f"gact{gu}"])

                def emit_masks(c):
                    bc = 2 + c % 2
                    for tl in range(ntl):
                        fi = cnt["f"] % 2
                        cnt["f"] += 1
                        if tl == 2:
                            src, sk = Pfa[c % 2], f"Pfa{c % 2}"
                        else:
                            src, sk = Pf[fi], f"Pf{fi}"
                            TT(p, "pool", Pf[fi][:], E[:, tl, :, 1, :], E[:, tl, :, 0, c:c + 1].to_broadcast([128, 8, 128]),
                               ALU.mult, ["E"], [f"Pf{fi}"])
                        STT(p, Mh[fi][:], src[:], THR, src[:], ALU.is_ge, ALU.mult, [sk], [f"Mh{fi}"])
                        for h in range(8):
                            MM(p, self.PS[bc][:, tl * 128:(tl + 1) * 128], Mh[fi][:, h, :], Dh[:, tl, h, :], h == 0, h == 7,
                               [f"Mh{fi}", "Dh"], [f"ps{bc}"])

                def emit_pf_act(c):
                    if ntl < 3:
                        return
                    for h in range(8):
                        ACTV(p, Pfa[c % 2][:, h, :], E[:, 2, h, 1, :], AF.Copy, ["E"], [f"Pfa{c % 2}"],
                             scale=E[:, 2, h, 0, c:c + 1])

                def emit_wT(c):
                    eg, cc = divmod(c, EG)
                    gi, gu, bc = eg % 2, c % 3, 2 + c % 2
                    TT(p, "dve", wT[gi][cc][:, 0:nt], self.PS[bc][:, 0:nt], gact[gu][:, 0:nt], ALU.mult,
                       [f"ps{bc}", f"gact{gu}"], [f"wT{gi}_{cc}"])

                def emit_vload(eg):
                    for cc in range(EG):
                        p.dma("pool", vb[eg % 2][cc][:], self.vv[l * NCH + eg * EG + cc], [], [f"vb{eg % 2}_{cc}"])

                def emit_out_tile(eg, tl):
                    gi = eg % 2
                    for dq in range(4):
                        bo = 4 + dq
                        for cc in range(EG):
                            MM(p, self.PS[bo][:], wT[gi][cc][:, tl * 128:(tl + 1) * 128], vb[gi][cc][:, dq * 512:(dq + 1) * 512],
                               cc == 0, cc == EG - 1, [f"wT{gi}_{cc}", f"vb{gi}_{cc}"], [f"ps{bo}"])
                        dst = acc[:, tl, dq * 512:(dq + 1) * 512]
                        if eg == 0:
                            CPY(p, "act", dst, self.PS[bo][:], [f"ps{bo}"], ["acc"])
                        else:
                            TT(p, "dve", dst, self.PS[bo][:], dst, ALU.add, [f"ps{bo}", "acc"], ["acc"])

                n_g = n_ch // EG
                emit_uload(0)
                emit_uload(1)
                emit_vload(0)
                if n_g > 1:
                    emit_vload(1)
                emit_pf_act(0)
                emit_act(0)
                for c in range(n_ch):
                    eg, cc = divmod(c, EG)
                    if c + 2 < n_ch:
                        emit_uload(c + 2)
                    if c + 1 < n_ch:
                        emit_pf_act(c + 1)
                        emit_act(c + 1)
                    emit_masks(c)
                    if c >= 1:
                        emit_wT(c - 1)
                    if eg >= 1 and cc < ntl:
                        emit_out_tile(eg - 1, cc)
                    if cc == EG - 1 and eg >= 1 and eg + 1 < n_g:
                        emit_vload(eg + 1)
                    if cc == EG - 1:
                        p.flush()
                emit_wT(n_ch - 1)
                for tl in range(ntl):
                    emit_out_tile(n_g - 1, tl)
                p.flush()
                if self.gstop == 3:
                    return
                self.load_bcast("sp", g2v, self.mod_row(l, 5), "Pc", ["modscr"])
                if last:
                    self.load_bcast("sp", fgv, self.fing[0], ["ub0", "ub1"])
                for tl in range(ntl):
                    i = tl % 2
                    t = b0 + tl
                    rows = slice(t * 128, (t + 1) * 128)
                    p.dma("sp", xb[i][:], self.xres[rows, :], self.xkeys(t), ["gx0"])
                    TT(p, "dve", acc[:, tl, :], acc[:, tl, :], g2v, ALU.mult, ["acc", "Pc"], ["acc"])
                    TT(p, "pool", xb[i][:], xb[i][:], acc[:, tl, :], ALU.add, ["gx0", "acc"], ["gx0"])
                    if not last or self.dbg:
                        p.dma("act", self.xres[rows, :], xb[i][:], ["gx0"], self.xkeys(t))
                    if last and t >= 2:
                        self.norm_tile(xb[i][:], "gx0", fgv, None, ["ub0", "ub1"], acc[:, tl, :], "acc", sm[i], f"gsm{i}",
                                       hb[i][:], "gh0", None)
                        p.dma("act", self.y[(t - 2) * 128:(t - 1) * 128, :], acc[:, tl, :], ["acc"], ["y"])
                p.flush()

    def stage_A(self, l):
        self.p.barrier()
        self.lst = ExitStack()


def _t5_bucket(dist):
    max_exact = 16
    d = np.maximum(dist, 0)
    large = max_exact + (np.log(np.maximum(d, 1).astype(np.float32) / max_exact)
                         / np.float32(np.log(128 / max_exact)) * (32 - max_exact)).astype(np.int32)
    large = np.minimum(large, 31)
    return np.where(d < max_exact, d, large)


def _chunk_cols(w, ncols):
    Lx, Kd, N = w.shape
    a = w.reshape(Lx, Kd // 128, 128, N // ncols, ncols).transpose(0, 3, 2, 1, 4)
    return np.ascontiguousarray(a).reshape(Lx * (N // ncols), 128, (Kd // 128) * ncols)


_CACHE = {}


def prepare_shared(inp):
    f = lambda a: np.asarray(a, dtype=np.float32)
    w_in = f(inp["w_in"])
    sh = {}
    sh["ada_w"] = _chunk_cols(f(inp["ada_w"]), 512)
    sh["ada_b"] = f(inp["ada_b"])
    sh["n1g"] = f(inp["norm1_g"])
    sh["n2g"] = f(inp["norm2_g"])
    sh["fing"] = f(inp["final_g"]).reshape(1, D)
    sh["w_in_c"] = _chunk_cols(w_in, 128)
    kcols = w_in[:, :, 3072:3328].reshape(L, D, 4, 64)
    kd = np.concatenate([kcols, kcols], axis=3).reshape(L, D, 512)
    sh["w_kd"] = _chunk_cols(kd, 128)
    sh["w_v"] = _chunk_cols(np.ascontiguousarray(w_in[:, :, 3328:3584]), 256)
    sh["w_co"] = _chunk_cols(f(inp["w_conv_out"]), 128)
    sh["w_ao"] = _chunk_cols(f(inp["w_attn_out"]), 128)
    sh["w_o"] = _chunk_cols(f(inp["w_out"]), 512)
    sh["w_pq"] = _chunk_cols(f(inp["w_pq"]), 128)
    sk = f(inp["sub_keys"])
    sh["skT"] = np.ascontiguousarray(sk.transpose(0, 4, 1, 2, 3)).reshape(L, 128, D)
    pu = f(inp["peer_u"])
    sh["uT"] = np.ascontiguousarray(pu.reshape(L, NCH, 128, KD, 128).transpose(0, 1, 4, 3, 2)).reshape(L * NCH, 128, D)
    sh["vv"] = f(inp["peer_v"]).reshape(L * NCH, 128, D)
    cp = np.concatenate([f(inp["dw_w"]).transpose(0, 2, 1), f(inp["dw_b"])[:, :, None],
                         f(inp["conv_ln_g"])[:, :, None], f(inp["conv_ln_b"])[:, :, None]], axis=2)
    sh["chanpar"] = np.ascontiguousarray(cp.reshape(L, 8, 128, 34).transpose(0, 2, 1, 3)).reshape(L, 128, 8 * 34)
    sh["sinks"] = f(inp["attn_sinks"])
    qi = np.arange(128)[:, None] + 128
    kj = np.arange(256)[None, :]
    dist = qi - kj
    bucket = _t5_bucket(dist)
    rb = f(inp["rel_bias"])
    tab = rb[bucket]
    band = (dist >= 0) & (dist < 128)
    tab = np.where(band[:, :, None], tab, np.float32(NEG))
    sh["biasT"] = np.ascontiguousarray(tab.transpose(0, 2, 1)).reshape(128, 16 * 256)
    return sh


def per_core(inp, c):
    b, qtr = c // 4, c % 4
    s0 = qtr * 2048
    x = np.asarray(inp["x"], dtype=np.float32)
    xw = np.zeros((T, D), np.float32)
    if qtr == 0:
        xw[256:] = x[b, 0:2048]
    else:
        xw[:] = x[b, s0 - 256:s0 + 2048]
    cc = np.asarray(inp["c"], dtype=np.float32)[b]
    flags = np.zeros((128, 2), np.float32)
    flags[:, 0] = 0.0 if qtr == 0 else 1.0
    flags[:, 1] = NEG if qtr == 0 else 0.0
    return {"x_in": xw, "c_in": np.ascontiguousarray(cc.reshape(KD, 128).T), "flags": flags}


def kernel(**inputs):
    if "nc" not in _CACHE:
        _CACHE["nc"] = K().build()
    nc = _CACHE["nc"]
    sh = prepare_shared(inputs)
    in_maps = []
    for c in range(8):
        m = dict(sh)
        m.update(per_core(inputs, c))
        in_maps.append(m)
    res = run_bass_kernel_spmd(nc, in_maps, core_ids=list(range(8)))
    out = np.zeros((2, SEQ, D), np.float32)
    for c in range(8):
        b, qtr = c // 4, c % 4
        out[b, qtr * 2048:(qtr + 1) * 2048] = res.results[c]["y"]
    return out
```

```python
import numpy as np
from contextlib import ExitStack
import concourse.bass as bass
import concourse.mybir as mybir
from concourse.bass_utils import run_bass_kernel_spmd

F32 = mybir.dt.float32
BF16 = mybir.dt.bfloat16
AF = mybir.ActivationFunctionType
ALU = mybir.AluOpType
AX = mybir.AxisListType

L = 2
D = 2048
KD = 16
NB = 18
T = NB * 128
GS = 384
NG = T // GS
SEQ = 8192
NEG = -30000.0
EPS = 1e-6
NCH = 128
TB = 3
EG = 4

ENGS = ["pe", "act", "dve", "pool", "sp"]
ENGMAP = {"pe": "tensor", "act": "scalar", "dve": "vector", "pool": "gpsimd", "sp": "sync"}
EPOCH = 1 << 30
NDMASEM = 6


class Prog:
    def __init__(self, nc, stack, block):
        self.nc, self.stack, self.block = nc, stack, block
        self.q = {e: [] for e in ENGS}
        self.cnt = {e: 0 for e in ENGS}
        self.known = {e: {} for e in ENGS}
        self.last_w, self.readers = {}, {}
        self.nsem = 0
        self.sems = {e: self._newsem(e) for e in ENGS}
        self.dsem = {qn: [[self._newsem("d" + qn), 0] for _ in range(NDMASEM)] for qn in ["sp", "act", "pool"]}
        self.dsem_i = {qn: 0 for qn in self.dsem}
        self.n_instr = 0

    def _newsem(self, name):
        self.nsem += 1
        return self.stack.enter_context(self.nc.semaphore(f"s{self.nsem}_{name}"))

    def _deps(self, eng, reads, writes):
        deps = []
        for k in reads:
            deps.extend(self.last_w.get(k, {}).values())
        for k in writes:
            for ev in self.last_w.get(k, {}).values():
                if ev[2] != eng:
                    deps.append(ev)
            for ev in self.readers.get(k, {}).values():
                if ev[2] != eng:
                    deps.append(ev)
        waits = {}
        kn = self.known[eng]
        for (sem, val, src) in deps:
            sid = id(sem)
            if kn.get(sid, 0) >= val:
                continue
            if sid not in waits or waits[sid][1] < val:
                waits[sid] = (sem, val)
        for sid, (sem, val) in waits.items():
            kn[sid] = val
        return list(waits.values())

    def _commit(self, ev, reads, writes):
        sid = id(ev[0])
        for k in writes:
            self.last_w.setdefault(k, {})[sid] = ev
        for k in reads:
            self.readers.setdefault(k, {})[sid] = ev

    def op(self, eng, fn, reads=(), writes=()):
        psr = [k for k in reads if k.startswith("ps") and k[2:].isdigit()]
        if psr:
            writes = list(writes) + psr
        waits = self._deps(eng, reads, writes)
        if self.cnt[eng] >= EPOCH:
            self.sems[eng] = self._newsem(eng)
            self.cnt[eng] = 0
        self.cnt[eng] += 1
        sem = self.sems[eng]
        ev = (sem, self.cnt[eng], eng)
        self.q[eng].append((waits, fn, sem, 1))
        self._commit(ev, reads, writes)
        self.n_instr += 1

    def dma(self, qn, out, in_, reads=(), writes=(), **kw):
        slot = self.dsem[qn][self.dsem_i[qn] % NDMASEM]
        self.dsem_i[qn] += 1
        sem, cnt = slot
        waits = self._deps(qn, reads, writes)
        if cnt > 0 and self.known[qn].get(id(sem), 0) < cnt:
            waits.append((sem, cnt))
            self.known[qn][id(sem)] = cnt
        slot[1] = cnt + 16
        ev = (sem, cnt + 16, "dma_" + qn)
        self.q[qn].append((waits, lambda e: e.dma_start(out=out, in_=in_, **kw), sem, 16))
        self._commit(ev, reads, writes)
        self.n_instr += 1

    def barrier(self):
        evs = [(self.sems[e], self.cnt[e]) for e in ENGS if self.cnt[e] > 0]
        for qn in self.dsem:
            evs += [(s, c) for (s, c) in self.dsem[qn] if c > 0]
        for e in ENGS:
            waits = []
            for (s, v) in evs:
                if s is self.sems[e]:
                    continue
                if self.known[e].get(id(s), 0) < v:
                    waits.append((s, v))
                    self.known[e][id(s)] = v
            if waits:
                self.q[e].append((waits, None, None, 0))

    def flush(self):
        for e in ENGS:
            items, self.q[e] = self.q[e], []
            if not items:
                continue

            def body(engobj, items=items):
                for (waits, fn, sem, inc) in items:
                    for (s, v) in waits:
                        engobj.wait_ge(s, v)
                    if fn is not None:
                        fn(engobj).then_inc(sem, inc)

            getattr(self.block, ENGMAP[e])(body)


def MM(p, out, lhsT, rhs, st, sp, r, w):
    p.op("pe", lambda e: e.matmul(out, lhsT=lhsT, rhs=rhs, start=st, stop=sp), r, w)


def ACTV(p, out, in_, func, r, w, bias=None, scale=None, accum=None):
    kw = {}
    if bias is not None:
        kw["bias"] = bias
    if scale is not None:
        kw["scale"] = scale
    if accum is not None:
        kw["accum_out"] = accum
    p.op("act", lambda e: e.activation(out=out, in_=in_, func=func, **kw), r, w)


def TT(p, eng, out, a, b, op, r, w):
    p.op(eng, lambda e: e.tensor_tensor(out=out, in0=a, in1=b, op=op), r, w)


def TS(p, eng, out, a, s1, op0, r, w, s2=None, op1=None):
    if op1 is None:
        p.op(eng, lambda e: e.tensor_scalar(out=out, in0=a, scalar1=s1, scalar2=None, op0=op0), r, w)
    else:
        p.op(eng, lambda e: e.tensor_scalar(out=out, in0=a, scalar1=s1, scalar2=s2, op0=op0, op1=op1), r, w)


def STT(p, out, a, s, b, op0, op1, r, w):
    p.op("dve", lambda e: e.scalar_tensor_tensor(out=out, in0=a, scalar=s, in1=b, op0=op0, op1=op1), r, w)


def CPY(p, eng, out, in_, r, w):
    if eng == "act":
        p.op("act", lambda e: e.activation(out=out, in_=in_, func=AF.Copy), r, w)
    else:
        p.op(eng, lambda e: e.tensor_copy(out=out, in_=in_), r, w)


class K:
    def __init__(self, dbg=None, nlayers=L):
        self.dbg = dbg
        self.nlayers = nlayers
        self.gstop = 0
        self.geg = 1
        self.in_shapes = {}
        self.only_g = False
        self.g1hp = 16
        self.g1mode = 1
        self.g1var = 0
        self.pipe_out = False
        nc = self.nc = bass.Bass("TRN2", target_bir_lowering=False)

        def din(name, shape, dt=F32):
            self.in_shapes[name] = list(shape)
            return nc.dram_tensor(name, list(shape), dt, kind="ExternalInput").ap()

        def dscr(name, shape, dt):
            kind = "ExternalOutput" if dbg else "Internal"
            return nc.dram_tensor(name, list(shape), dt, kind=kind).ap()

        self.x_in = din("x_in", [T, D])
        self.c_in = din("c_in", [128, KD])
        self.flags = din("flags", [128, 2])
        self.biasT = din("biasT", [128, 16 * 256])
        self.ada_w = din("ada_w", [L * 24, 128, KD * 512])
        self.ada_b = din("ada_b", [L, 6 * D])
        self.n1g = din("n1g", [L, D])
        self.n2g = din("n2g", [L, D])
        self.fing = din("fing", [1, D])
        self.w_in_c = din("w_in_c", [L * 60, 128, D])
        self.w_kd = din("w_kd", [L * 4, 128, D])
        self.w_v = din("w_v", [L, 128, KD * 256])
        self.w_co = din("w_co", [L * 16, 128, 1024])
        self.w_ao = din("w_ao", [L * 16, 128, 1024])
        self.w_o = din("w_o", [L * 4, 128, KD * 512])
        self.w_pq = din("w_pq", [L * 16, 128, D])
        self.skT = din("skT", [L, 128, D])
        self.uT = din("uT", [L * NCH, 128, D])
        self.vv = din("vv", [L * NCH, 128, D])
        self.chanpar = din("chanpar", [L, 128, 8 * 34])
        self.sinks = din("sinks", [L, 16])
        self.y = nc.dram_tensor("y", [16 * 128, D], F32, kind="ExternalOutput").ap()

        self.xres = dscr("xres", [T, D], F32)
        self.modscr = dscr("modscr", [L, 6 * D], F32)
        self.qT_s = dscr("qT_s", [8, 128, T], BF16)
        self.kdT_s = dscr("kdT_s", [4, 128, T], BF16)
        self.v_s = dscr("v_s", [T, 256], BF16)
        self.gT_s = dscr("gT_s", [32, 128, T], BF16)
        self.mixT_s = dscr("mixT_s", [16, 128, T], BF16)
        if dbg:
            self.dbgA = dscr("dbgA", [16, 128, T], BF16)
            self.dbgB = dscr("dbgB", [16, 128, T], BF16)

    def build(self):
        nc = self.nc
        with ExitStack() as st, nc.Block() as block:
            p = self.p = Prog(nc, st, block)
            self.PS = [st.enter_context(nc.psum_tensor(f"ps{i}", [128, 512], F32)) for i in range(8)]
            self.ident = st.enter_context(nc.sbuf_tensor("ident", [128, 128], BF16))
            self.ones = st.enter_context(nc.sbuf_tensor("ones", [128, 128], BF16))
            self.flg = st.enter_context(nc.sbuf_tensor("flg", [128, 2], F32))
            ident, ones = self.ident, self.ones
            p.op("pool", lambda e: e.memset(ident[:], 0.0), [], ["ident"])
            p.op("pool", lambda e: e.affine_select(out=ident[:], in_=ident[:], pattern=[[-1, 128]],
                                                   compare_op=ALU.not_equal, fill=1.0, base=0,
                                                   channel_multiplier=1), ["ident"], ["ident"])
            p.op("pool", lambda e: e.memset(ones[:], 1.0), [], ["ones"])
            p.dma("sp", self.flg[:], self.flags, [], ["flg"])
            if self.only_g:
                self.stage_G(0)
                p.barrier()
                p.flush()
                return nc
            self.stage_mods()
            done = False
            for l in range(self.nlayers):
                for nm in ["A", "B", "C", "D", "E", "F", "G"]:
                    getattr(self, "stage_" + nm)(l)
                    if self.dbg == f"{l}{nm}":
                        done = True
                        break
                if done:
                    break
            p.barrier()
            p.flush()
            if getattr(self, "lst", None) is not None:
                self.lst.close()
        return nc

    def stage_ctx(self):
        self.p.barrier()
        return ExitStack()

    def sb(self, st, name, shape, dt):
        self.uid = getattr(self, "uid", 0) + 1
        return st.enter_context(self.nc.sbuf_tensor(f"{name}_u{self.uid}", list(shape), dt))

    def stage_mods(self):
        p = self.p
        with self.stage_ctx() as st:
            cs = self.sb(st, "cs", [128, KD], F32)
            wb = [self.sb(st, f"adab{i}", [128, KD * 512], F32) for i in range(2)]
            ab = [self.sb(st, f"ab{i}", [1, 512], F32) for i in range(2)]
            mr = [self.sb(st, f"mr{i}", [1, 512], F32) for i in range(2)]
            p.dma("sp", cs[:], self.c_in, [], ["cs"])
            ACTV(p, cs[:], cs[:], AF.Silu, ["cs"], ["cs"])
            for l in range(self.nlayers):
                for n in range(24):
                    i = n % 2
                    p.dma("sp" if i == 0 else "act", wb[i][:], self.ada_w[l * 24 + n], [], [f"adab{i}"])
                    p.dma("sp", ab[i][:], self.ada_b[l:l + 1, n * 512:(n + 1) * 512], [], [f"ab{i}"])
                    ps = self.PS[i]
                    for k in range(KD):
                        MM(p, ps[0:1, :], cs[:, k:k + 1], wb[i][:, k * 512:(k + 1) * 512], k == 0, k == KD - 1,
                           ["cs", f"adab{i}"], [f"ps{i}"])
                    TT(p, "dve", mr[i][:], ps[0:1, :], ab[i][:], ALU.add, [f"ps{i}", f"ab{i}"], [f"mr{i}"])
                    p.dma("sp", self.modscr[l:l + 1, n * 512:(n + 1) * 512], mr[i][:], [f"mr{i}"], ["modscr"])
            p.flush()

    def load_bcast(self, q, dst, src_row, key, reads=()):
        keys = key if isinstance(key, list) else [key]
        self.p.dma(q, dst, src_row.partition_broadcast(128), list(reads), keys)

    def mod_row(self, l, i):
        return self.modscr[l, i * D:(i + 1) * D]

    def make_gm(self, gm, sh, tmp, tk, l, gain_row, sc_i, sh_i, pfx, kgm=None, ksh=None):
        p = self.p
        kgm = kgm or pfx + "gm"
        ksh = ksh or pfx + "sh"
        self.load_bcast("sp", gm[:], gain_row, kgm)
        self.load_bcast("sp", tmp[:], self.mod_row(l, sc_i), tk, ["modscr"])
        self.load_bcast("sp", sh[:], self.mod_row(l, sh_i), ksh, ["modscr"])
        STT(p, gm[:], tmp[:], 1.0, gm[:], ALU.add, ALU.mult, [tk, kgm], [kgm])

    def norm_tile(self, xt, xk, gm, sh, gk, hb, hk, sm, smk, junk, jk, tmp):
        p = self.p
        ACTV(p, junk, xt, AF.Square, [xk], [jk, smk + "ss"], accum=sm[:, 0:1])
        TS(p, "dve", sm[:, 1:2], sm[:, 0:1], 1.0 / D, ALU.mult, [smk + "ss"], [smk + "a"], s2=EPS, op1=ALU.add)
        ACTV(p, sm[:, 2:3], sm[:, 1:2], AF.Sqrt, [smk + "a"], [smk + "b"])
        p.op("dve", lambda e: e.reciprocal(out=sm[:, 3:4], in_=sm[:, 2:3]), [smk + "b"], [smk + "r"])
        if sh is None:
            STT(p, hb, xt, sm[:, 3:4], gm[:], ALU.mult, ALU.mult, [xk, smk + "r"] + gk, [hk])
        else:
            STT(p, tmp, xt, sm[:, 3:4], gm[:], ALU.mult, ALU.mult, [xk, smk + "r"] + gk, ["ntmp"])
            TT(p, "dve", hb, tmp, sh[:], ALU.add, ["ntmp"] + gk, [hk])

    def transpose_tile(self, hb, hk, dst_fn, dk, bank0):
        p = self.p
        for half in range(2):
            bi = bank0 + half
            psb = self.PS[bi][:].bitcast(BF16)
            for j in range(8):
                k = half * 8 + j
                p.op("pe", lambda e, j=j, k=k, psb=psb: e.transpose(psb[:, j * 128:(j + 1) * 128],
                                                                   hb[:, k * 128:(k + 1) * 128], self.ident[:]),
                     [hk, "ident"], [f"ps{bi}"])
            src = psb[:, 0:1024].rearrange("p (j t) -> p j t", j=8)
            CPY(p, "act" if half == 0 else "dve", dst_fn(half * 8), src, [f"ps{bi}"], [dk])

    def xsrc(self, l):
        return self.x_in if l == 0 else self.xres

    def xkeys(self, t, nq=None):
        return [f"xr{t}_{q}" for q in (range(4) if nq is None else [nq])]

    def stage_B(self, l):
        p = self.p
        self.convT = convT = self.sb(self.lst, "convT", [128, 8, T], BF16)
        with self.stage_ctx() as st:
            hT = self.sb(st, "hT", [128, KD, T], BF16)
            with ExitStack() as st2:
                gm = self.sb(st2, "n1gm", [128, D], F32)
                sh = self.sb(st2, "n1sh", [128, D], F32)
                tmp = self.sb(st2, "ntmp", [128, D], F32)
                self.make_gm(gm, sh, tmp, "ntmp", l, self.n1g[l], 1, 0, "n1")
                xb = [self.sb(st2, f"xb{i}", [128, D], F32) for i in range(2)]
                hb = [self.sb(st2, f"hb{i}", [128, D], BF16) for i in range(2)]
                sm = [self.sb(st2, f"sm{i}", [128, 4], F32) for i in range(2)]
                for t in range(NB):
                    i = t % 2
                    p.dma("sp", xb[i][:], self.xsrc(l)[t * 128:(t + 1) * 128, :], self.xkeys(t), [f"xb{i}"])
                    self.norm_tile(xb[i][:], f"xb{i}", gm, sh, ["n1gm", "n1sh"], hb[i][:], f"hb{i}", sm[i], f"sm{i}",
                                   hb[i][:], f"hb{i}", tmp[:])
                    self.transpose_tile(hb[i], f"hb{i}", lambda k0, t=t: hT[:, k0:k0 + 8, t * 128:(t + 1) * 128],
                                        f"hT{t // 3}", 2 * i)
                p.flush()
            p.barrier()
            if self.dbg == f"{l}A":
                for k in range(KD):
                    p.dma("sp", self.dbgA[k], hT[:, k, :], [f"hT{g}" for g in range(NG)], ["dbgA"])
                p.flush()
                return
            wt = [self.sb(st, f"wt{i}", [128, D], BF16) for i in range(3)]
            stg = [self.sb(st, f"stg{i}", [128, GS], BF16) for i in range(3)]
            sig = [self.sb(st, f"sig{i}", [128, GS], F32) for i in range(2)]
            upad = [self.sb(st, f"upad{i}", [128, 32 + T], F32) for i in range(2)]
            cacc = self.sb(st, "cacc", [128, T], F32)
            cp = self.sb(st, "cp", [128, 8, 34], F32)
            wv = self.sb(st, "wv", [128, KD * 256], BF16)
            vst = [self.sb(st, f"vst{i}", [128, 256], BF16) for i in range(2)]
            p.dma("sp", cp[:], self.chanpar[l].rearrange("p (c t) -> p c t", c=8), [], ["cp"])
            for i in range(2):
                p.op("pool", lambda e, i=i: e.memset(upad[i][:], 0.0), [], [f"upad{i}"])
            state = {"w": 0, "ps": 0, "stg": 0}

            def proj(wsrc, evac):
                wi = state["w"] % 3
                state["w"] += 1
                p.dma("pool", wt[wi][:], wsrc, [], [f"wt{wi}"])
                for g in range(NG):
                    bi = state["ps"] % 4
                    state["ps"] += 1
                    ps = self.PS[bi]
                    for k in range(KD):
                        MM(p, ps[:, 0:GS], wt[wi][:, k * 128:(k + 1) * 128], hT[:, k, g * GS:(g + 1) * GS],
                           k == 0, k == KD - 1, [f"wt{wi}", f"hT{g}"], [f"ps{bi}"])
                    evac(g, ps[:, 0:GS], f"ps{bi}")

            def evac_store(dst, func):
                def f(g, ps, pk):
                    si = state["stg"] % 3
                    state["stg"] += 1
                    ACTV(p, stg[si][:], ps, func, [pk], [f"stg{si}"])
                    p.dma("sp", dst[:, g * GS:(g + 1) * GS], stg[si][:], [f"stg{si}"], ["projout"])
                return f

            jobs = []
            for c in range(8):
                jobs.append((self.w_in_c[l * 60 + 16 + c], evac_store(self.qT_s[c], AF.Copy)))
            for c in range(4):
                jobs.append((self.w_kd[l * 4 + c], evac_store(self.kdT_s[c], AF.Copy)))
            for c in range(32):
                jobs.append((self.w_in_c[l * 60 + 28 + c], evac_store(self.gT_s[c], AF.Sigmoid)))
            p.dma("pool", wv[:], self.w_v[l], [], ["wv"])
            for t in range(NB):
                bi = 4 + (t % 2)
                ps = self.PS[bi]
                for k in range(KD):
                    MM(p, ps[:, 0:256], hT[:, k, t * 128:(t + 1) * 128], wv[:, k * 256:(k + 1) * 256],
                       k == 0, k == KD - 1, ["wv", f"hT{t // 3}"], [f"ps{bi}"])
                CPY(p, "act", vst[t % 2][:], ps[:, 0:256], [f"ps{bi}"], [f"vst{t % 2}"])
                p.dma("sp", self.v_s[t * 128:(t + 1) * 128, :], vst[t % 2][:], [f"vst{t % 2}"], ["projout"])
            p.flush()
            for c in range(8):
                ui = c % 2
                up = upad[ui]
                wa = state["w"] % 3
                state["w"] += 1
                wbi = state["w"] % 3
                state["w"] += 1
                p.dma("pool", wt[wa][:], self.w_in_c[l * 60 + c], [], [f"wt{wa}"])
                p.dma("pool", wt[wbi][:], self.w_in_c[l * 60 + 8 + c], [], [f"wt{wbi}"])
                for g in range(NG):
                    ba = (2 * g) % 4
                    bb = (2 * g + 1) % 4
                    for k in range(KD):
                        MM(p, self.PS[ba][:, 0:GS], wt[wa][:, k * 128:(k + 1) * 128], hT[:, k, g * GS:(g + 1) * GS],
                           k == 0, k == KD - 1, [f"wt{wa}", f"hT{g}"], [f"ps{ba}"])
                    for k in range(KD):
                        MM(p, self.PS[bb][:, 0:GS], wt[wbi][:, k * 128:(k + 1) * 128], hT[:, k, g * GS:(g + 1) * GS],
                           k == 0, k == KD - 1, [f"wt{wbi}", f"hT{g}"], [f"ps{bb}"])
                    ACTV(p, sig[g % 2][:], self.PS[bb][:, 0:GS], AF.Sigmoid, [f"ps{bb}"], [f"sig{g % 2}"])
                    TT(p, "dve", up[:, 32 + g * GS:32 + (g + 1) * GS], self.PS[ba][:, 0:GS], sig[g % 2][:], ALU.mult,
                       [f"ps{ba}", f"sig{g % 2}"], [f"upad{ui}"])
                TS(p, "dve", up[:, 32:32 + 256], up[:, 32:32 + 256], self.flg[:, 0:1], ALU.mult,
                   [f"upad{ui}", "flg"], [f"upad{ui}"])
                TS(p, "dve", cacc[:], up[:, 2:2 + T], cp[:, c, 0:1], ALU.mult, [f"upad{ui}", "cp"], ["cacc"],
                   s2=cp[:, c, 31:32], op1=ALU.add)
                for j in range(1, 31):
                    dst = cacc[:] if j < 30 else convT[:, c, :]
                    STT(p, dst, up[:, 2 + j:2 + j + T], cp[:, c, j:j + 1], cacc[:], ALU.mult, ALU.add,
                        [f"upad{ui}", "cp", "cacc"], ["cacc"] if j < 30 else ["convT"])
                for _ in range(6 if c < 7 else len(jobs)):
                    if jobs:
                        proj(*jobs.pop(0))
                p.flush()

    def stage_C(self, l):
        p = self.p
        convT = self.convT
        self.p.barrier()
        self.uactT = uactT = self.sb(self.lst, "uactT", [128, 8, T], BF16)
        with ExitStack() as st2:
            cp = self.sb(st2, "cp2", [128, 8, 34], F32)
            sq = [self.sb(st2, f"sq{i}", [128, GS], BF16) for i in range(2)]
            mean = self.sb(st2, "mean", [128, GS], F32)
            m2 = self.sb(st2, "m2", [128, GS], F32)
            var = self.sb(st2, "var", [128, GS], F32)
            rstd = self.sb(st2, "rstd", [128, GS], F32)
            t1 = [self.sb(st2, f"t1{i}", [128, GS], F32) for i in range(2)]
            t2 = [self.sb(st2, f"t2{i}", [128, GS], F32) for i in range(2)]
            p.dma("sp", cp[:], self.chanpar[l].rearrange("p (c t) -> p c t", c=8), [], ["cp2"])
            for g in range(NG):
                sl = slice(g * GS, (g + 1) * GS)
                b1, b2 = 2 * (g % 2), 2 * (g % 2) + 1
                for c in range(8):
                    MM(p, self.PS[b1][:, 0:GS], self.ones[:], convT[:, c, sl], c == 0, c == 7, ["ones", "convT"],
                       [f"ps{b1}"])
                for c in range(8):
                    ACTV(p, sq[c % 2][:], convT[:, c, sl], AF.Square, ["convT"], [f"sq{c % 2}"])
                    MM(p, self.PS[b2][:, 0:GS], self.ones[:], sq[c % 2][:], c == 0, c == 7, ["ones", f"sq{c % 2}"],
                       [f"ps{b2}"])
                ACTV(p, mean[:], self.PS[b1][:, 0:GS], AF.Copy, [f"ps{b1}"], ["mean"], scale=1.0 / 1024)
                TT(p, "dve", m2[:], mean[:], mean[:], ALU.mult, ["mean"], ["m2"])
                STT(p, var[:], self.PS[b2][:, 0:GS], 1.0 / 1024, m2[:], ALU.mult, ALU.subtract, [f"ps{b2}", "m2"], ["var"])
                TS(p, "dve", var[:], var[:], EPS, ALU.add, ["var"], ["var"])
                ACTV(p, var[:], var[:], AF.Sqrt, ["var"], ["var"])
                p.op("dve", lambda e: e.reciprocal(out=rstd[:], in_=var[:]), ["var"], ["rstd"])
                for c in range(8):
                    i = c % 2
                    TT(p, "dve", t1[i][:], convT[:, c, sl], mean[:], ALU.subtract, ["convT", "mean"], [f"t1{i}"])
                    TT(p, "pool", t2[i][:], t1[i][:], rstd[:], ALU.mult, [f"t1{i}", "rstd"], [f"t2{i}"])
                    ACTV(p, uactT[:, c, sl], t2[i][:], AF.Silu, [f"t2{i}", "cp2"], ["uactT"],
                         bias=cp[:, c, 33:34], scale=cp[:, c, 32:33])
            p.flush()
        if self.dbg == f"{l}C":
            p.barrier()
            for c in range(8):
                p.dma("sp", self.dbgA[c], uactT[:, c, :], ["uactT"], ["dbgA"])
                p.dma("sp", self.dbgB[c], convT[:, c, :], ["convT"], ["dbgB"])
            p.flush()

    def stage_D(self, l):
        p = self.p
        p.barrier()
        self.attnT = attnT = self.sb(self.lst, "attnT", [128, 8, T], BF16)
        p.op("pool", lambda e: e.memset(attnT[:, :, 0:128], 0.0), [], ["attnT"])
        with ExitStack() as st:
            bias = self.sb(st, "bias", [128, 16, 256], F32)
            bias2 = self.sb(st, "bias2", [128, 16, 256], F32)
            snk = self.sb(st, "snk", [128, 16], F32)
            qb = [self.sb(st, f"qb{i}", [128, 8, 128], BF16) for i in range(2)]
            kb = [self.sb(st, f"kb{i}", [128, 4, 256], BF16) for i in range(2)]
            vb = [self.sb(st, f"vb{i}", [128, 2, 256], BF16) for i in range(2)]
            sc = [self.sb(st, f"sc{i}", [128, 2, 256], F32) for i in range(2)]
            pb = [self.sb(st, f"pb{i}", [128, 2, 256], BF16) for i in range(2)]
            pT = [self.sb(st, f"pT{i}", [128, 4, 128], BF16) for i in range(2)]
            sm = [self.sb(st, f"asm{i}", [128, 16], F32) for i in range(2)]
            rs = [self.sb(st, f"rs{i}", [128, 16], F32) for i in range(2)]
            es = [self.sb(st, f"es{i}", [128, 16], F32) for i in range(2)]
            ab = [self.sb(st, f"ab_{i}", [128, 1024], BF16) for i in range(2)]
            p.dma("sp", bias[:], self.biasT.rearrange("p (h s) -> p h s", h=16), [], ["bias"])
            p.dma("sp", bias2[:], self.biasT.rearrange("p (h s) -> p h s", h=16), [], ["bias2"])
            self.load_bcast("sp", snk[:], self.sinks[l], "snk")
            TS(p, "dve", bias2[:, :, 0:128], bias2[:, :, 0:128], self.flg[:, 1:2], ALU.add, ["bias2", "flg"], ["bias2"])
            for b in range(1, NB):
                i = b % 2
                bt, bk = (bias2, "bias2") if b == 2 else (bias, "bias")
                t0 = b * 128
                p.dma("sp", qb[i][:], self.qT_s[:, :, t0:t0 + 128].rearrange("c p t -> p c t"), ["projout"], [f"qb{i}"])
                p.dma("sp", kb[i][:], self.kdT_s[:, :, t0 - 128:t0 + 128].rearrange("c p t -> p c t"), ["projout"],
                      [f"kb{i}"])
                p.dma("sp", vb[i][:], self.v_s[t0 - 128:t0 + 128, :].rearrange("(a p) d -> p a d", p=128), ["projout"],
                      [f"vb{i}"])
                ao_banks = (6, 7)
                for m in range(8):
                    j = m % 2
                    g = m // 2
                    tb_ = 4 + j
                    for hh in range(2):
                        lo = 64 * hh
                        sbk = 2 * j + hh
                        MM(p, self.PS[sbk][:, 0:256], qb[i][lo:lo + 64, m, :], kb[i][lo:lo + 64, g, :],
                           True, True, [f"qb{i}", f"kb{i}"], [f"ps{sbk}"])
                        STT(p, sc[j][:, hh, :], self.PS[sbk][:, 0:256], 0.125, bt[:, 2 * m + hh, :],
                            ALU.mult, ALU.add, [f"ps{sbk}", bk], [f"sc{j}"])
                    p.op("dve", lambda e, j=j, m=m: e.tensor_reduce(out=sm[j][:, 0:2], in_=sc[j][:], axis=AX.X,
                                                                    op=ALU.max), [f"sc{j}"], [f"asm{j}a"])
                    TT(p, "dve", sm[j][:, 2:4], sm[j][:, 0:2], snk[:, 2 * m:2 * m + 2], ALU.max, [f"asm{j}a", "snk"],
                       [f"asm{j}b"])
                    TS(p, "dve", sm[j][:, 4:6], sm[j][:, 2:4], -1.0, ALU.mult, [f"asm{j}b"], [f"asm{j}c"])
                    TT(p, "dve", sm[j][:, 6:8], snk[:, 2 * m:2 * m + 2], sm[j][:, 4:6], ALU.add, ["snk", f"asm{j}c"],
                       [f"asm{j}d"])
                    ACTV(p, es[i][:, 2 * m:2 * m + 2], sm[j][:, 6:8], AF.Exp, [f"asm{j}d"], [f"es{i}"])
                    for hh in range(2):
                        ACTV(p, pb[j][:, hh, :], sc[j][:, hh, :], AF.Exp, [f"sc{j}", f"asm{j}c"], [f"pb{j}", f"rs{i}"],
                             bias=sm[j][:, 4 + hh:5 + hh], accum=rs[i][:, 2 * m + hh:2 * m + hh + 1])
                    psb = self.PS[tb_][:].bitcast(BF16)
                    for hh in range(2):
                        for half in range(2):
                            n = hh * 2 + half
                            p.op("pe", lambda e, psb=psb, n=n, j=j, hh=hh, half=half: e.transpose(
                                psb[:, n * 128:(n + 1) * 128], pb[j][:, hh, half * 128:(half + 1) * 128], self.ident[:]),
                                 [f"pb{j}", "ident"], [f"ps{tb_}"])
                    CPY(p, "act" if j == 0 else "dve", pT[j][:], psb[:, 0:512].rearrange("p (n t) -> p n t", n=4),
                        [f"ps{tb_}"], [f"pT{j}"])
                    for hh in range(2):
                        h = 2 * m + hh
                        bo = ao_banks[h // 8]
                        col = (h % 8) * 64
                        for half in range(2):
                            MM(p, self.PS[bo][:, col:col + 64], pT[j][:, hh * 2 + half, :],
                               vb[i][:, half, g * 64:(g + 1) * 64], half == 0, half == 1,
                               [f"pT{j}", f"vb{i}"], [f"ps{bo}"])
                TT(p, "dve", rs[i][:], rs[i][:], es[i][:], ALU.add, [f"rs{i}", f"es{i}"], [f"rs{i}"])
                p.op("dve", lambda e, i=i: e.reciprocal(out=rs[i][:], in_=rs[i][:]), [f"rs{i}"], [f"rs{i}"])
                for hb_ in range(2):
                    bo = ao_banks[hb_]
                    TT(p, "dve", ab[i][:, hb_ * 512:(hb_ + 1) * 512].rearrange("p (h d) -> p h d", h=8),
                       self.PS[bo][:].rearrange("p (h d) -> p h d", h=8),
                       rs[i][:, hb_ * 8:(hb_ + 1) * 8].unsqueeze(2).to_broadcast([128, 8, 64]), ALU.mult,
                       [f"ps{bo}", f"rs{i}"], [f"ab_{i}"])
                psb = self.PS[4 + i][:].bitcast(BF16)
                for k in range(8):
                    p.op("pe", lambda e, psb=psb, k=k, i=i: e.transpose(psb[:, k * 128:(k + 1) * 128],
                                                                       ab[i][:, k * 128:(k + 1) * 128], self.ident[:]),
                         [f"ab_{i}", "ident"], [f"ps{4 + i}"])
                CPY(p, "act", attnT[:, :, t0:t0 + 128], psb[:, 0:1024].rearrange("p (k t) -> p k t", k=8),
                    [f"ps{4 + i}"], ["attnT"])
                p.flush()
        if self.dbg == f"{l}D":
            p.barrier()
            for c in range(8):
                p.dma("sp", self.dbgA[c], attnT[:, c, :], ["attnT"], ["dbgA"])
            p.flush()

    def stage_E(self, l):
        p = self.p
        p.barrier()
        uactT, attnT = self.uactT, self.attnT
        with ExitStack() as st:
            wc = [self.sb(st, f"wc{i}", [128, 1024], BF16) for i in range(2)]
            wa = [self.sb(st, f"wa{i}", [128, 1024], BF16) for i in range(2)]
            gc = [self.sb(st, f"gc{i}", [128, T], BF16) for i in range(2)]
            ga = [self.sb(st, f"ga{i}", [128, T], BF16) for i in range(2)]
            mx = [self.sb(st, f"mx{i}", [128, T], BF16) for i in range(2)]
            t1 = [self.sb(st, f"e1{i}", [128, GS], F32) for i in range(2)]
            t2 = [self.sb(st, f"e2{i}", [128, GS], F32) for i in range(2)]
            for c in range(16):
                i = c % 2
                p.dma("pool", wc[i][:], self.w_co[l * 16 + c], [], [f"wc{i}"])
                p.dma("pool", wa[i][:], self.w_ao[l * 16 + c], [], [f"wa{i}"])
                p.dma("sp", gc[i][:], self.gT_s[c], ["projout"], [f"gc{i}"])
                p.dma("sp", ga[i][:], self.gT_s[16 + c], ["projout"], [f"ga{i}"])
                for g in range(NG):
                    sl = slice(g * GS, (g + 1) * GS)
                    b1, b2 = 2 * (g % 2), 2 * (g % 2) + 1
                    j = g % 2
                    for k in range(8):
                        MM(p, self.PS[b1][:, 0:GS], wc[i][:, k * 128:(k + 1) * 128], uactT[:, k, sl], k == 0, k == 7,
                           [f"wc{i}", "uactT"], [f"ps{b1}"])
                    for k in range(8):
                        MM(p, self.PS[b2][:, 0:GS], wa[i][:, k * 128:(k + 1) * 128], attnT[:, k, sl], k == 0, k == 7,
                           [f"wa{i}", "attnT"], [f"ps{b2}"])
                    TT(p, "dve", t1[j][:], self.PS[b1][:, 0:GS], gc[i][:, sl], ALU.mult, [f"ps{b1}", f"gc{i}"], [f"e1{j}"])
                    TT(p, "dve", t2[j][:], self.PS[b2][:, 0:GS], ga[i][:, sl], ALU.mult, [f"ps{b2}", f"ga{i}"], [f"e2{j}"])
                    TT(p, "pool", mx[i][:, sl], t1[j][:], t2[j][:], ALU.add, [f"e1{j}", f"e2{j}"], [f"mx{i}"])
                p.dma("sp", self.mixT_s[c], mx[i][:], [f"mx{i}"], ["mixout"])
                p.flush()
        self.lst.close()

    def stage_F(self, l):
        p = self.p
        p.barrier()
        with ExitStack() as st:
            mixT = self.sb(st, "mixT", [128, KD, T], BF16)
            wo = [self.sb(st, f"wo{i}", [128, KD * 512], BF16) for i in range(2)]
            g1 = [self.sb(st, f"g1{i}", [128, 512], F32) for i in range(2)]
            xt = [self.sb(st, f"fx{i}", [128, 512], F32) for i in range(3)]
            tm = [self.sb(st, f"ft{i}", [128, 512], F32) for i in range(2)]
            for k in range(KD):
                p.dma("sp" if k % 2 == 0 else "act", mixT[:, k, :], self.mixT_s[k], ["mixout"], ["mixT"])
            n = 0
            for nq in range(4):
                i = nq % 2
                p.dma("pool", wo[i][:], self.w_o[l * 4 + nq], [], [f"wo{i}"])
                self.load_bcast("sp", g1[i][:], self.modscr[l, 2 * D + nq * 512:2 * D + (nq + 1) * 512], f"g1{i}", ["modscr"])
                for t in range(NB):
                    bi = t % 4
                    xi = n % 3
                    n += 1
                    rows = slice(t * 128, (t + 1) * 128)
                    cols = slice(nq * 512, (nq + 1) * 512)
                    p.dma("sp", xt[xi][:], self.xsrc(l)[rows, cols], self.xkeys(t, nq), [f"fx{xi}"])
                    for k in range(KD):
                        MM(p, self.PS[bi][:], mixT[:, k, rows], wo[i][:, k * 512:(k + 1) * 512], k == 0, k == KD - 1,
                           ["mixT", f"wo{i}"], [f"ps{bi}"])
                    TT(p, "dve", tm[t % 2][:], self.PS[bi][:], g1[i][:], ALU.mult, [f"ps{bi}", f"g1{i}"], [f"ft{t % 2}"])
                    TT(p, "pool", xt[xi][:], tm[t % 2][:], xt[xi][:], ALU.add, [f"ft{t % 2}", f"fx{xi}"], [f"fx{xi}"])
                    p.dma("act", self.xres[rows, cols], xt[xi][:], [f"fx{xi}"], self.xkeys(t, nq))
                p.flush()

    def stage_G(self, l):
        p = self.p
        p.barrier()
        last = (l == L - 1)
        with ExitStack() as st:
            skb = self.sb(st, "skb", [128, 16, 128], BF16)
            p.dma("pool", skb[:], self.skT[l].rearrange("p (h n) -> p h n", h=16), [], ["skb"])
            h2T = self.sb(st, "h2T", [128, KD, TB * 128], BF16)
            E = self.sb(st, "E", [128, TB, 8, 2, 128], F32)
            pthr = self.sb(st, "pthr", [128, TB, 8], F32)
            acc = self.sb(st, "acc", [128, TB, D], F32)
            xb1 = self.sb(st, "gx0", [128, D], F32)
            xb = [xb1, xb1]
            Dh = self.sb(st, "Dh", [128, TB, 8, 128], BF16)
            hb1 = self.sb(st, "gh0", [128, D], BF16)
            hb = [hb1, hb1]
            sm = [self.sb(st, f"gsm{i}", [128, 4], F32) for i in range(2)]
            qp = [self.sb(st, f"qp{i}", [128, TB * 128], BF16) for i in range(2)]
            nm = [self.sb(st, f"nm{i}", [128, 4], F32) for i in range(2)]
            top = self.sb(st, "top", [128, 8, 2, 16], F32)
            tops = self.sb(st, "tops", [128, 8, 16], F32)
            etmp = self.sb(st, "etmp", [128, 128], F32)
            Pc = self.sb(st, "Pc", [128, 8, 16, 16], F32)
            pct = self.sb(st, "pct", [128, 16, 16], F32)
            best = self.sb(st, "best", [128, 8, 16], F32)
            Z = self.sb(st, "Z", [128, 16], F32)
            uball = self.sb(st, "uball", [128, 3, D], BF16)
            ub = [uball[:, i, :] for i in range(3)]
            vb = [[self.sb(st, f"vb{g}_{c}", [128, D], BF16) for c in range(EG)] for g in range(2)]
            gmv, shv, tmpv = acc[:, 1, :], acc[:, 2, :], acc[:, 0, :]
            g2v = Pc[:].rearrange("p h a b -> p (h a b)")
            fgv = uball[:, 0:2, :].rearrange("p a d -> p (a d)").bitcast(F32)
            gact = [self.sb(st, f"gact{i}", [128, TB * 128], F32) for i in range(3)]
            Pf = [self.sb(st, f"Pf{i}", [128, 8, 128], F32) for i in range(2)]
            Pfa = [self.sb(st, f"Pfa{i}", [128, 8, 128], F32) for i in range(2)]
            Mh = [self.sb(st, f"Mh{i}", [128, 8, 128], BF16) for i in range(2)]
            wT = [[self.sb(st, f"wT{i}_{c}", [128, TB * 128], BF16) for c in range(EG)] for i in range(2)]
            t_first = 2 if last else 1
            n_t = NB - t_first
            n_b = -(-n_t // TB)
            sizes = [n_t // n_b + (1 if i < n_t % n_b else 0) for i in range(n_b)]
            starts = [t_first + sum(sizes[:i]) for i in range(n_b)]
            for b0, ntl in zip(starts, sizes):
                nt = ntl * 128
                self.make_gm(gmv, shv, tmpv, "acc", l, self.n2g[l], 4, 3, "bc", kgm="acc", ksh="acc")
                for tl in range(ntl):
                    i = tl % 2
                    t = b0 + tl
                    p.dma("sp", xb[i][:], self.xres[t * 128:(t + 1) * 128, :], self.xkeys(t), ["gx0"])
                    self.norm_tile(xb[i][:], "gx0", gmv, shv, ["acc"], hb[i][:], "gh0", sm[i], f"gsm{i}",
                                   hb[i][:], "gh0", tmpv)
                    self.transpose_tile(hb[i], "gh0", lambda k0, tl=tl: h2T[:, k0:k0 + 8, tl * 128:(tl + 1) * 128],
                                        "h2T", 2 * i)
                if self.gstop == 5:
                    p.flush()
                    return
                for hp in range(self.g1hp):
                    i = hp % 2
                    p.dma("pool", ub[i][:], self.w_pq[l * 16 + hp], [], [f"ub{i}"])
                    bi = 4 + i
                    for k in range(KD):
                        MM(p, self.PS[bi][:, 0:nt], ub[i][:, k * 128:(k + 1) * 128], h2T[:, k, 0:nt], k == 0, k == KD - 1,
                           [f"ub{i}", "h2T"], [f"ps{bi}"])
                    CPY(p, "act", qp[i][:, 0:nt], self.PS[bi][:, 0:nt], [f"ps{bi}"], [f"qp{i}"])
                    bs = 6 + i
                    for tl in range(ntl if self.g1mode != 2 else 0):
                        if self.g1var == 1:
                            MM(p, self.PS[bs][:, tl * 128:(tl + 1) * 128], qp[i][:, tl * 128:(tl + 1) * 128], self.ident[:],
                               True, True, [f"qp{i}", "ident"], [f"ps{bs}"])
                        elif self.g1var == 2:
                            MM(p, self.PS[bs][:, tl * 128:(tl + 1) * 128], skb[:, hp, :], qp[i][:, tl * 128:(tl + 1) * 128],
                               True, True, [f"qp{i}", "skb"], [f"ps{bs}"])
                        elif self.g1var == 3:
                            MM(p, self.PS[2][:, tl * 128:(tl + 1) * 128], qp[i][:, tl * 128:(tl + 1) * 128], skb[:, hp, :],
                               True, True, [f"qp{i}", "skb"], [f"ps2"])
                        else:
                            MM(p, self.PS[bs][:, tl * 128:(tl + 1) * 128], qp[i][:, tl * 128:(tl + 1) * 128], skb[:, hp, :],
                               True, True, [f"qp{i}", "skb"], [f"ps{bs}"])
                    for tl in range(ntl if self.g1mode == 1 else 0):
                        pss = self.PS[bs][:, tl * 128:(tl + 1) * 128]
                        nmc = nm[i][:, tl:tl + 1]
                        p.op("dve", lambda e, pss=pss, nmc=nmc: e.tensor_reduce(out=nmc, in_=pss, axis=AX.X,
                                                                                op=ALU.max, negate=True),
                             [f"ps{bs}"], [f"nm{i}_{tl}"])
                        ACTV(p, E[:, tl, hp // 2, hp % 2, :], pss, AF.Exp, [f"ps{bs}", f"nm{i}_{tl}"], ["E"], bias=nmc)
                p.flush()
                if self.gstop == 1:
                    return
                for tl in range(ntl):
                    for hp in range(16):
                        h, pp = hp // 2, hp % 2
                        src = E[:, tl, h, pp, :]
                        p.op("dve", lambda e, src=src, h=h, pp=pp: e.max(out=top[:, h, pp, 0:8], in_=src), ["E"], ["top"])
                        p.op("dve", lambda e, src=src, h=h, pp=pp: e.match_replace(out=etmp[:], in_to_replace=top[:, h, pp, 0:8],
                                                                                   in_values=src, imm_value=-1.0),
                             ["E", "top"], ["etmp"])
                        p.op("dve", lambda e, h=h, pp=pp: e.max(out=top[:, h, pp, 8:16], in_=etmp[:]), ["etmp"], ["top"])

                    def top16_of_products(a_ap):
                        TT(p, "dve", Pc[:], a_ap.unsqueeze(3).to_broadcast([128, 8, 16, 16]),
                           top[:, :, 1, :].unsqueeze(2).to_broadcast([128, 8, 16, 16]), ALU.mult, ["top", "tops"], ["Pc"])
                        for h in range(8):
                            p.op("dve", lambda e, h=h: e.max(out=best[:, h, 0:8], in_=Pc[:, h]), ["Pc"], ["best"])
                            p.op("dve", lambda e, h=h: e.match_replace(out=pct[:], in_to_replace=best[:, h, 0:8],
                                                                       in_values=Pc[:, h], imm_value=-1.0),
                                 ["Pc", "best"], ["pct"])
                            p.op("dve", lambda e, h=h: e.max(out=best[:, h, 8:16], in_=pct[:]), ["pct"], ["best"])

                    top16_of_products(top[:, :, 0, :])
                    p.op("dve", lambda e: e.tensor_reduce(out=Z[:, 0:8], in_=best[:], axis=AX.X, op=ALU.add), ["best"], ["Z"])
                    p.op("dve", lambda e: e.reciprocal(out=Z[:, 8:16], in_=Z[:, 0:8]), ["Z"], ["Z"])
                    rz = Z[:, 8:16]
                    TT(p, "dve", E[:, tl, :, 0, :], E[:, tl, :, 0, :], rz.unsqueeze(2).to_broadcast([128, 8, 128]), ALU.mult,
                       ["E", "Z"], ["E"])
                    TT(p, "dve", tops[:], top[:, :, 0, :], rz.unsqueeze(2).to_broadcast([128, 8, 16]), ALU.mult,
                       ["top", "Z"], ["tops"])
                    top16_of_products(tops[:])
                    CPY(p, "dve", pthr[:, tl, :], best[:, :, 15], ["best"], ["pthr"])
                    p.op("dve", lambda e, tl=tl: e.reciprocal(out=Z[:, 0:8], in_=pthr[:, tl, :]), ["pthr"], ["Z"])
                    TT(p, "dve", E[:, tl, :, 0, :], E[:, tl, :, 0, :], Z[:, 0:8].unsqueeze(2).to_broadcast([128, 8, 128]),
                       ALU.mult, ["E", "Z"], ["E"])
                    TT(p, "dve", Dh[:, tl], self.ident[:].unsqueeze(1).to_broadcast([128, 8, 128]),
                       pthr[:, tl, :].unsqueeze(2).to_broadcast([128, 8, 128]), ALU.mult, ["ident", "pthr"], ["Dh"])
                p.flush()
                if self.gstop == 2:
                    return
                n_ch = (self.geg * EG) if self.gstop else NCH
                THR = 1.0 - 1e-6
                cnt = {"f": 0}

                def emit_uload(c):
                    p.dma("pool", ub[c % 3][:], self.uT[l * NCH + c], [], [f"ub{c % 3}"])

                def emit_act(c):
                    ui, gu = c % 2, c % 3
                    for k in range(KD):
                        MM(p, self.PS[ui][:, 0:nt], ub[gu][:, k * 128:(k + 1) * 128], h2T[:, k, 0:nt], k == 0, k == KD - 1,
                           [f"ub{gu}", "h2T"], [f"ps{ui}"])
                    ACTV(p, gact[gu][:, 0:nt], self.PS[ui][:, 0:nt], AF.Gelu, [f"ps{ui}"], [f"gact{gu}"])

                def emit_masks(c):
                    bc = 2 + c % 2
                    for tl in range(ntl):
                        fi = cnt["f"] % 2
                        cnt["f"] += 1
                        if tl == 2:
                            src, sk = Pfa[c % 2], f"Pfa{c % 2}"
                        else:
                            src, sk = Pf[fi], f"Pf{fi}"
                            TT(p, "pool", Pf[fi][:], E[:, tl, :, 1, :], E[:, tl, :, 0, c:c + 1].to_broadcast([128, 8, 128]),
                               ALU.mult, ["E"], [f"Pf{fi}"])
                        STT(p, Mh[fi][:], src[:], THR, src[:], ALU.is_ge, ALU.mult, [sk], [f"Mh{fi}"])
                        for h in range(8):
                            MM(p, self.PS[bc][:, tl * 128:(tl + 1) * 128], Mh[fi][:, h, :], Dh[:, tl, h, :], h == 0, h == 7,
                               [f"Mh{fi}", "Dh"], [f"ps{bc}"])

                def emit_pf_act(c):
                    if ntl < 3:
                        return
                    for h in range(8):
                        ACTV(p, Pfa[c % 2][:, h, :], E[:, 2, h, 1, :], AF.Copy, ["E"], [f"Pfa{c % 2}"],
                             scale=E[:, 2, h, 0, c:c + 1])

                def emit_wT(c):
                    eg, cc = divmod(c, EG)
                    gi, gu, bc = eg % 2, c % 3, 2 + c % 2
                    TT(p, "dve", wT[gi][cc][:, 0:nt], self.PS[bc][:, 0:nt], gact[gu][:, 0:nt], ALU.mult,
                       [f"ps{bc}", f"gact{gu}"], [f"wT{gi}_{cc}"])

                def emit_vload(eg):
                    for cc in range(EG):
                        p.dma("pool", vb[eg % 2][cc][:], self.vv[l * NCH + eg * EG + cc], [], [f"vb{eg % 2}_{cc}"])

                def emit_out_tile(eg, tl):
                    gi = eg % 2
                    for dq in range(4):
                        bo = 4 + dq
                        for cc in range(EG):
                            MM(p, self.PS[bo][:], wT[gi][cc][:, tl * 128:(tl + 1) * 128], vb[gi][cc][:, dq * 512:(dq + 1) * 512],
                               cc == 0, cc == EG - 1, [f"wT{gi}_{cc}", f"vb{gi}_{cc}"], [f"ps{bo}"])
                        dst = acc[:, tl, dq * 512:(dq + 1) * 512]
                        if eg == 0:
                            CPY(p, "act", dst, self.PS[bo][:], [f"ps{bo}"], ["acc"])
                        else:
                            TT(p, "dve", dst, self.PS[bo][:], dst, ALU.add, [f"ps{bo}", "acc"], ["acc"])

                n_g = n_ch // EG
                emit_uload(0)
                emit_uload(1)
                emit_vload(0)
                if n_g > 1:
                    emit_vload(1)
                emit_pf_act(0)
                emit_act(0)
                for c in range(n_ch):
                    eg, cc = divmod(c, EG)
                    if c + 2 < n_ch:
                        emit_uload(c + 2)
                    if c + 1 < n_ch:
                        emit_pf_act(c + 1)
                        emit_act(c + 1)
                    emit_masks(c)
                    if c >= 1:
                        emit_wT(c - 1)
                    if eg >= 1 and cc < ntl:
                        emit_out_tile(eg - 1, cc)
                    if cc == EG - 1 and eg >= 1 and eg + 1 < n_g:
                        emit_vload(eg + 1)
                    if cc == EG - 1:
                        p.flush()
                emit_wT(n_ch - 1)
                for tl in range(ntl):
                    emit_out_tile(n_g - 1, tl)
                p.flush()
                if self.gstop == 3:
                    return
                self.load_bcast("sp", g2v, self.mod_row(l, 5), "Pc", ["modscr"])
                if last:
                    self.load_bcast("sp", fgv, self.fing[0], ["ub0", "ub1"])
                for tl in range(ntl):
                    i = tl % 2
                    t = b0 + tl
                    rows = slice(t * 128, (t + 1) * 128)
                    p.dma("sp", xb[i][:], self.xres[rows, :], self.xkeys(t), ["gx0"])
                    TT(p, "dve", acc[:, tl, :], acc[:, tl, :], g2v, ALU.mult, ["acc", "Pc"], ["acc"])
                    TT(p, "pool", xb[i][:], xb[i][:], acc[:, tl, :], ALU.add, ["gx0", "acc"], ["gx0"])
                    if not last or self.dbg:
                        p.dma("act", self.xres[rows, :], xb[i][:], ["gx0"], self.xkeys(t))
                    if last and t >= 2:
                        self.norm_tile(xb[i][:], "gx0", fgv, None, ["ub0", "ub1"], acc[:, tl, :], "acc", sm[i], f"gsm{i}",
                                       hb[i][:], "gh0", None)
                        p.dma("act", self.y[(t - 2) * 128:(t - 1) * 128, :], acc[:, tl, :], ["acc"], ["y"])
                p.flush()

    def stage_A(self, l):
        self.p.barrier()
        self.lst = ExitStack()


def _t5_bucket(dist):
    max_exact = 16
    d = np.maximum(dist, 0)
    large = max_exact + (np.log(np.maximum(d, 1).astype(np.float32) / max_exact)
                         / np.float32(np.log(128 / max_exact)) * (32 - max_exact)).astype(np.int32)
    large = np.minimum(large, 31)
    return np.where(d < max_exact, d, large)


def _chunk_cols(w, ncols):
    Lx, Kd, N = w.shape
    a = w.reshape(Lx, Kd // 128, 128, N // ncols, ncols).transpose(0, 3, 2, 1, 4)
    return np.ascontiguousarray(a).reshape(Lx * (N // ncols), 128, (Kd // 128) * ncols)


_CACHE = {}


def prepare_shared(inp):
    f = lambda a: np.asarray(a, dtype=np.float32)
    w_in = f(inp["w_in"])
    sh = {}
    sh["ada_w"] = _chunk_cols(f(inp["ada_w"]), 512)
    sh["ada_b"] = f(inp["ada_b"])
    sh["n1g"] = f(inp["norm1_g"])
    sh["n2g"] = f(inp["norm2_g"])
    sh["fing"] = f(inp["final_g"]).reshape(1, D)
    sh["w_in_c"] = _chunk_cols(w_in, 128)
    kcols = w_in[:, :, 3072:3328].reshape(L, D, 4, 64)
    kd = np.concatenate([kcols, kcols], axis=3).reshape(L, D, 512)
    sh["w_kd"] = _chunk_cols(kd, 128)
    sh["w_v"] = _chunk_cols(np.ascontiguousarray(w_in[:, :, 3328:3584]), 256)
    sh["w_co"] = _chunk_cols(f(inp["w_conv_out"]), 128)
    sh["w_ao"] = _chunk_cols(f(inp["w_attn_out"]), 128)
    sh["w_o"] = _chunk_cols(f(inp["w_out"]), 512)
    sh["w_pq"] = _chunk_cols(f(inp["w_pq"]), 128)
    sk = f(inp["sub_keys"])
    sh["skT"] = np.ascontiguousarray(sk.transpose(0, 4, 1, 2, 3)).reshape(L, 128, D)
    pu = f(inp["peer_u"])
    sh["uT"] = np.ascontiguousarray(pu.reshape(L, NCH, 128, KD, 128).transpose(0, 1, 4, 3, 2)).reshape(L * NCH, 128, D)
    sh["vv"] = f(inp["peer_v"]).reshape(L * NCH, 128, D)
    cp = np.concatenate([f(inp["dw_w"]).transpose(0, 2, 1), f(inp["dw_b"])[:, :, None],
                         f(inp["conv_ln_g"])[:, :, None], f(inp["conv_ln_b"])[:, :, None]], axis=2)
    sh["chanpar"] = np.ascontiguousarray(cp.reshape(L, 8, 128, 34).transpose(0, 2, 1, 3)).reshape(L, 128, 8 * 34)
    sh["sinks"] = f(inp["attn_sinks"])
    qi = np.arange(128)[:, None] + 128
    kj = np.arange(256)[None, :]
    dist = qi - kj
    bucket = _t5_bucket(dist)
    rb = f(inp["rel_bias"])
    tab = rb[bucket]
    band = (dist >= 0) & (dist < 128)
    tab = np.where(band[:, :, None], tab, np.float32(NEG))
    sh["biasT"] = np.ascontiguousarray(tab.transpose(0, 2, 1)).reshape(128, 16 * 256)
    return sh


def per_core(inp, c):
    b, qtr = c // 4, c % 4
    s0 = qtr * 2048
    x = np.asarray(inp["x"], dtype=np.float32)
    xw = np.zeros((T, D), np.float32)
    if qtr == 0:
        xw[256:] = x[b, 0:2048]
    else:
        xw[:] = x[b, s0 - 256:s0 + 2048]
    cc = np.asarray(inp["c"], dtype=np.float32)[b]
    flags = np.zeros((128, 2), np.float32)
    flags[:, 0] = 0.0 if qtr == 0 else 1.0
    flags[:, 1] = NEG if qtr == 0 else 0.0
    return {"x_in": xw, "c_in": np.ascontiguousarray(cc.reshape(KD, 128).T), "flags": flags}


def kernel(**inputs):
    if "nc" not in _CACHE:
        _CACHE["nc"] = K().build()
    nc = _CACHE["nc"]
    sh = prepare_shared(inputs)
    in_maps = []
    for c in range(8):
        m = dict(sh)
        m.update(per_core(inputs, c))
        in_maps.append(m)
    res = run_bass_kernel_spmd(nc, in_maps, core_ids=list(range(8)))
    out = np.zeros((2, SEQ, D), np.float32)
    for c in range(8):
        b, qtr = c // 4, c % 4
        out[b, qtr * 2048:(qtr + 1) * 2048] = res.results[c]["y"]
    return out
```

```python
import numpy as np
from contextlib import ExitStack
import concourse.bass as bass
import concourse.mybir as mybir
from concourse.bass_utils import run_bass_kernel_spmd

F32 = mybir.dt.float32
BF16 = mybir.dt.bfloat16
AF = mybir.ActivationFunctionType
ALU = mybir.AluOpType
AX = mybir.AxisListType

L = 2
D = 2048
KD = 16
NB = 18
T = NB * 128
GS = 384
NG = T // GS
SEQ = 8192
NEG = -30000.0
EPS = 1e-6
NCH = 128
TB = 3
EG = 4

ENGS = ["pe", "act", "dve", "pool", "sp"]
ENGMAP = {"pe": "tensor", "act": "scalar", "dve": "vector", "pool": "gpsimd", "sp": "sync"}
EPOCH = 1 << 30
NDMASEM = 6


class Prog:
    def __init__(self, nc, stack, block):
        self.nc, self.stack, self.block = nc, stack, block
        self.q = {e: [] for e in ENGS}
        self.cnt = {e: 0 for e in ENGS}
        self.known = {e: {} for e in ENGS}
        self.last_w, self.readers = {}, {}
        self.nsem = 0
        self.sems = {e: self._newsem(e) for e in ENGS}
        self.dsem = {qn: [[self._newsem("d" + qn), 0] for _ in range(NDMASEM)] for qn in ["sp", "act", "pool"]}
        self.dsem_i = {qn: 0 for qn in self.dsem}
        self.n_instr = 0

    def _newsem(self, name):
        self.nsem += 1
        return self.stack.enter_context(self.nc.semaphore(f"s{self.nsem}_{name}"))

    def _deps(self, eng, reads, writes):
        deps = []
        for k in reads:
            deps.extend(self.last_w.get(k, {}).values())
        for k in writes:
            for ev in self.last_w.get(k, {}).values():
                if ev[2] != eng:
                    deps.append(ev)
            for ev in self.readers.get(k, {}).values():
                if ev[2] != eng:
                    deps.append(ev)
        waits = {}
        kn = self.known[eng]
        for (sem, val, src) in deps:
            sid = id(sem)
            if kn.get(sid, 0) >= val:
                continue
            if sid not in waits or waits[sid][1] < val:
                waits[sid] = (sem, val)
        for sid, (sem, val) in waits.items():
            kn[sid] = val
        return list(waits.values())

    def _commit(self, ev, reads, writes):
        sid = id(ev[0])
        for k in writes:
            self.last_w.setdefault(k, {})[sid] = ev
        for k in reads:
            self.readers.setdefault(k, {})[sid] = ev

    def op(self, eng, fn, reads=(), writes=()):
        psr = [k for k in reads if k.startswith("ps") and k[2:].isdigit()]
        if psr:
            writes = list(writes) + psr
        waits = self._deps(eng, reads, writes)
        if self.cnt[eng] >= EPOCH:
            self.sems[eng] = self._newsem(eng)
            self.cnt[eng] = 0
        self.cnt[eng] += 1
        sem = self.sems[eng]
        ev = (sem, self.cnt[eng], eng)
        self.q[eng].append((waits, fn, sem, 1))
        self._commit(ev, reads, writes)
        self.n_instr += 1

    def dma(self, qn, out, in_, reads=(), writes=(), **kw):
        slot = self.dsem[qn][self.dsem_i[qn] % NDMASEM]
        self.dsem_i[qn] += 1
        sem, cnt = slot
        waits = self._deps(qn, reads, writes)
        if cnt > 0 and self.known[qn].get(id(sem), 0) < cnt:
            waits.append((sem, cnt))
            self.known[qn][id(sem)] = cnt
        slot[1] = cnt + 16
        ev = (sem, cnt + 16, "dma_" + qn)
        self.q[qn].append((waits, lambda e: e.dma_start(out=out, in_=in_, **kw), sem, 16))
        self._commit(ev, reads, writes)
        self.n_instr += 1

    def barrier(self):
        evs = [(self.sems[e], self.cnt[e]) for e in ENGS if self.cnt[e] > 0]
        for qn in self.dsem:
            evs += [(s, c) for (s, c) in self.dsem[qn] if c > 0]
        for e in ENGS:
            waits = []
            for (s, v) in evs:
                if s is self.sems[e]:
                    continue
                if self.known[e].get(id(s), 0) < v:
                    waits.append((s, v))
                    self.known[e][id(s)] = v
            if waits:
                self.q[e].append((waits, None, None, 0))

    def flush(self):
        for e in ENGS:
            items, self.q[e] = self.q[e], []
            if not items:
                continue

            def body(engobj, items=items):
                for (waits, fn, sem, inc) in items:
                    for (s, v) in waits:
                        engobj.wait_ge(s, v)
                    if fn is not None:
                        fn(engobj).then_inc(sem, inc)

            getattr(self.block, ENGMAP[e])(body)


def MM(p, out, lhsT, rhs, st, sp, r, w):
    p.op("pe", lambda e: e.matmul(out, lhsT=lhsT, rhs=rhs, start=st, stop=sp), r, w)


def ACTV(p, out, in_, func, r, w, bias=None, scale=None, accum=None):
    kw = {}
    if bias is not None:
        kw["bias"] = bias
    if scale is not None:
        kw["scale"] = scale
    if accum is not None:
        kw["accum_out"] = accum
    p.op("act", lambda e: e.activation(out=out, in_=in_, func=func, **kw), r, w)


def TT(p, eng, out, a, b, op, r, w):
    p.op(eng, lambda e: e.tensor_tensor(out=out, in0=a, in1=b, op=op), r, w)


def TS(p, eng, out, a, s1, op0, r, w, s2=None, op1=None):
    if op1 is None:
        p.op(eng, lambda e: e.tensor_scalar(out=out, in0=a, scalar1=s1, scalar2=None, op0=op0), r, w)
    else:
        p.op(eng, lambda e: e.tensor_scalar(out=out, in0=a, scalar1=s1, scalar2=s2, op0=op0, op1=op1), r, w)


def STT(p, out, a, s, b, op0, op1, r, w):
    p.op("dve", lambda e: e.scalar_tensor_tensor(out=out, in0=a, scalar=s, in1=b, op0=op0, op1=op1), r, w)


def CPY(p, eng, out, in_, r, w):
    if eng == "act":
        p.op("act", lambda e: e.activation(out=out, in_=in_, func=AF.Copy), r, w)
    else:
        p.op(eng, lambda e: e.tensor_copy(out=out, in_=in_), r, w)


class K:
    def __init__(self, dbg=None, nlayers=L):
        self.dbg = dbg
        self.nlayers = nlayers
        self.gstop = 0
        self.geg = 1
        self.in_shapes = {}
        self.only_g = False
        self.g1hp = 16
        self.g1mode = 1
        self.g1var = 0
        self.pipe_out = False
        nc = self.nc = bass.Bass("TRN2", target_bir_lowering=False)

        def din(name, shape, dt=F32):
            self.in_shapes[name] = list(shape)
            return nc.dram_tensor(name, list(shape), dt, kind="ExternalInput").ap()

        def dscr(name, shape, dt):
            kind = "ExternalOutput" if dbg else "Internal"
            return nc.dram_tensor(name, list(shape), dt, kind=kind).ap()

        self.x_in = din("x_in", [T, D])
        self.c_in = din("c_in", [128, KD])
        self.flags = din("flags", [128, 2])
        self.biasT = din("biasT", [128, 16 * 256])
        self.ada_w = din("ada_w", [L * 24, 128, KD * 512])
        self.ada_b = din("ada_b", [L, 6 * D])
        self.n1g = din("n1g", [L, D])
        self.n2g = din("n2g", [L, D])
        self.fing = din("fing", [1, D])
        self.w_in_c = din("w_in_c", [L * 60, 128, D])
        self.w_kd = din("w_kd", [L * 4, 128, D])
        self.w_v = din("w_v", [L, 128, KD * 256])
        self.w_co = din("w_co", [L * 16, 128, 1024])
        self.w_ao = din("w_ao", [L * 16, 128, 1024])
        self.w_o = din("w_o", [L * 4, 128, KD * 512])
        self.w_pq = din("w_pq", [L * 16, 128, D])
        self.skT = din("skT", [L, 128, D])
        self.uT = din("uT", [L * NCH, 128, D])
        self.vv = din("vv", [L * NCH, 128, D])
        self.chanpar = din("chanpar", [L, 128, 8 * 34])
        self.sinks = din("sinks", [L, 16])
        self.y = nc.dram_tensor("y", [16 * 128, D], F32, kind="ExternalOutput").ap()

        self.xres = dscr("xres", [T, D], F32)
        self.modscr = dscr("modscr", [L, 6 * D], F32)
        self.qT_s = dscr("qT_s", [8, 128, T], BF16)
        self.kdT_s = dscr("kdT_s", [4, 128, T], BF16)
        self.v_s = dscr("v_s", [T, 256], BF16)
        self.gT_s = dscr("gT_s", [32, 128, T], BF16)
        self.mixT_s = dscr("mixT_s", [16, 128, T], BF16)
        if dbg:
            self.dbgA = dscr("dbgA", [16, 128, T], BF16)
            self.dbgB = dscr("dbgB", [16, 128, T], BF16)

    def build(self):
        nc = self.nc
        with ExitStack() as st, nc.Block() as block:
            p = self.p = Prog(nc, st, block)
            self.PS = [st.enter_context(nc.psum_tensor(f"ps{i}", [128, 512], F32)) for i in range(8)]
            self.ident = st.enter_context(nc.sbuf_tensor("ident", [128, 128], BF16))
            self.ones = st.enter_context(nc.sbuf_tensor("ones", [128, 128], BF16))
            self.flg = st.enter_context(nc.sbuf_tensor("flg", [128, 2], F32))
            ident, ones = self.ident, self.ones
            p.op("pool", lambda e: e.memset(ident[:], 0.0), [], ["ident"])
            p.op("pool", lambda e: e.affine_select(out=ident[:], in_=ident[:], pattern=[[-1, 128]],
                                                   compare_op=ALU.not_equal, fill=1.0, base=0,
                                                   channel_multiplier=1), ["ident"], ["ident"])
            p.op("pool", lambda e: e.memset(ones[:], 1.0), [], ["ones"])
            p.dma("sp", self.flg[:], self.flags, [], ["flg"])
            if self.only_g:
                self.stage_G(0)
                p.barrier()
                p.flush()
                return nc
            self.stage_mods()
            done = False
            for l in range(self.nlayers):
                for nm in ["A", "B", "C", "D", "E", "F", "G"]:
                    getattr(self, "stage_" + nm)(l)
                    if self.dbg == f"{l}{nm}":
                        done = True
                        break
                if done:
                    break
            p.barrier()
            p.flush()
            if getattr(self, "lst", None) is not None:
                self.lst.close()
        return nc

    def stage_ctx(self):
        self.p.barrier()
        return ExitStack()

    def sb(self, st, name, shape, dt):
        self.uid = getattr(self, "uid", 0) + 1
        return st.enter_context(self.nc.sbuf_tensor(f"{name}_u{self.uid}", list(shape), dt))

    def stage_mods(self):
        p = self.p
        with self.stage_ctx() as st:
            cs = self.sb(st, "cs", [128, KD], F32)
            wb = [self.sb(st, f"adab{i}", [128, KD * 512], F32) for i in range(2)]
            ab = [self.sb(st, f"ab{i}", [1, 512], F32) for i in range(2)]
            mr = [self.sb(st, f"mr{i}", [1, 512], F32) for i in range(2)]
            p.dma("sp", cs[:], self.c_in, [], ["cs"])
            ACTV(p, cs[:], cs[:], AF.Silu, ["cs"], ["cs"])
            for l in range(self.nlayers):
                for n in range(24):
                    i = n % 2
                    p.dma("sp" if i == 0 else "act", wb[i][:], self.ada_w[l * 24 + n], [], [f"adab{i}"])
                    p.dma("sp", ab[i][:], self.ada_b[l:l + 1, n * 512:(n + 1) * 512], [], [f"ab{i}"])
                    ps = self.PS[i]
                    for k in range(KD):
                        MM(p, ps[0:1, :], cs[:, k:k + 1], wb[i][:, k * 512:(k + 1) * 512], k == 0, k == KD - 1,
                           ["cs", f"adab{i}"], [f"ps{i}"])
                    TT(p, "dve", mr[i][:], ps[0:1, :], ab[i][:], ALU.add, [f"ps{i}", f"ab{i}"], [f"mr{i}"])
                    p.dma("sp", self.modscr[l:l + 1, n * 512:(n + 1) * 512], mr[i][:], [f"mr{i}"], ["modscr"])
            p.flush()

    def load_bcast(self, q, dst, src_row, key, reads=()):
        keys = key if isinstance(key, list) else [key]
        self.p.dma(q, dst, src_row.partition_broadcast(128), list(reads), keys)

    def mod_row(self, l, i):
        return self.modscr[l, i * D:(i + 1) * D]

    def make_gm(self, gm, sh, tmp, tk, l, gain_row, sc_i, sh_i, pfx, kgm=None, ksh=None):
        p = self.p
        kgm = kgm or pfx + "gm"
        ksh = ksh or pfx + "sh"
        self.load_bcast("sp", gm[:], gain_row, kgm)
        self.load_bcast("sp", tmp[:], self.mod_row(l, sc_i), tk, ["modscr"])
        self.load_bcast("sp", sh[:], self.mod_row(l, sh_i), ksh, ["modscr"])
        STT(p, gm[:], tmp[:], 1.0, gm[:], ALU.add, ALU.mult, [tk, kgm], [kgm])

    def norm_tile(self, xt, xk, gm, sh, gk, hb, hk, sm, smk, junk, jk, tmp):
        p = self.p
        ACTV(p, junk, xt, AF.Square, [xk], [jk, smk + "ss"], accum=sm[:, 0:1])
        TS(p, "dve", sm[:, 1:2], sm[:, 0:1], 1.0 / D, ALU.mult, [smk + "ss"], [smk + "a"], s2=EPS, op1=ALU.add)
        ACTV(p, sm[:, 2:3], sm[:, 1:2], AF.Sqrt, [smk + "a"], [smk + "b"])
        p.op("dve", lambda e: e.reciprocal(out=sm[:, 3:4], in_=sm[:, 2:3]), [smk + "b"], [smk + "r"])
        if sh is None:
            STT(p, hb, xt, sm[:, 3:4], gm[:], ALU.mult, ALU.mult, [xk, smk + "r"] + gk, [hk])
        else:
            STT(p, tmp, xt, sm[:, 3:4], gm[:], ALU.mult, ALU.mult, [xk, smk + "r"] + gk, ["ntmp"])
            TT(p, "dve", hb, tmp, sh[:], ALU.add, ["ntmp"] + gk, [hk])

    def transpose_tile(self, hb, hk, dst_fn, dk, bank0):
        p = self.p
        for half in range(2):
            bi = bank0 + half
            psb = self.PS[bi][:].bitcast(BF16)
            for j in range(8):
                k = half * 8 + j
                p.op("pe", lambda e, j=j, k=k, psb=psb: e.transpose(psb[:, j * 128:(j + 1) * 128],
                                                                   hb[:, k * 128:(k + 1) * 128], self.ident[:]),
                     [hk, "ident"], [f"ps{bi}"])
            src = psb[:, 0:1024].rearrange("p (j t) -> p j t", j=8)
            CPY(p, "act" if half == 0 else "dve", dst_fn(half * 8), src, [f"ps{bi}"], [dk])

    def xsrc(self, l):
        return self.x_in if l == 0 else self.xres

    def xkeys(self, t, nq=None):
        return [f"xr{t}_{q}" for q in (range(4) if nq is None else [nq])]

    def stage_B(self, l):
        p = self.p
        self.convT = convT = self.sb(self.lst, "convT", [128, 8, T], BF16)
        with self.stage_ctx() as st:
            hT = self.sb(st, "hT", [128, KD, T], BF16)
            with ExitStack() as st2:
                gm = self.sb(st2, "n1gm", [128, D], F32)
                sh = self.sb(st2, "n1sh", [128, D], F32)
                tmp = self.sb(st2, "ntmp", [128, D], F32)
                self.make_gm(gm, sh, tmp, "ntmp", l, self.n1g[l], 1, 0, "n1")
                xb = [self.sb(st2, f"xb{i}", [128, D], F32) for i in range(2)]
                hb = [self.sb(st2, f"hb{i}", [128, D], BF16) for i in range(2)]
                sm = [self.sb(st2, f"sm{i}", [128, 4], F32) for i in range(2)]
                for t in range(NB):
                    i = t % 2
                    p.dma("sp", xb[i][:], self.xsrc(l)[t * 128:(t + 1) * 128, :], self.xkeys(t), [f"xb{i}"])
                    self.norm_tile(xb[i][:], f"xb{i}", gm, sh, ["n1gm", "n1sh"], hb[i][:], f"hb{i}", sm[i], f"sm{i}",
                                   hb[i][:], f"hb{i}", tmp[:])
                    self.transpose_tile(hb[i], f"hb{i}", lambda k0, t=t: hT[:, k0:k0 + 8, t * 128:(t + 1) * 128],
                                        f"hT{t // 3}", 2 * i)
                p.flush()
            p.barrier()
            if self.dbg == f"{l}A":
                for k in range(KD):
                    p.dma("sp", self.dbgA[k], hT[:, k, :], [f"hT{g}" for g in range(NG)], ["dbgA"])
                p.flush()
                return
            wt = [self.sb(st, f"wt{i}", [128, D], BF16) for i in range(3)]
            stg = [self.sb(st, f"stg{i}", [128, GS], BF16) for i in range(3)]
            sig = [self.sb(st, f"sig{i}", [128, GS], F32) for i in range(2)]
            upad = [self.sb(st, f"upad{i}", [128, 32 + T], F32) for i in range(2)]
            cacc = self.sb(st, "cacc", [128, T], F32)
            cp = self.sb(st, "cp", [128, 8, 34], F32)
            wv = self.sb(st, "wv", [128, KD * 256], BF16)
            vst = [self.sb(st, f"vst{i}", [128, 256], BF16) for i in range(2)]
            p.dma("sp", cp[:], self.chanpar[l].rearrange("p (c t) -> p c t", c=8), [], ["cp"])
            for i in range(2):
                p.op("pool", lambda e, i=i: e.memset(upad[i][:], 0.0), [], [f"upad{i}"])
            state = {"w": 0, "ps": 0, "stg": 0}

            def proj(wsrc, evac):
                wi = state["w"] % 3
                state["w"] += 1
                p.dma("pool", wt[wi][:], wsrc, [], [f"wt{wi}"])
                for g in range(NG):
                    bi = state["ps"] % 4
                    state["ps"] += 1
                    ps = self.PS[bi]
                    for k in range(KD):
                        MM(p, ps[:, 0:GS], wt[wi][:, k * 128:(k + 1) * 128], hT[:, k, g * GS:(g + 1) * GS],
                           k == 0, k == KD - 1, [f"wt{wi}", f"hT{g}"], [f"ps{bi}"])
                    evac(g, ps[:, 0:GS], f"ps{bi}")

            def evac_store(dst, func):
                def f(g, ps, pk):
                    si = state["stg"] % 3
                    state["stg"] += 1
                    ACTV(p, stg[si][:], ps, func, [pk], [f"stg{si}"])
                    p.dma("sp", dst[:, g * GS:(g + 1) * GS], stg[si][:], [f"stg{si}"], ["projout"])
                return f

            jobs = []
            for c in range(8):
                jobs.append((self.w_in_c[l * 60 + 16 + c], evac_store(self.qT_s[c], AF.Copy)))
            for c in range(4):
                jobs.append((self.w_kd[l * 4 + c], evac_store(self.kdT_s[c], AF.Copy)))
            for c in range(32):
                jobs.append((self.w_in_c[l * 60 + 28 + c], evac_store(self.gT_s[c], AF.Sigmoid)))
            p.dma("pool", wv[:], self.w_v[l], [], ["wv"])
            for t in range(NB):
                bi = 4 + (t % 2)
                ps = self.PS[bi]
                for k in range(KD):
                    MM(p, ps[:, 0:256], hT[:, k, t * 128:(t + 1) * 128], wv[:, k * 256:(k + 1) * 256],
                       k == 0, k == KD - 1, ["wv", f"hT{t // 3}"], [f"ps{bi}"])
                CPY(p, "act", vst[t % 2][:], ps[:, 0:256], [f"ps{bi}"], [f"vst{t % 2}"])
                p.dma("sp", self.v_s[t * 128:(t + 1) * 128, :], vst[t % 2][:], [f"vst{t % 2}"], ["projout"])
            p.flush()
            for c in range(8):
                ui = c % 2
                up = upad[ui]
                wa = state["w"] % 3
                state["w"] += 1
                wbi = state["w"] % 3
                state["w"] += 1
                p.dma("pool", wt[wa][:], self.w_in_c[l * 60 + c], [], [f"wt{wa}"])
                p.dma("pool", wt[wbi][:], self.w_in_c[l * 60 + 8 + c], [], [f"wt{wbi}"])
                for g in range(NG):
                    ba = (2 * g) % 4
                    bb = (2 * g + 1) % 4
                    for k in range(KD):
                        MM(p, self.PS[ba][:, 0:GS], wt[wa][:, k * 128:(k + 1) * 128], hT[:, k, g * GS:(g + 1) * GS],
                           k == 0, k == KD - 1, [f"wt{wa}", f"hT{g}"], [f"ps{ba}"])
                    for k in range(KD):
                        MM(p, self.PS[bb][:, 0:GS], wt[wbi][:, k * 128:(k + 1) * 128], hT[:, k, g * GS:(g + 1) * GS],
                           k == 0, k == KD - 1, [f"wt{wbi}", f"hT{g}"], [f"ps{bb}"])
                    ACTV(p, sig[g % 2][:], self.PS[bb][:, 0:GS], AF.Sigmoid, [f"ps{bb}"], [f"sig{g % 2}"])
                    TT(p, "dve", up[:, 32 + g * GS:32 + (g + 1) * GS], self.PS[ba][:, 0:GS], sig[g % 2][:], ALU.mult,
                       [f"ps{ba}", f"sig{g % 2}"], [f"upad{ui}"])
                TS(p, "dve", up[:, 32:32 + 256], up[:, 32:32 + 256], self.flg[:, 0:1], ALU.mult,
                   [f"upad{ui}", "flg"], [f"upad{ui}"])
                TS(p, "dve", cacc[:], up[:, 2:2 + T], cp[:, c, 0:1], ALU.mult, [f"upad{ui}", "cp"], ["cacc"],
                   s2=cp[:, c, 31:32], op1=ALU.add)
                for j in range(1, 31):
                    dst = cacc[:] if j < 30 else convT[:, c, :]
                    STT(p, dst, up[:, 2 + j:2 + j + T], cp[:, c, j:j + 1], cacc[:], ALU.mult, ALU.add,
                        [f"upad{ui}", "cp", "cacc"], ["cacc"] if j < 30 else ["convT"])
                for _ in range(6 if c < 7 else len(jobs)):
                    if jobs:
                        proj(*jobs.pop(0))
                p.flush()

    def stage_C(self, l):
        p = self.p
        convT = self.convT
        self.p.barrier()
        self.uactT = uactT = self.sb(self.lst, "uactT", [128, 8, T], BF16)
        with ExitStack() as st2:
            cp = self.sb(st2, "cp2", [128, 8, 34], F32)
            sq = [self.sb(st2, f"sq{i}", [128, GS], BF16) for i in range(2)]
            mean = self.sb(st2, "mean", [128, GS], F32)
            m2 = self.sb(st2, "m2", [128, GS], F32)
            var = self.sb(st2, "var", [128, GS], F32)
            rstd = self.sb(st2, "rstd", [128, GS], F32)
            t1 = [self.sb(st2, f"t1{i}", [128, GS], F32) for i in range(2)]
            t2 = [self.sb(st2, f"t2{i}", [128, GS], F32) for i in range(2)]
            p.dma("sp", cp[:], self.chanpar[l].rearrange("p (c t) -> p c t", c=8), [], ["cp2"])
            for g in range(NG):
                sl = slice(g * GS, (g + 1) * GS)
                b1, b2 = 2 * (g % 2), 2 * (g % 2) + 1
                for c in range(8):
                    MM(p, self.PS[b1][:, 0:GS], self.ones[:], convT[:, c, sl], c == 0, c == 7, ["ones", "convT"],
                       [f"ps{b1}"])
                for c in range(8):
                    ACTV(p, sq[c % 2][:], convT[:, c, sl], AF.Square, ["convT"], [f"sq{c % 2}"])
                    MM(p, self.PS[b2][:, 0:GS], self.ones[:], sq[c % 2][:], c == 0, c == 7, ["ones", f"sq{c % 2}"],
                       [f"ps{b2}"])
                ACTV(p, mean[:], self.PS[b1][:, 0:GS], AF.Copy, [f"ps{b1}"], ["mean"], scale=1.0 / 1024)
                TT(p, "dve", m2[:], mean[:], mean[:], ALU.mult, ["mean"], ["m2"])
                STT(p, var[:], self.PS[b2][:, 0:GS], 1.0 / 1024, m2[:], ALU.mult, ALU.subtract, [f"ps{b2}", "m2"], ["var"])
                TS(p, "dve", var[:], var[:], EPS, ALU.add, ["var"], ["var"])
                ACTV(p, var[:], var[:], AF.Sqrt, ["var"], ["var"])
                p.op("dve", lambda e: e.reciprocal(out=rstd[:], in_=var[:]), ["var"], ["rstd"])
                for c in range(8):
                    i = c % 2
                    TT(p, "dve", t1[i][:], convT[:, c, sl], mean[:], ALU.subtract, ["convT", "mean"], [f"t1{i}"])
                    TT(p, "pool", t2[i][:], t1[i][:], rstd[:], ALU.mult, [f"t1{i}", "rstd"], [f"t2{i}"])
                    ACTV(p, uactT[:, c, sl], t2[i][:], AF.Silu, [f"t2{i}", "cp2"], ["uactT"],
                         bias=cp[:, c, 33:34], scale=cp[:, c, 32:33])
            p.flush()
        if self.dbg == f"{l}C":
            p.barrier()
            for c in range(8):
                p.dma("sp", self.dbgA[c], uactT[:, c, :], ["uactT"], ["dbgA"])
                p.dma("sp", self.dbgB[c], convT[:, c, :], ["convT"], ["dbgB"])
            p.flush()

    def stage_D(self, l):
        p = self.p
        p.barrier()
        self.attnT = attnT = self.sb(self.lst, "attnT", [128, 8, T], BF16)
        p.op("pool", lambda e: e.memset(attnT[:, :, 0:128], 0.0), [], ["attnT"])
        with ExitStack() as st:
            bias = self.sb(st, "bias", [128, 16, 256], F32)
            bias2 = self.sb(st, "bias2", [128, 16, 256], F32)
            snk = self.sb(st, "snk", [128, 16], F32)
            qb = [self.sb(st, f"qb{i}", [128, 8, 128], BF16) for i in range(2)]
            kb = [self.sb(st, f"kb{i}", [128, 4, 256], BF16) for i in range(2)]
            vb = [self.sb(st, f"vb{i}", [128, 2, 256], BF16) for i in range(2)]
            sc = [self.sb(st, f"sc{i}", [128, 2, 256], F32) for i in range(2)]
            pb = [self.sb(st, f"pb{i}", [128, 2, 256], BF16) for i in range(2)]
            pT = [self.sb(st, f"pT{i}", [128, 4, 128], BF16) for i in range(2)]
            sm = [self.sb(st, f"asm{i}", [128, 16], F32) for i in range(2)]
            rs = [self.sb(st, f"rs{i}", [128, 16], F32) for i in range(2)]
            es = [self.sb(st, f"es{i}", [128, 16], F32) for i in range(2)]
            ab = [self.sb(st, f"ab_{i}", [128, 1024], BF16) for i in range(2)]
            p.dma("sp", bias[:], self.biasT.rearrange("p (h s) -> p h s", h=16), [], ["bias"])
            p.dma("sp", bias2[:], self.biasT.rearrange("p (h s) -> p h s", h=16), [], ["bias2"])
            self.load_bcast("sp", snk[:], self.sinks[l], "snk")
            TS(p, "dve", bias2[:, :, 0:128], bias2[:, :, 0:128], self.flg[:, 1:2], ALU.add, ["bias2", "flg"], ["bias2"])
            for b in range(1, NB):
                i = b % 2
                bt, bk = (bias2, "bias2") if b == 2 else (bias, "bias")
                t0 = b * 128
                p.dma("sp", qb[i][:], self.qT_s[:, :, t0:t0 + 128].rearrange("c p t -> p c t"), ["projout"], [f"qb{i}"])
                p.dma("sp", kb[i][:], self.kdT_s[:, :, t0 - 128:t0 + 128].rearrange("c p t -> p c t"), ["projout"],
                      [f"kb{i}"])
                p.dma("sp", vb[i][:], self.v_s[t0 - 128:t0 + 128, :].rearrange("(a p) d -> p a d", p=128), ["projout"],
                      [f"vb{i}"])
                ao_banks = (6, 7)
                for m in range(8):
                    j = m % 2
                    g = m // 2
                    tb_ = 4 + j
                    for hh in range(2):
                        lo = 64 * hh
                        sbk = 2 * j + hh
                        MM(p, self.PS[sbk][:, 0:256], qb[i][lo:lo + 64, m, :], kb[i][lo:lo + 64, g, :],
                           True, True, [f"qb{i}", f"kb{i}"], [f"ps{sbk}"])
                        STT(p, sc[j][:, hh, :], self.PS[sbk][:, 0:256], 0.125, bt[:, 2 * m + hh, :],
                            ALU.mult, ALU.add, [f"ps{sbk}", bk], [f"sc{j}"])
                    p.op("dve", lambda e, j=j, m=m: e.tensor_reduce(out=sm[j][:, 0:2], in_=sc[j][:], axis=AX.X,
                                                                    op=ALU.max), [f"sc{j}"], [f"asm{j}a"])
                    TT(p, "dve", sm[j][:, 2:4], sm[j][:, 0:2], snk[:, 2 * m:2 * m + 2], ALU.max, [f"asm{j}a", "snk"],
                       [f"asm{j}b"])
                    TS(p, "dve", sm[j][:, 4:6], sm[j][:, 2:4], -1.0, ALU.mult, [f"asm{j}b"], [f"asm{j}c"])
                    TT(p, "dve", sm[j][:, 6:8], snk[:, 2 * m:2 * m + 2], sm[j][:, 4:6], ALU.add, ["snk", f"asm{j}c"],
                       [f"asm{j}d"])
                    ACTV(p, es[i][:, 2 * m:2 * m + 2], sm[j][:, 6:8], AF.Exp, [f"asm{j}d"], [f"es{i}"])
                    for hh in range(2):
                        ACTV(p, pb[j][:, hh, :], sc[j][:, hh, :], AF.Exp, [f"sc{j}", f"asm{j}c"], [f"pb{j}", f"rs{i}"],
                             bias=sm[j][:, 4 + hh:5 + hh], accum=rs[i][:, 2 * m + hh:2 * m + hh + 1])
                    psb = self.PS[tb_][:].bitcast(BF16)
                    for hh in range(2):
                        for half in range(2):
                            n = hh * 2 + half
                            p.op("pe", lambda e, psb=psb, n=n, j=j, hh=hh, half=half: e.transpose(
                                psb[:, n * 128:(n + 1) * 128], pb[j][:, hh, half * 128:(half + 1) * 128], self.ident[:]),
                                 [f"pb{j}", "ident"], [f"ps{tb_}"])
                    CPY(p, "act" if j == 0 else "dve", pT[j][:], psb[:, 0:512].rearrange("p (n t) -> p n t", n=4),
                        [f"ps{tb_}"], [f"pT{j}"])
                    for hh in range(2):
                        h = 2 * m + hh
                        bo = ao_banks[h // 8]
                        col = (h % 8) * 64
                        for half in range(2):
                            MM(p, self.PS[bo][:, col:col + 64], pT[j][:, hh * 2 + half, :],
                               vb[i][:, half, g * 64:(g + 1) * 64], half == 0, half == 1,
                               [f"pT{j}", f"vb{i}"], [f"ps{bo}"])
                TT(p, "dve", rs[i][:], rs[i][:], es[i][:], ALU.add, [f"rs{i}", f"es{i}"], [f"rs{i}"])
                p.op("dve", lambda e, i=i: e.reciprocal(out=rs[i][:], in_=rs[i][:]), [f"rs{i}"], [f"rs{i}"])
                for hb_ in range(2):
                    bo = ao_banks[hb_]
                    TT(p, "dve", ab[i][:, hb_ * 512:(hb_ + 1) * 512].rearrange("p (h d) -> p h d", h=8),
                       self.PS[bo][:].rearrange("p (h d) -> p h d", h=8),
                       rs[i][:, hb_ * 8:(hb_ + 1) * 8].unsqueeze(2).to_broadcast([128, 8, 64]), ALU.mult,
                       [f"ps{bo}", f"rs{i}"], [f"ab_{i}"])
                psb = self.PS[4 + i][:].bitcast(BF16)
                for k in range(8):
                    p.op("pe", lambda e, psb=psb, k=k, i=i: e.transpose(psb[:, k * 128:(k + 1) * 128],
                                                                       ab[i][:, k * 128:(k + 1) * 128], self.ident[:]),
                         [f"ab_{i}", "ident"], [f"ps{4 + i}"])
                CPY(p, "act", attnT[:, :, t0:t0 + 128], psb[:, 0:1024].rearrange("p (k t) -> p k t", k=8),
                    [f"ps{4 + i}"], ["attnT"])
                p.flush()
        if self.dbg == f"{l}D":
            p.barrier()
            for c in range(8):
                p.dma("sp", self.dbgA[c], attnT[:, c, :], ["attnT"], ["dbgA"])
            p.flush()

    def stage_E(self, l):
        p = self.p
        p.barrier()
        uactT, attnT = self.uactT, self.attnT
        with ExitStack() as st:
            wc = [self.sb(st, f"wc{i}", [128, 1024], BF16) for i in range(2)]
            wa = [self.sb(st, f"wa{i}", [128, 1024], BF16) for i in range(2)]
            gc = [self.sb(st, f"gc{i}", [128, T], BF16) for i in range(2)]
            ga = [self.sb(st, f"ga{i}", [128, T], BF16) for i in range(2)]
            mx = [self.sb(st, f"mx{i}", [128, T], BF16) for i in range(2)]
            t1 = [self.sb(st, f"e1{i}", [128, GS], F32) for i in range(2)]
            t2 = [self.sb(st, f"e2{i}", [128, GS], F32) for i in range(2)]
            for c in range(16):
                i = c % 2
                p.dma("pool", wc[i][:], self.w_co[l * 16 + c], [], [f"wc{i}"])
                p.dma("pool", wa[i][:], self.w_ao[l * 16 + c], [], [f"wa{i}"])
                p.dma("sp", gc[i][:], self.gT_s[c], ["projout"], [f"gc{i}"])
                p.dma("sp", ga[i][:], self.gT_s[16 + c], ["projout"], [f"ga{i}"])
                for g in range(NG):
                    sl = slice(g * GS, (g + 1) * GS)
                    b1, b2 = 2 * (g % 2), 2 * (g % 2) + 1
                    j = g % 2
                    for k in range(8):
                        MM(p, self.PS[b1][:, 0:GS], wc[i][:, k * 128:(k + 1) * 128], uactT[:, k, sl], k == 0, k == 7,
                           [f"wc{i}", "uactT"], [f"ps{b1}"])
                    for k in range(8):
                        MM(p, self.PS[b2][:, 0:GS], wa[i][:, k * 128:(k + 1) * 128], attnT[:, k, sl], k == 0, k == 7,
                           [f"wa{i}", "attnT"], [f"ps{b2}"])
                    TT(p, "dve", t1[j][:], self.PS[b1][:, 0:GS], gc[i][:, sl], ALU.mult, [f"ps{b1}", f"gc{i}"], [f"e1{j}"])
                    TT(p, "dve", t2[j][:], self.PS[b2][:, 0:GS], ga[i][:, sl], ALU.mult, [f"ps{b2}", f"ga{i}"], [f"e2{j}"])
                    TT(p, "pool", mx[i][:, sl], t1[j][:], t2[j][:], ALU.add, [f"e1{j}", f"e2{j}"], [f"mx{i}"])
                p.dma("sp", self.mixT_s[c], mx[i][:], [f"mx{i}"], ["mixout"])
                p.flush()
        self.lst.close()

    def stage_F(self, l):
        p = self.p
        p.barrier()
        with ExitStack() as st:
            mixT = self.sb(st, "mixT", [128, KD, T], BF16)
            wo = [self.sb(st, f"wo{i}", [128, KD * 512], BF16) for i in range(2)]
            g1 = [self.sb(st, f"g1{i}", [128, 512], F32) for i in range(2)]
            xt = [self.sb(st, f"fx{i}", [128, 512], F32) for i in range(3)]
            tm = [self.sb(st, f"ft{i}", [128, 512], F32) for i in range(2)]
            for k in range(KD):
                p.dma("sp" if k % 2 == 0 else "act", mixT[:, k, :], self.mixT_s[k], ["mixout"], ["mixT"])
            n = 0
            for nq in range(4):
                i = nq % 2
                p.dma("pool", wo[i][:], self.w_o[l * 4 + nq], [], [f"wo{i}"])
                self.load_bcast("sp", g1[i][:], self.modscr[l, 2 * D + nq * 512:2 * D + (nq + 1) * 512], f"g1{i}", ["modscr"])
                for t in range(NB):
                    bi = t % 4
                    xi = n % 3
                    n += 1
                    rows = slice(t * 128, (t + 1) * 128)
                    cols = slice(nq * 512, (nq + 1) * 512)
                    p.dma("sp", xt[xi][:], self.xsrc(l)[rows, cols], self.xkeys(t, nq), [f"fx{xi}"])
                    for k in range(KD):
                        MM(p, self.PS[bi][:], mixT[:, k, rows], wo[i][:, k * 512:(k + 1) * 512], k == 0, k == KD - 1,
                           ["mixT", f"wo{i}"], [f"ps{bi}"])
                    TT(p, "dve", tm[t % 2][:], self.PS[bi][:], g1[i][:], ALU.mult, [f"ps{bi}", f"g1{i}"], [f"ft{t % 2}"])
                    TT(p, "pool", xt[xi][:], tm[t % 2][:], xt[xi][:], ALU.add, [f"ft{t % 2}", f"fx{xi}"], [f"fx{xi}"])
                    p.dma("act", self.xres[rows, cols], xt[xi][:], [f"fx{xi}"], self.xkeys(t, nq))
                p.flush()

    def stage_G(self, l):
        p = self.p
        p.barrier()
        last = (l == L - 1)
        with ExitStack() as st:
            skb = self.sb(st, "skb", [128, 16, 128], BF16)
            p.dma("pool", skb[:], self.skT[l].rearrange("p (h n) -> p h n", h=16), [], ["skb"])
            h2T = self.sb(st, "h2T", [128, KD, TB * 128], BF16)
            E = self.sb(st, "E", [128, TB, 8, 2, 128], F32)
            pthr = self.sb(st, "pthr", [128, TB, 8], F32)
            acc = self.sb(st, "acc", [128, TB, D], F32)
            xb1 = self.sb(st, "gx0", [128, D], F32)
            xb = [xb1, xb1]
            Dh = self.sb(st, "Dh", [128, TB, 8, 128], BF16)
            hb1 = self.sb(st, "gh0", [128, D], BF16)
            hb = [hb1, hb1]
            sm = [self.sb(st, f"gsm{i}", [128, 4], F32) for i in range(2)]
            qp = [self.sb(st, f"qp{i}", [128, TB * 128], BF16) for i in range(2)]
            nm = [self.sb(st, f"nm{i}", [128, 4], F32) for i in range(2)]
            top = self.sb(st, "top", [128, 8, 2, 16], F32)
            tops = self.sb(st, "tops", [128, 8, 16], F32)
            etmp = self.sb(st, "etmp", [128, 128], F32)
            Pc = self.sb(st, "Pc", [128, 8, 16, 16], F32)
            pct = self.sb(st, "pct", [128, 16, 16], F32)
            best = self.sb(st, "best", [128, 8, 16], F32)
            Z = self.sb(st, "Z", [128, 16], F32)
            uball = self.sb(st, "uball", [128, 3, D], BF16)
            ub = [uball[:, i, :] for i in range(3)]
            vb = [[self.sb(st, f"vb{g}_{c}", [128, D], BF16) for c in range(EG)] for g in range(2)]
            gmv, shv, tmpv = acc[:, 1, :], acc[:, 2, :], acc[:, 0, :]
            g2v = Pc[:].rearrange("p h a b -> p (h a b)")
            fgv = uball[:, 0:2, :].rearrange("p a d -> p (a d)").bitcast(F32)
            gact = [self.sb(st, f"gact{i}", [128, TB * 128], F32) for i in range(3)]
            Pf = [self.sb(st, f"Pf{i}", [128, 8, 128], F32) for i in range(2)]
            Pfa = [self.sb(st, f"Pfa{i}", [128, 8, 128], F32) for i in range(2)]
            Mh = [self.sb(st, f"Mh{i}", [128, 8, 128], BF16) for i in range(2)]
            wT = [[self.sb(st, f"wT{i}_{c}", [128, TB * 128], BF16) for c in range(EG)] for i in range(2)]
            t_first = 2 if last else 1
            n_t = NB - t_first
            n_b = -(-n_t // TB)
            sizes = [n_t // n_b + (1 if i < n_t % n_b else 0) for i in range(n_b)]
            starts = [t_first + sum(sizes[:i]) for i in range(n_b)]
            for b0, ntl in zip(starts, sizes):
                nt = ntl * 128
                self.make_gm(gmv, shv, tmpv, "acc", l, self.n2g[l], 4, 3, "bc", kgm="acc", ksh="acc")
                for tl in range(ntl):
                    i = tl % 2
                    t = b0 + tl
                    p.dma("sp", xb[i][:], self.xres[t * 128:(t + 1) * 128, :], self.xkeys(t), ["gx0"])
                    self.norm_tile(xb[i][:], "gx0", gmv, shv, ["acc"], hb[i][:], "gh0", sm[i], f"gsm{i}",
                                   hb[i][:], "gh0", tmpv)
                    self.transpose_tile(hb[i], "gh0", lambda k0, tl=tl: h2T[:, k0:k0 + 8, tl * 128:(tl + 1) * 128],
                                        "h2T", 2 * i)
                if self.gstop == 5:
                    p.flush()
                    return
                for hp in range(self.g1hp):
                    i = hp % 2
                    p.dma("pool", ub[i][:], self.w_pq[l * 16 + hp], [], [f"ub{i}"])
                    bi = 4 + i
                    for k in range(KD):
                        MM(p, self.PS[bi][:, 0:nt], ub[i][:, k * 128:(k + 1) * 128], h2T[:, k, 0:nt], k == 0, k == KD - 1,
                           [f"ub{i}", "h2T"], [f"ps{bi}"])
                    CPY(p, "act", qp[i][:, 0:nt], self.PS[bi][:, 0:nt], [f"ps{bi}"], [f"qp{i}"])
                    bs = 6 + i
                    for tl in range(ntl if self.g1mode != 2 else 0):
                        if self.g1var == 1:
                            MM(p, self.PS[bs][:, tl * 128:(tl + 1) * 128], qp[i][:, tl * 128:(tl + 1) * 128], self.ident[:],
                               True, True, [f"qp{i}", "ident"], [f"ps{bs}"])
                        elif self.g1var == 2:
                            MM(p, self.PS[bs][:, tl * 128:(tl + 1) * 128], skb[:, hp, :], qp[i][:, tl * 128:(tl + 1) * 128],
                               True, True, [f"qp{i}", "skb"], [f"ps{bs}"])
                        elif self.g1var == 3:
                            MM(p, self.PS[2][:, tl * 128:(tl + 1) * 128], qp[i][:, tl * 128:(tl + 1) * 128], skb[:, hp, :],
                               True, True, [f"qp{i}", "skb"], [f"ps2"])
                        else:
                            MM(p, self.PS[bs][:, tl * 128:(tl + 1) * 128], qp[i][:, tl * 128:(tl + 1) * 128], skb[:, hp, :],
                               True, True, [f"qp{i}", "skb"], [f"ps{bs}"])
                    for tl in range(ntl if self.g1mode == 1 else 0):
                        pss = self.PS[bs][:, tl * 128:(tl + 1) * 128]
                        nmc = nm[i][:, tl:tl + 1]
                        p.op("dve", lambda e, pss=pss, nmc=nmc: e.tensor_reduce(out=nmc, in_=pss, axis=AX.X,
                                                                                op=ALU.max, negate=True),
                             [f"ps{bs}"], [f"nm{i}_{tl}"])
                        ACTV(p, E[:, tl, hp // 2, hp % 2, :], pss, AF.Exp, [f"ps{bs}", f"nm{i}_{tl}"], ["E"], bias=nmc)
                p.flush()
                if self.gstop == 1:
                    return
                for tl in range(ntl):
                    for hp in range(16):
                        h, pp = hp // 2, hp % 2
                        src = E[:, tl, h, pp, :]
                        p.op("dve", lambda e, src=src, h=h, pp=pp: e.max(out=top[:, h, pp, 0:8], in_=src), ["E"], ["top"])
                        p.op("dve", lambda e, src=src, h=h, pp=pp: e.match_replace(out=etmp[:], in_to_replace=top[:, h, pp, 0:8],
                                                                                   in_values=src, imm_value=-1.0),
                             ["E", "top"], ["etmp"])
                        p.op("dve", lambda e, h=h, pp=pp: e.max(out=top[:, h, pp, 8:16], in_=etmp[:]), ["etmp"], ["top"])

                    def top16_of_products(a_ap):
                        TT(p, "dve", Pc[:], a_ap.unsqueeze(3).to_broadcast([128, 8, 16, 16]),
                           top[:, :, 1, :].unsqueeze(2).to_broadcast([128, 8, 16, 16]), ALU.mult, ["top", "tops"], ["Pc"])
                        for h in range(8):
                            p.op("dve", lambda e, h=h: e.max(out=best[:, h, 0:8], in_=Pc[:, h]), ["Pc"], ["best"])
                            p.op("dve", lambda e, h=h: e.match_replace(out=pct[:], in_to_replace=best[:, h, 0:8],
                                                                       in_values=Pc[:, h], imm_value=-1.0),
                                 ["Pc", "best"], ["pct"])
                            p.op("dve", lambda e, h=h: e.max(out=best[:, h, 8:16], in_=pct[:]), ["pct"], ["best"])

                    top16_of_products(top[:, :, 0, :])
                    p.op("dve", lambda e: e.tensor_reduce(out=Z[:, 0:8], in_=best[:], axis=AX.X, op=ALU.add), ["best"], ["Z"])
                    p.op("dve", lambda e: e.reciprocal(out=Z[:, 8:16], in_=Z[:, 0:8]), ["Z"], ["Z"])
                    TT(p, "dve", pthr[:, tl, :], best[:, :, 15], Z[:, 8:16], ALU.mult, ["best", "Z"], ["pthr"])
                    p.op("dve", lambda e: e.reciprocal(out=Z[:, 0:8], in_=best[:, :, 15]), ["best", "Z"], ["Z"])
                    TT(p, "dve", E[:, tl, :, 0, :], E[:, tl, :, 0, :], Z[:, 0:8].unsqueeze(2).to_broadcast([128, 8, 128]),
                       ALU.mult, ["E", "Z"], ["E"])
                    TT(p, "dve", Dh[:, tl], self.ident[:].unsqueeze(1).to_broadcast([128, 8, 128]),
                       pthr[:, tl, :].unsqueeze(2).to_broadcast([128, 8, 128]), ALU.mult, ["ident", "pthr"], ["Dh"])
                p.flush()
                if self.gstop == 2:
                    return
                n_ch = (self.geg * EG) if self.gstop else NCH
                THR = 1.0 - 1e-6
                cnt = {"f": 0}

                def emit_uload(c):
                    p.dma("pool", ub[c % 3][:], self.uT[l * NCH + c], [], [f"ub{c % 3}"])

                def emit_act(c):
                    ui, gu = c % 2, c % 3
                    for k in range(KD):
                        MM(p, self.PS[ui][:, 0:nt], ub[gu][:, k * 128:(k + 1) * 128], h2T[:, k, 0:nt], k == 0, k == KD - 1,
                           [f"ub{gu}", "h2T"], [f"ps{ui}"])
                    ACTV(p, gact[gu][:, 0:nt], self.PS[ui][:, 0:nt], AF.Gelu, [f"ps{ui}"], [f"gact{gu}"])

                def emit_masks(c):
                    bc = 2 + c % 2
                    for tl in range(ntl):
                        fi = cnt["f"] % 2
                        cnt["f"] += 1
                        if tl == 2:
                            src, sk = Pfa[c % 2], f"Pfa{c % 2}"
                        else:
                            src, sk = Pf[fi], f"Pf{fi}"
                            TT(p, "pool", Pf[fi][:], E[:, tl, :, 1, :], E[:, tl, :, 0, c:c + 1].to_broadcast([128, 8, 128]),
                               ALU.mult, ["E"], [f"Pf{fi}"])
                        STT(p, Mh[fi][:], src[:], THR, src[:], ALU.is_ge, ALU.mult, [sk], [f"Mh{fi}"])
                        for h in range(8):
                            MM(p, self.PS[bc][:, tl * 128:(tl + 1) * 128], Mh[fi][:, h, :], Dh[:, tl, h, :], h == 0, h == 7,
                               [f"Mh{fi}", "Dh"], [f"ps{bc}"])

                def emit_pf_act(c):
                    if ntl < 3:
                        return
                    for h in range(8):
                        ACTV(p, Pfa[c % 2][:, h, :], E[:, 2, h, 1, :], AF.Copy, ["E"], [f"Pfa{c % 2}"],
                             scale=E[:, 2, h, 0, c:c + 1])

                def emit_wT(c):
                    eg, cc = divmod(c, EG)
                    gi, gu, bc = eg % 2, c % 3, 2 + c % 2
                    TT(p, "dve", wT[gi][cc][:, 0:nt], self.PS[bc][:, 0:nt], gact[gu][:, 0:nt], ALU.mult,
                       [f"ps{bc}", f"gact{gu}"], [f"wT{gi}_{cc}"])

                def emit_vload(eg):
                    for cc in range(EG):
                        p.dma("pool", vb[eg % 2][cc][:], self.vv[l * NCH + eg * EG + cc], [], [f"vb{eg % 2}_{cc}"])

                def emit_out_tile(eg, tl):
                    gi = eg % 2
                    for dq in range(4):
                        bo = 4 + dq
                        for cc in range(EG):
                            MM(p, self.PS[bo][:], wT[gi][cc][:, tl * 128:(tl + 1) * 128], vb[gi][cc][:, dq * 512:(dq + 1) * 512],
                               cc == 0, cc == EG - 1, [f"wT{gi}_{cc}", f"vb{gi}_{cc}"], [f"ps{bo}"])
                        dst = acc[:, tl, dq * 512:(dq + 1) * 512]
                        if eg == 0:
                            CPY(p, "act", dst, self.PS[bo][:], [f"ps{bo}"], ["acc"])
                        else:
                            TT(p, "dve", dst, self.PS[bo][:], dst, ALU.add, [f"ps{bo}", "acc"], ["acc"])

                n_g = n_ch // EG
                emit_uload(0)
                emit_uload(1)
                emit_vload(0)
                if n_g > 1:
                    emit_vload(1)
                emit_pf_act(0)
                emit_act(0)
                for c in range(n_ch):
                    eg, cc = divmod(c, EG)
                    if c + 2 < n_ch:
                        emit_uload(c + 2)
                    if c + 1 < n_ch:
                        emit_pf_act(c + 1)
                        emit_act(c + 1)
                    emit_masks(c)
                    if c >= 1:
                        emit_wT(c - 1)
                    if eg >= 1 and cc < ntl:
                        emit_out_tile(eg - 1, cc)
                    if cc == EG - 1 and eg >= 1 and eg + 1 < n_g:
                        emit_vload(eg + 1)
                    if cc == EG - 1:
                        p.flush()
                emit_wT(n_ch - 1)
                for tl in range(ntl):
                    emit_out_tile(n_g - 1, tl)
                p.flush()
                if self.gstop == 3:
                    return
                self.load_bcast("sp", g2v, self.mod_row(l, 5), "Pc", ["modscr"])
                if last:
                    self.load_bcast("sp", fgv, self.fing[0], ["ub0", "ub1"])
                for tl in range(ntl):
                    i = tl % 2
                    t = b0 + tl
                    rows = slice(t * 128, (t + 1) * 128)
                    p.dma("sp", xb[i][:], self.xres[rows, :], self.xkeys(t), ["gx0"])
                    TT(p, "dve", acc[:, tl, :], acc[:, tl, :], g2v, ALU.mult, ["acc", "Pc"], ["acc"])
                    TT(p, "pool", xb[i][:], xb[i][:], acc[:, tl, :], ALU.add, ["gx0", "acc"], ["gx0"])
                    if not last or self.dbg:
                        p.dma("act", self.xres[rows, :], xb[i][:], ["gx0"], self.xkeys(t))
                    if last and t >= 2:
                        self.norm_tile(xb[i][:], "gx0", fgv, None, ["ub0", "ub1"], acc[:, tl, :], "acc", sm[i], f"gsm{i}",
                                       hb[i][:], "gh0", None)
                        p.dma("act", self.y[(t - 2) * 128:(t - 1) * 128, :], acc[:, tl, :], ["acc"], ["y"])
                p.flush()

    def stage_A(self, l):
        self.p.barrier()
        self.lst = ExitStack()


def _t5_bucket(dist):
    max_exact = 16
    d = np.maximum(dist, 0)
    large = max_exact + (np.log(np.maximum(d, 1).astype(np.float32) / max_exact)
                         / np.float32(np.log(128 / max_exact)) * (32 - max_exact)).astype(np.int32)
    large = np.minimum(large, 31)
    return np.where(d < max_exact, d, large)


def _chunk_cols(w, ncols):
    Lx, Kd, N = w.shape
    a = w.reshape(Lx, Kd // 128, 128, N // ncols, ncols).transpose(0, 3, 2, 1, 4)
    return np.ascontiguousarray(a).reshape(Lx * (N // ncols), 128, (Kd // 128) * ncols)


_CACHE = {}


def prepare_shared(inp):
    f = lambda a: np.asarray(a, dtype=np.float32)
    w_in = f(inp["w_in"])
    sh = {}
    sh["ada_w"] = _chunk_cols(f(inp["ada_w"]), 512)
    sh["ada_b"] = f(inp["ada_b"])
    sh["n1g"] = f(inp["norm1_g"])
    sh["n2g"] = f(inp["norm2_g"])
    sh["fing"] = f(inp["final_g"]).reshape(1, D)
    sh["w_in_c"] = _chunk_cols(w_in, 128)
    kcols = w_in[:, :, 3072:3328].reshape(L, D, 4, 64)
    kd = np.concatenate([kcols, kcols], axis=3).reshape(L, D, 512)
    sh["w_kd"] = _chunk_cols(kd, 128)
    sh["w_v"] = _chunk_cols(np.ascontiguousarray(w_in[:, :, 3328:3584]), 256)
    sh["w_co"] = _chunk_cols(f(inp["w_conv_out"]), 128)
    sh["w_ao"] = _chunk_cols(f(inp["w_attn_out"]), 128)
    sh["w_o"] = _chunk_cols(f(inp["w_out"]), 512)
    sh["w_pq"] = _chunk_cols(f(inp["w_pq"]), 128)
    sk = f(inp["sub_keys"])
    sh["skT"] = np.ascontiguousarray(sk.transpose(0, 4, 1, 2, 3)).reshape(L, 128, D)
    pu = f(inp["peer_u"])
    sh["uT"] = np.ascontiguousarray(pu.reshape(L, NCH, 128, KD, 128).transpose(0, 1, 4, 3, 2)).reshape(L * NCH, 128, D)
    sh["vv"] = f(inp["peer_v"]).reshape(L * NCH, 128, D)
    cp = np.concatenate([f(inp["dw_w"]).transpose(0, 2, 1), f(inp["dw_b"])[:, :, None],
                         f(inp["conv_ln_g"])[:, :, None], f(inp["conv_ln_b"])[:, :, None]], axis=2)
    sh["chanpar"] = np.ascontiguousarray(cp.reshape(L, 8, 128, 34).transpose(0, 2, 1, 3)).reshape(L, 128, 8 * 34)
    sh["sinks"] = f(inp["attn_sinks"])
    qi = np.arange(128)[:, None] + 128
    kj = np.arange(256)[None, :]
    dist = qi - kj
    bucket = _t5_bucket(dist)
    rb = f(inp["rel_bias"])
    tab = rb[bucket]
    band = (dist >= 0) & (dist < 128)
    tab = np.where(band[:, :, None], tab, np.float32(NEG))
    sh["biasT"] = np.ascontiguousarray(tab.transpose(0, 2, 1)).reshape(128, 16 * 256)
    return sh


def per_core(inp, c):
    b, qtr = c // 4, c % 4
    s0 = qtr * 2048
    x = np.asarray(inp["x"], dtype=np.float32)
    xw = np.zeros((T, D), np.float32)
    if qtr == 0:
        xw[256:] = x[b, 0:2048]
    else:
        xw[:] = x[b, s0 - 256:s0 + 2048]
    cc = np.asarray(inp["c"], dtype=np.float32)[b]
    flags = np.zeros((128, 2), np.float32)
    flags[:, 0] = 0.0 if qtr == 0 else 1.0
    flags[:, 1] = NEG if qtr == 0 else 0.0
    return {"x_in": xw, "c_in": np.ascontiguousarray(cc.reshape(KD, 128).T), "flags": flags}


def kernel(**inputs):
    if "nc" not in _CACHE:
        _CACHE["nc"] = K().build()
    nc = _CACHE["nc"]
    sh = prepare_shared(inputs)
    in_maps = []
    for c in range(8):
        m = dict(sh)
        m.update(per_core(inputs, c))
        in_maps.append(m)
    res = run_bass_kernel_spmd(nc, in_maps, core_ids=list(range(8)))
    out = np.zeros((2, SEQ, D), np.float32)
    for c in range(8):
        b, qtr = c // 4, c % 4
        out[b, qtr * 2048:(qtr + 1) * 2048] = res.results[c]["y"]
    return out
```
